# Optimizing a Trainium2 kernel written in Bass

```python
import math
import jax, jax.numpy as jnp
from jax import lax
import numpy as np

D_MODEL = 2048
BATCH = 8
SEQ = 2048
DEPTH = 1
DEC_BATCH = 2
DEC_SEQ = 16384
PAST_LEN = 128

HEAD_DIM = 128
N_HEADS_NA = 4
N_HEADS_PER_DIL = 4
DIL_CONFIGS = ((128, 1), (512, 4), (2048, 16))
N_HEADS_DIL = N_HEADS_PER_DIL * len(DIL_CONFIGS)
W_NA = N_HEADS_NA * HEAD_DIM
W_DIL = N_HEADS_DIL * HEAD_DIM
W_DIL_OUT = N_HEADS_PER_DIL * HEAD_DIM
GRID_W = 64
NA_ROWS = 8
NA_COLS = 16
NA_Q_COLS = 16
NA_KEY_COLS = 32
ROT_DIM = HEAD_DIM // 4
ROPE_THETA = 500000.0
BAND_BLOCK = 128
N_GROUPS = 4
EXPERTS_PER_GROUP = 8
N_EXPERTS = N_GROUPS * EXPERTS_PER_GROUP
TOP_K = 2
D_EXPERT = 1024
MOE_BLOCK = 128
EPS = 1e-6
NEG = -1e30
IN_SIZES = [W_NA, W_NA, W_NA, W_DIL, W_DIL, W_DIL, D_MODEL, D_MODEL]
IN_WIDTH = sum(IN_SIZES)
IN_OFFSETS = [int(v) for v in np.cumsum(IN_SIZES)[:-1]]

kernel_name = "hybrid_natten_dilated_hmoe_encoder"


def rms_norm(x, g):
    xf = x.astype(jnp.float32)
    y = xf * lax.rsqrt(jnp.mean(xf * xf, axis=-1, keepdims=True) + EPS)
    return (y * g.astype(jnp.float32)).astype(x.dtype)


def partial_rope(x, pos):
    half = ROT_DIM // 2
    inv = 1.0 / (ROPE_THETA ** (jnp.arange(half, dtype=jnp.float32) * (2.0 / ROT_DIM)))
    ang = pos[:, None] * inv[None, :]
    cos = jnp.cos(ang)[None, :, None, :]
    sin = jnp.sin(ang)[None, :, None, :]
    xf = x.astype(jnp.float32)
    x1 = xf[..., :half]
    x2 = xf[..., half:ROT_DIM]
    out = jnp.concatenate([x1 * cos - x2 * sin, x2 * cos + x1 * sin, xf[..., ROT_DIM:]], axis=-1)
    return out.astype(x.dtype)


def neighbourhood_attention(q, k, v, rpb):
    B, T, H, hd = q.shape
    rows = T // GRID_W
    win_r = min(NA_ROWS, rows)
    r = np.arange(rows)
    rs = np.clip(r - win_r // 2, 0, rows - win_r)
    row_idx = rs[:, None] + np.arange(win_r)[None, :]
    dr_idx = row_idx - r[:, None] + (NA_ROWS - 1)
    grid = lambda t: t.reshape(B, rows, GRID_W, H, hd).transpose(0, 3, 1, 2, 4)
    qg, kg, vg = grid(q), grid(k), grid(v)
    scale = HEAD_DIM ** -0.5
    outs = []
    for qc0 in range(0, GRID_W, NA_Q_COLS):
        kc0 = min(max(qc0 - NA_COLS // 2, 0), GRID_W - NA_KEY_COLS)
        c = np.arange(qc0, qc0 + NA_Q_COLS)
        cs = np.clip(c - NA_COLS // 2, 0, GRID_W - NA_COLS)
        cc = np.arange(kc0, kc0 + NA_KEY_COLS)
        valid = (cc[None, :] >= cs[:, None]) & (cc[None, :] < cs[:, None] + NA_COLS)
        dc_idx = np.clip(cc[None, :] - c[:, None] + NA_COLS - 1, 0, 2 * NA_COLS - 2)
        kb = kg[:, :, row_idx, kc0:kc0 + NA_KEY_COLS]
        vb = vg[:, :, row_idx, kc0:kc0 + NA_KEY_COLS]
        qb = qg[:, :, :, qc0:qc0 + NA_Q_COLS]
        s = jnp.einsum('bhrqd,bhrwkd->bhrqwk', qb, kb).astype(jnp.float32) * scale
        bias = rpb[:, dr_idx[:, None, :, None], dc_idx[None, :, None, :]]
        s = s + bias[None].astype(jnp.float32)
        s = jnp.where(valid[:, None, :], s, NEG)
        p = jax.nn.softmax(s, axis=(-2, -1))
        outs.append(jnp.einsum('bhrqwk,bhrwkd->bhrqd', p.astype(v.dtype), vb))
    o = jnp.concatenate(outs, axis=3)
    return o.transpose(0, 2, 3, 1, 4).reshape(B, T, H * hd)


def band_attend(q, k, v, half):
    lead = q.shape[:-2]
    L, hd = q.shape[-2], q.shape[-1]
    blk = min(BAND_BLOCK, L)
    nb = -(-L // blk)
    Lp = nb * blk
    kb_len = blk + 2 * half
    pad_lead = [(0, 0)] * len(lead)
    qp = jnp.pad(q, pad_lead + [(0, Lp - L), (0, 0)])
    kp = jnp.pad(k, pad_lead + [(half, Lp - L + half), (0, 0)])
    vp = jnp.pad(v, pad_lead + [(half, Lp - L + half), (0, 0)])
    idx = np.arange(nb)[:, None] * blk + np.arange(kb_len)[None, :]
    kb = kp[..., idx, :]
    vb = vp[..., idx, :]
    qb = qp.reshape(lead + (nb, blk, hd))
    s = jnp.einsum('...nqd,...nkd->...nqk', qb, kb).astype(jnp.float32) * (HEAD_DIM ** -0.5)
    base = np.arange(nb)[:, None, None] * blk
    qpos = base + np.arange(blk)[None, :, None]
    kpos = base + np.arange(kb_len)[None, None, :] - half
    valid = (np.abs(kpos - qpos) <= half) & (kpos >= 0) & (kpos < L)
    s = jnp.where(valid, s, NEG)
    lse = jax.nn.logsumexp(s, axis=-1)
    p = jnp.exp(s - lse[..., None])
    o = jnp.einsum('...nqk,...nkd->...nqd', p.astype(v.dtype), vb)
    o = o.reshape(lead + (Lp, hd))[..., :L, :]
    lse = lse.reshape(lead + (Lp,))[..., :L]
    return o, lse


def dilated_attention(q, k, v):
    B, T, _, hd = q.shape
    Hg = N_HEADS_PER_DIL
    outs, lses = [], []
    for g, (window, dil) in enumerate(DIL_CONFIGS):
        half = window // 2 // dil
        L = T // dil
        regroup = lambda t: t[:, :, g * Hg:(g + 1) * Hg].reshape(B, L, dil, Hg, hd).transpose(0, 2, 3, 1, 4)
        o, lse = band_attend(regroup(q), regroup(k), regroup(v), half)
        outs.append(o.transpose(0, 3, 1, 2, 4).reshape(B, T, Hg, hd))
        lses.append(lse.transpose(0, 3, 1, 2).reshape(B, T, Hg))
    alpha = jax.nn.softmax(jnp.stack(lses, axis=0), axis=0)
    o = jnp.sum(alpha[..., None].astype(q.dtype) * jnp.stack(outs, axis=0), axis=0)
    return o.reshape(B, T, Hg * hd)


def hier_moe(h, wrg, brg, wre, bre, w_gate, w_up, w_down):
    N, D = h.shape
    glog = (h @ wrg).astype(jnp.float32) + brg.astype(jnp.float32)
    gprob = jax.nn.softmax(glog, axis=-1)
    gsel = jnp.argmax(glog, axis=-1)
    elog = (h @ wre).astype(jnp.float32).reshape(N, N_GROUPS, EXPERTS_PER_GROUP) \
        + bre.astype(jnp.float32).reshape(N_GROUPS, EXPERTS_PER_GROUP)
    elog_sel = jnp.take_along_axis(elog, gsel[:, None, None], axis=1)[:, 0]
    eprob = jax.nn.softmax(elog_sel, axis=-1)
    topv, topi = lax.top_k(eprob, TOP_K)
    gp = jnp.take_along_axis(gprob, gsel[:, None], axis=1)
    gate = gp * topv / jnp.sum(topv, axis=-1, keepdims=True)
    eid = (gsel[:, None] * EXPERTS_PER_GROUP + topi).reshape(-1)
    tok = jnp.arange(N * TOP_K) // TOP_K
    order = jnp.argsort(eid)
    se, st, sg = eid[order], tok[order], gate.reshape(-1)[order]
    counts = jnp.bincount(eid, length=N_EXPERTS)
    starts = jnp.cumsum(counts) - counts
    pcounts = (counts + MOE_BLOCK - 1) // MOE_BLOCK * MOE_BLOCK
    pends = jnp.cumsum(pcounts)
    pstarts = pends - pcounts
    pos = pstarts[se] + jnp.arange(N * TOP_K) - starts[se]
    nblk = -(-(N * TOP_K) // MOE_BLOCK) + N_EXPERTS
    P = nblk * MOE_BLOCK
    xbuf = jnp.zeros((P, D), h.dtype).at[pos].set(h[st])
    bexp = jnp.minimum(jnp.searchsorted(pends, jnp.arange(nblk) * MOE_BLOCK, side='right'), N_EXPERTS - 1)

    def expert_block(args):
        xb, e = args
        a = xb @ w_gate[e]
        u = xb @ w_up[e]
        return (jax.nn.silu(a) * u) @ w_down[e]

    ybuf = lax.map(expert_block, (xbuf.reshape(nblk, MOE_BLOCK, D), bexp)).reshape(P, D)
    return jnp.zeros((N, D), h.dtype).at[st].add(ybuf[pos] * sg[:, None].astype(h.dtype))


def encoder_layer(x, norm1_g, w_in, qn_a, kn_a, rpb_a, qn_b, kn_b, w_branch_a, w_branch_b, w_out,
                  norm2_g, router_group_w, router_group_b, router_expert_w, router_expert_b,
                  w_gate, w_up, w_down):
    B, T, D = x.shape
    h = rms_norm(x, norm1_g)
    proj = h @ w_in
    qa, ka, va, qb, kb, vb, ga, gb = jnp.split(proj, IN_OFFSETS, axis=-1)
    heads = lambda t, n: t.reshape(B, T, n, HEAD_DIM)
    qa = rms_norm(heads(qa, N_HEADS_NA), qn_a)
    ka = rms_norm(heads(ka, N_HEADS_NA), kn_a)
    ya = neighbourhood_attention(qa, ka, heads(va, N_HEADS_NA), rpb_a) @ w_branch_a
    pos = jnp.arange(T, dtype=jnp.float32)
    qb = partial_rope(rms_norm(heads(qb, N_HEADS_DIL), qn_b), pos)
    kb = partial_rope(rms_norm(heads(kb, N_HEADS_DIL), kn_b), pos)
    yb = dilated_attention(qb, kb, heads(vb, N_HEADS_DIL)) @ w_branch_b
    mix = jax.nn.sigmoid(ga) * ya + jax.nn.sigmoid(gb) * yb
    x = x + mix @ w_out
    h2 = rms_norm(x, norm2_g).reshape(B * T, D)
    y = hier_moe(h2, router_group_w, router_group_b, router_expert_w, router_expert_b, w_gate, w_up, w_down)
    return x + y.reshape(B, T, D)


def setup_inputs(seed: int = 0) -> dict:
    key = jax.random.key(seed)
    ks = jax.random.split(key, 24)
    nrm = lambda k, shape, scale: jax.random.normal(k, shape, jnp.float32) * scale
    return {
        "x_prompt": nrm(ks[0], (BATCH, SEQ, D_MODEL), 1.0),
        "x_sample": nrm(ks[1], (DEC_BATCH, DEC_SEQ, D_MODEL), 1.0),
        "norm1_g": 1.0 + nrm(ks[2], (DEPTH, D_MODEL), 0.02),
        "w_in": nrm(ks[3], (DEPTH, D_MODEL, IN_WIDTH), D_MODEL ** -0.5),
        "qn_a": 1.0 + nrm(ks[4], (DEPTH, HEAD_DIM), 0.02),
        "kn_a": 1.0 + nrm(ks[5], (DEPTH, HEAD_DIM), 0.02),
        "rpb_a": nrm(ks[6], (DEPTH, N_HEADS_NA, 2 * NA_ROWS - 1, 2 * NA_COLS - 1), 0.1),
        "qn_b": 1.0 + nrm(ks[7], (DEPTH, HEAD_DIM), 0.02),
        "kn_b": 1.0 + nrm(ks[8], (DEPTH, HEAD_DIM), 0.02),
        "w_branch_a": nrm(ks[9], (DEPTH, W_NA, D_MODEL), W_NA ** -0.5),
        "w_branch_b": nrm(ks[10], (DEPTH, W_DIL_OUT, D_MODEL), W_DIL_OUT ** -0.5),
        "w_out": nrm(ks[11], (DEPTH, D_MODEL, D_MODEL), D_MODEL ** -0.5),
        "norm2_g": 1.0 + nrm(ks[12], (DEPTH, D_MODEL), 0.02),
        "router_group_w": nrm(ks[13], (DEPTH, D_MODEL, N_GROUPS), D_MODEL ** -0.5),
        "router_group_b": nrm(ks[14], (DEPTH, N_GROUPS), 0.01),
        "router_expert_w": nrm(ks[15], (DEPTH, D_MODEL, N_EXPERTS), D_MODEL ** -0.5),
        "router_expert_b": nrm(ks[16], (DEPTH, N_EXPERTS), 0.01),
        "w_gate": nrm(ks[17], (DEPTH, N_EXPERTS, D_MODEL, D_EXPERT), D_MODEL ** -0.5),
        "w_up": nrm(ks[18], (DEPTH, N_EXPERTS, D_MODEL, D_EXPERT), D_MODEL ** -0.5),
        "w_down": nrm(ks[19], (DEPTH, N_EXPERTS, D_EXPERT, D_MODEL), D_EXPERT ** -0.5),
    }


def reference(x_prompt, x_sample, norm1_g, w_in, qn_a, kn_a, rpb_a, qn_b, kn_b, w_branch_a,
              w_branch_b, w_out, norm2_g, router_group_w, router_group_b, router_expert_w,
              router_expert_b, w_gate, w_up, w_down):
    y_prompt = x_prompt
    y_sample = x_sample
    for l in range(DEPTH):
        args = (norm1_g[l], w_in[l], qn_a[l], kn_a[l], rpb_a[l], qn_b[l], kn_b[l], w_branch_a[l],
                w_branch_b[l], w_out[l], norm2_g[l], router_group_w[l], router_group_b[l],
                router_expert_w[l], router_expert_b[l], w_gate[l], w_up[l], w_down[l])
        y_prompt = encoder_layer(y_prompt, *args)
        y_sample = encoder_layer(y_sample, *args)
    return (y_prompt, y_sample)
```

```python
import math
from contextlib import ExitStack
import numpy as np
import concourse.bass as bass
import concourse.mybir as mybir
from concourse.bass_utils import run_bass_kernel_spmd

F32 = mybir.dt.float32
BF16 = mybir.dt.bfloat16
I32 = mybir.dt.int32
ALU = mybir.AluOpType
AF = mybir.ActivationFunctionType
AX = mybir.AxisListType

D = 2048
HD = 128
U = 2048
WIN = 4096
EPS = 1e-6
SCALE = HD ** -0.5
NEXP = 32
DEXP = 1024
DILS = (1, 4, 16)

CH = 16000
NDSEM = 24
GAP = 2


class Buf:
    __slots__ = ("name", "w", "r")

    def __init__(self, name):
        self.name = name
        self.w = None
        self.r = {}


class Prog:
    ENGS = ("pe", "act", "dve", "pool", "sp")

    def __init__(self, nc):
        self.nc = nc
        self.streams = {e: [] for e in self.ENGS}
        self.cnt = {e: 0 for e in self.ENGS}
        self.dcnt = {e: 0 for e in self.ENGS}
        self.waited = {e: {} for e in self.ENGS}
        self.semobjs = {}
        self.outstanding = []
        self.deferred = []
        self._cms = []

    def _sem(self, key):
        if key not in self.semobjs:
            cm = self.nc.semaphore("s_%s_%s_%d" % key)
            self._cms.append(cm)
            self.semobjs[key] = cm.__enter__()
        return self.semobjs[key]

    def _cref(self, eng, k):
        return (("c", eng, k // CH), k % CH + 1)

    def _add_wait(self, eng, waits, ref):
        key, val = ref
        if self.waited[eng].get(key, 0) >= val:
            return
        self.waited[eng][key] = val
        waits.append((key, val))

    def _deps(self, eng, reads, writes):
        refs = []
        for b in reads:
            if b.w is not None:
                refs.append(b.w)
        for b in writes:
            if b.w is not None:
                refs.append(b.w)
            refs.extend(b.r.items())
        return refs

    def _commit(self, ref, reads, writes):
        for b in reads:
            if b.r.get(ref[0], 0) < ref[1]:
                b.r[ref[0]] = ref[1]
        for b in writes:
            b.w = ref
            b.r = {}

    def op(self, eng, fn, reads=(), writes=()):
        waits = []
        k = self.cnt[eng]
        for ref in self._deps(eng, reads, writes):
            if ref[0][0] == "c" and ref[0][1] == eng:
                if eng == "pe":
                    continue
                if eng in ("dve", "act") and k - (ref[0][2] * CH + ref[1] - 1) >= GAP:
                    continue
            self._add_wait(eng, waits, ref)
        self.cnt[eng] += 1
        ref = self._cref(eng, k)
        self.streams[eng].append((waits, fn, (ref[0], 1)))
        self._commit(ref, reads, writes)
        return ref

    def dma(self, q, fn, reads=(), writes=(), defer=False):
        waits = []
        for ref in self._deps(q, reads, writes):
            self._add_wait(q, waits, ref)
        j = self.dcnt[q]
        self.dcnt[q] += 1
        key = ("d", q, j % NDSEM)
        prev = 16 * (j // NDSEM)
        if prev > 0:
            self._add_wait(q, waits, (key, prev))
        ref = (key, prev + 16)
        self.streams[q].append((waits, fn, (key, 16)))
        self._commit(ref, reads, writes)
        (self.deferred if defer else self.outstanding).append(ref)
        return ref

    def barrier(self, include_deferred=False):
        refs = []
        if include_deferred:
            refs.extend(self.deferred)
            self.deferred = []
        for e in self.ENGS:
            if self.cnt[e] > 0:
                refs.append(self._cref(e, self.cnt[e] - 1))
        refs.extend(self.outstanding)
        self.outstanding = []
        for e in self.ENGS:
            waits = []
            for ref in refs:
                self._add_wait(e, waits, ref)
            if waits:
                self.streams[e].append((waits, None, None))

    def flush(self, include_deferred=False):
        self.barrier(include_deferred)
        nc = self.nc
        for e in self.ENGS:
            for waits, fn, inc in self.streams[e]:
                for key, _ in waits:
                    self._sem(key)
                if inc is not None:
                    self._sem(inc[0])
        prog = self
        streams = self.streams
        self.streams = {e: [] for e in self.ENGS}

        def run(engname):
            def body(eng):
                for waits, fn, inc in streams[engname]:
                    for key, val in waits:
                        eng.wait_ge(prog.semobjs[key], val)
                    if fn is not None:
                        fn(eng).then_inc(prog.semobjs[inc[0]], inc[1])
            return body

        with nc.Block() as block:
            block.tensor(run("pe"))
            block.scalar(run("act"))
            block.vector(run("dve"))
            block.gpsimd(run("pool"))
            block.sync(run("sp"))

    def close(self):
        for cm in reversed(self._cms):
            cm.__exit__(None, None, None)


def _cst_layout(NU):
    off = {}
    cur = 0

    def add(name, n):
        nonlocal cur
        off[name] = (cur, n)
        cur += n
    add("ident", 128)
    add("ones", 128)
    add("onesm", 128)
    add("ustrict", 128)
    add("rm", 32)
    add("r2m", 64)
    add("ssel", 32)
    add("j2", 128)
    add("band4", 512)
    add("colvalid", 128)
    add("iota32", 32)
    add("iota8", 8)
    add("iota4", 4)
    add("rbias", 36)
    add("gqk", 4)
    add("negband", 256)
    add("rvint", 10)
    add("rv", NU * 16 * 14)
    add("kvb", NU * 3 * 8 * 4)
    return off, cur


def _unit_geom(c, u):
    if u == 0:
        return 2048, 0
    s0 = (c % 4) * 4096
    return 16384, s0 + (u - 1) * 2048


def _build_cst(c, NU, rbias, gqk):
    off, NC = _cst_layout(NU)
    cst = np.zeros((128, NC), np.float32)

    def put(name, arr):
        o, n = off[name]
        cst[:, o:o + n] = np.asarray(arr, np.float32).reshape(128, n)
    p = np.arange(128)
    put("ident", np.eye(128))
    put("ones", np.ones((128, 128)))
    put("onesm", np.full((128, 128), 1.0 / 128))
    put("ustrict", (p[:, None] < p[None, :]).astype(np.float32))
    rm = np.zeros((128, 32), np.float32)
    for m in range(16):
        rm[m + 16, m] = -1.0
        rm[m, m + 16] = 1.0
    put("rm", rm)
    r2m = np.zeros((128, 64), np.float32)
    for m in range(32):
        r2m[m, m] = 1.0
    r2m[:, 32:64] = rm
    put("r2m", r2m)
    ssel = np.zeros((128, 32), np.float32)
    for m in range(32):
        ssel[m, m] = 1.0
        ssel[32 + m, m] = 1.0
    put("ssel", ssel)
    j2 = np.zeros((128, 128), np.float32)
    for b in range(2):
        for q in range(64):
            j2[b * 64 + q, b * 64 + 63 - q] = 1.0
    put("j2", j2)
    bandA = (p[:, None] >= p[None, :]).astype(np.float32)
    bandB = (p[:, None] <= p[None, :]).astype(np.float32)
    put("band4", np.stack([bandA, bandB, bandA, bandB], axis=1))
    kc = np.arange(64)
    cs = np.clip(kc - 8, 0, 48)
    cv = ((kc[:, None] >= cs[None, :]) & (kc[:, None] < cs[None, :] + 16)).astype(np.float32)
    put("colvalid", np.tile(cv, (2, 2)))
    put("iota32", np.tile(np.arange(32, dtype=np.float32), (128, 1)))
    put("iota8", np.tile(np.arange(8, dtype=np.float32), (128, 1)))
    put("iota4", np.tile(np.arange(4, dtype=np.float32), (128, 1)))
    put("rbias", np.tile(rbias.reshape(1, 36), (128, 1)))
    put("gqk", gqk)
    rv = np.zeros((128, NU, 16, 7, 2), np.float32)
    kvd = np.zeros((128, NU, 3, 8, 4), np.float32)
    a = p // 64
    for u in range(NU):
        T, st = _unit_geom(c, u)
        rows = T // 64
        R0 = st // 64
        for n in range(16):
            for di in range(7):
                kr = R0 + 2 * (n + di - 3) + a
                for b in range(2):
                    qr = R0 + 2 * n + b
                    rs = min(max(qr - 4, 0), rows - 8)
                    rv[:, u, n, di, b] = ((kr >= rs) & (kr < rs + 8) & (kr >= 0) & (kr < rows)).astype(np.float32)
        for g, d in enumerate(DILS):
            nb = 16 // d
            blocks = [(r, n) for r in range(d) for n in range(nb)]
            for pi in range(8):
                for bi in range(2):
                    r, n = blocks[2 * pi + bi]
                    for ab in range(2):
                        m = n + ab
                        wp = 1024 + (m * 128 - 64 + p) * d + r
                        t = st - 1024 + wp
                        kvd[:, u, g, pi, bi * 2 + ab] = ((t >= 0) & (t < T)).astype(np.float32)
    put("rv", rv)
    put("kvb", (kvd - 1.0) * 30000.0)
    put("negband", (np.stack([bandA, bandB], axis=1) - 1.0) * 30000.0)
    rvint = np.zeros((128, 5, 2), np.float32)
    for di in range(5):
        for b in range(2):
            dd = 2 * (di - 2) + a - b
            rvint[:, di, b] = ((dd >= -4) & (dd <= 3)).astype(np.float32)
    put("rvint", rvint)
    return cst


def _rope_tables(c, NU, HALO):
    inv = 1.0 / (500000.0 ** (np.arange(16, dtype=np.float64) * (2.0 / 32)))
    tabs = []
    poss = []
    for u in range(NU):
        T, st = _unit_geom(c, u)
        poss.append(st + np.arange(U, dtype=np.float64))
    if HALO:
        s0 = (c % 4) * 4096
        poss.append(np.concatenate([s0 - 1024 + np.arange(1024.0), s0 + 4096 + np.arange(1024.0)]))
    for pos in poss:
        ang = np.float32(pos)[None, :].astype(np.float32) * inv.astype(np.float32)[:, None]
        cs_ = np.cos(ang.astype(np.float64))
        sn_ = np.sin(ang.astype(np.float64))
        tab = np.concatenate([cs_, cs_, sn_, sn_], 0)
        tabs.append(tab.astype(np.float32))
    return np.stack(tabs, 0)


def build_program(NU, CAP, HALO, debug=False):
    nc = bass.Bass("TRN2", target_bir_lowering=False)
    NT = NU * U
    NST = NU + (1 if HALO else 0)
    coff, NC = _cst_layout(NU)

    def dram_in(name, shape, dt=F32):
        return nc.dram_tensor(name, list(shape), dt, kind="ExternalInput").ap()

    x_own = dram_in("x_own", [NT, D])
    x_halo = dram_in("x_halo", [U, D]) if HALO else None
    cst_d = dram_in("cst", [128, NC])
    rope_d = dram_in("rope", [NST * 64, U])
    g12_d = dram_in("g12", [2, D])
    w_in = dram_in("w_in", [D, 10240])
    w_ab = dram_in("w_ab", [1024, D])
    w_out = dram_in("w_out", [D, D])
    wr_d = dram_in("wr", [D, 36])
    rpb_d = dram_in("rpb", [60, 31])
    w_gate = dram_in("w_gate", [NEXP * D, DEXP])
    w_up = dram_in("w_up", [NEXP * D, DEXP])
    w_down = dram_in("w_down", [NEXP * DEXP, D])
    y_out = nc.dram_tensor("y", [NT, D], F32, kind="ExternalOutput").ap()

    def scratch(name, shape, dt):
        kind = "ExternalOutput" if (debug and name in ("qT", "kTw", "vw", "gT", "oT", "x1s", "dbg")) else "Internal"
        return nc.dram_tensor(name, list(shape), dt, kind=kind).ap()

    qT = scratch("qT", [16 * 128, NT], BF16)
    kTw = scratch("kTw", [NU * 16 * 128, WIN], BF16)
    vw = scratch("vw", [NU * WIN, D], BF16)
    gT = scratch("gT", [32 * 128, NT], BF16)
    oT = scratch("oT", [8 * 128, NT], BF16)
    x1s = scratch("x1s", [NT, D], F32)
    xbuf = scratch("xbuf", [NEXP * CAP + 128, D], BF16)
    ybuf = scratch("ybuf", [NEXP * CAP + 128, D], F32)
    rpbp = scratch("rpbp", [60, 128], F32)
    dbg = scratch("dbg", [128, 64], F32) if debug else None

    P = Prog(nc)

    def cs(t, name):
        o, n = coff[name]
        return t[:, o:o + n]

    with ExitStack() as es:
        cst = es.enter_context(nc.sbuf_tensor("sb_cst", [128, NC], F32))
        identb = es.enter_context(nc.sbuf_tensor("sb_identb", [128, 128], BF16))
        onesb = es.enter_context(nc.sbuf_tensor("sb_onesb", [128, 128], BF16))
        onesmb = es.enter_context(nc.sbuf_tensor("sb_onesmb", [128, 128], BF16))
        ustrb = es.enter_context(nc.sbuf_tensor("sb_ustrb", [128, 128], BF16))
        rmb = es.enter_context(nc.sbuf_tensor("sb_rmb", [128, 32], BF16))
        r2mb = es.enter_context(nc.sbuf_tensor("sb_r2mb", [128, 64], BF16))
        sselb = es.enter_context(nc.sbuf_tensor("sb_sselb", [128, 32], BF16))
        idx_t = es.enter_context(nc.sbuf_tensor("sb_idx", [128, NT // 128, 2], I32))
        wts_t = es.enter_context(nc.sbuf_tensor("sb_wts", [128, NT // 128, 2], F32))
        B_cst = Buf("cst")
        B_cb = Buf("cstb")
        B_g = Buf("g12")
        B_idx = Buf("idx")
        B_zt = Buf("zt")
        identf = cs(cst, "ident")
        gqk = cs(cst, "gqk")

        P.dma("sp", lambda e: e.dma_start(out=cst[:], in_=cst_d[:, :]), writes=[B_cst])
        for dst, nm in ((identb, "ident"), (onesb, "ones"), (onesmb, "onesm"), (ustrb, "ustrict"), (rmb, "rm"), (r2mb, "r2m"), (sselb, "ssel")):
            P.op("dve", lambda e, dst=dst, nm=nm: e.tensor_copy(out=dst[:], in_=cs(cst, nm)), reads=[B_cst], writes=[B_cb])
        es_z = ExitStack()
        zt = es_z.enter_context(nc.sbuf_tensor("sb_zt", [128, 1, D], BF16))
        zf = es_z.enter_context(nc.sbuf_tensor("sb_zf", [128, 128], F32))
        negb = es_z.enter_context(nc.sbuf_tensor("sb_negb", [128, 2, 128], BF16))
        P.op("pool", lambda e: e.memset(zt[:], 0.0), writes=[B_zt])
        P.op("pool", lambda e: e.memset(zf[:], 0.0), writes=[B_zt])
        P.op("dve", lambda e: e.tensor_copy(out=negb[:].rearrange("p a b -> p (a b)"), in_=cs(cst, "negband")), reads=[B_cst], writes=[B_cb])
        nz = (NEXP * CAP + 128)
        r0 = 0
        while r0 < nz:
            nr = min(1024, nz - r0)
            P.dma("act", lambda e, r0=r0, nr=nr: e.dma_start(
                out=xbuf[r0:r0 + nr, :].rearrange("(t p) d -> p t d", p=128), in_=zt[:, 0:1, :].broadcast_to([128, nr // 128, D])), reads=[B_zt], defer=True)
            r0 += nr
        for (a0, a1) in ((0, 1024), (3072, 4096)):
            P.dma("sp", lambda e, a0=a0: e.dma_start(
                out=vw[a0:a0 + 1024, :].rearrange("(t p) d -> p t d", p=128), in_=zt[:, 0:1, :].broadcast_to([128, 8, D])), reads=[B_zt])
            for h in range(16):
                P.dma("sp", lambda e, a0=a0, h=h: e.dma_start(
                    out=kTw[h * 128:(h + 1) * 128, a0:a0 + 1024], in_=zt[:, 0, 0:1024]), reads=[B_zt])
        P.dma("sp", lambda e: e.dma_start(out=rpbp[0:60, :], in_=zf[0:60, :]), reads=[B_zt])
        P.flush()
        P.dma("sp", lambda e: e.dma_start(out=rpbp[0:60, 48:79], in_=rpb_d[:, :]))
        P.flush()

        with ExitStack() as es:
            g1bc = es.enter_context(nc.sbuf_tensor("sb_g1bc", [128, D], F32))
            xt = es.enter_context(nc.sbuf_tensor("sb_xt", [128, 3, D], F32))
            sqj = es.enter_context(nc.sbuf_tensor("sb_sqj", [128, D], BF16))
            hb = es.enter_context(nc.sbuf_tensor("sb_hb", [128, 3, D], BF16))
            st1 = es.enter_context(nc.sbuf_tensor("sb_st1", [128, 3, 4], F32))
            hT = es.enter_context(nc.sbuf_tensor("sb_hT", [128, 16, U], BF16))
            wt = es.enter_context(nc.sbuf_tensor("sb_wt", [128, 2, 16, 512], BF16))
            ropet = es.enter_context(nc.sbuf_tensor("sb_ropet", [64, U], F32))
            qs = es.enter_context(nc.sbuf_tensor("sb_qs", [128, 2, 512], F32))
            sq = es.enter_context(nc.sbuf_tensor("sb_sq", [128, 2, 512], BF16))
            rstd = es.enter_context(nc.sbuf_tensor("sb_rstd", [128, 2, 512], F32))
            qn = es.enter_context(nc.sbuf_tensor("sb_qn", [128, 4, 512], BF16))
            rt = es.enter_context(nc.sbuf_tensor("sb_rt", [64, 2, 512], BF16))
            vs = es.enter_context(nc.sbuf_tensor("sb_vs", [128, 2, 512], BF16))
            ptr = es.enter_context(nc.psum_tensor("ps_ptr", [128, 2, 8, 128], BF16))
            pacc = es.enter_context(nc.psum_tensor("ps_pacc", [128, 2, 512], F32))
            pm = es.enter_context(nc.psum_tensor("ps_pm", [128, 512], F32))
            prr = es.enter_context(nc.psum_tensor("ps_prr", [128, 2, 512], F32))
            B_xt = [Buf("xt0"), Buf("xt1"), Buf("xt2")]
            B_hb = [Buf("hb0"), Buf("hb1"), Buf("hb2")]
            B_st = [Buf("st0"), Buf("st1"), Buf("st2")]
            B_sqj = Buf("sqj")
            B_hT = [Buf("hT%d" % i) for i in range(16)]
            B_wt = [Buf("wt0"), Buf("wt1")]
            B_rope = Buf("rope")
            B_qs = [Buf("qs0"), Buf("qs1")]
            B_sq = [Buf("sq0"), Buf("sq1")]
            B_rstd = [Buf("rstd0"), Buf("rstd1")]
            B_qn = [Buf("qn0"), Buf("qn1"), Buf("qn2"), Buf("qn3")]
            B_rt = [Buf("rt0"), Buf("rt1")]
            B_vs = [Buf("vs0"), Buf("vs1")]
            B_ptr = [Buf("ptr0"), Buf("ptr1")]
            B_pacc = [Buf("pacc0"), Buf("pacc1")]
            B_pm = Buf("pm")
            B_prr = [Buf("prr0"), Buf("prr1")]
            cnt = {"x": 0, "tr": 0, "w": 0, "acc": 0, "q": 0, "v": 0}
            pend = []

            def pipe_push(main_fn, stages):
                main_fn()
                for st_ in pend:
                    if st_:
                        st_.pop(0)()
                pend.insert(0, [f for f in stages if f is not None])
                while pend and not pend[-1]:
                    pend.pop()

            def pipe_drain():
                while pend:
                    for st_ in pend:
                        if st_:
                            st_.pop(0)()
                    while pend and not pend[-1]:
                        pend.pop()
                    if pend and not any(pend):
                        pend.clear()
            P.dma("sp", lambda e: e.dma_start(out=g1bc[:], in_=g12_d[0, :].partition_broadcast(128)), writes=[B_g])

            def kv_dests(sti, ck):
                if sti < NU:
                    u = sti
                    d = [(u, 1024 + 512 * ck)]
                    if NU == 3 and u == 1 and ck >= 2:
                        d.append((2, 512 * (ck - 2)))
                    if NU == 3 and u == 2 and ck < 2:
                        d.append((1, 3072 + 512 * ck))
                    return d
                return [(1, 512 * ck)] if ck < 2 else [(2, 3072 + 512 * (ck - 2))]

            for sti in range(NST):
                halo = sti >= NU
                xsrc = x_halo if halo else x_own[sti * U:(sti + 1) * U, :]
                P.dma("sp", lambda e, sti=sti: e.dma_start(out=ropet[:], in_=rope_d[sti * 64:(sti + 1) * 64, :]), writes=[B_rope])
                for t in range(16):
                    xb = cnt["x"] % 3
                    cnt["x"] += 1
                    P.dma("sp", lambda e, t=t, xb=xb, xsrc=xsrc: e.dma_start(out=xt[:, xb, :], in_=xsrc[t * 128:(t + 1) * 128, :]), writes=[B_xt[xb]])
                    P.op("dve", lambda e, xb=xb: e.memset(st1[:, xb, 0:1], 0.0), writes=[B_st[xb]])
                    P.op("act", lambda e, xb=xb: e.activation(out=sqj[:], in_=xt[:, xb, :], func=AF.Square, accum_out=st1[:, xb, 0:1]),
                         reads=[B_xt[xb]], writes=[B_sqj, B_st[xb]])
                    P.op("act", lambda e, xb=xb: e.activation(out=st1[:, xb, 1:2], in_=st1[:, xb, 0:1], func=AF.Sqrt, bias=EPS, scale=1.0 / D),
                         reads=[B_st[xb]], writes=[B_st[xb]])
                    P.op("dve", lambda e, xb=xb: e.reciprocal(out=st1[:, xb, 2:3], in_=st1[:, xb, 1:2]),
                         reads=[B_st[xb]], writes=[B_st[xb]])
                    P.op("dve", lambda e, xb=xb: e.scalar_tensor_tensor(out=hb[:, xb, :], in0=xt[:, xb, :], scalar=st1[:, xb, 2:3], in1=g1bc[:], op0=ALU.mult, op1=ALU.mult),
                         reads=[B_xt[xb], B_st[xb], B_g], writes=[B_hb[xb]])
                    for half in range(2):
                        pb = cnt["tr"] % 2
                        cnt["tr"] += 1
                        for j in range(8):
                            c = half * 8 + j
                            P.op("pe", lambda e, xb=xb, c=c, j=j, pb=pb: e.transpose(out=ptr[:, pb, j, :], in_=hb[:, xb, c * 128:(c + 1) * 128], identity=identb[:]),
                                 reads=[B_hb[xb], B_cb], writes=[B_ptr[pb]])
                        eng = "act" if half == 0 else "dve"
                        if eng == "act":
                            P.op("act", lambda e, t=t, half=half, pb=pb: e.copy(out=hT[:, half * 8:(half + 1) * 8, t * 128:(t + 1) * 128], in_=ptr[:, pb, :, :]),
                                 reads=[B_ptr[pb]], writes=[B_hT[t]])
                        else:
                            P.op("dve", lambda e, t=t, half=half, pb=pb: e.tensor_copy(out=hT[:, half * 8:(half + 1) * 8, t * 128:(t + 1) * 128], in_=ptr[:, pb, :, :]),
                                 reads=[B_ptr[pb]], writes=[B_hT[t]])
                for cg in range(20):
                    kind = "q" if cg in (0, 3, 4, 5) else "k" if cg in (1, 6, 7, 8) else "v" if cg in (2, 9, 10, 11) else "g"
                    if halo and kind not in ("k", "v"):
                        continue
                    wb = cnt["w"] % 2
                    cnt["w"] += 1
                    P.dma("pool", lambda e, cg=cg, wb=wb: e.dma_start(out=wt[:, wb, :, :], in_=w_in[:, cg * 512:(cg + 1) * 512].rearrange("(c p) n -> p c n", p=128)),
                          writes=[B_wt[wb]])
                    if kind == "v":
                        pipe_drain()
                        hv0 = 0 if cg == 2 else 4 + (cg - 9) * 4
                        for t in range(16):
                            ab = cnt["acc"] % 2
                            cnt["acc"] += 1
                            for c in range(16):
                                P.op("pe", lambda e, t=t, c=c, wb=wb, ab=ab: e.matmul(out=pacc[:, ab, :], lhsT=hT[:, c, t * 128:(t + 1) * 128], rhs=wt[:, wb, c, :], start=(c == 0), stop=(c == 15)),
                                     reads=[B_hT[t], B_wt[wb]], writes=[B_pacc[ab]])
                            vb = cnt["v"] % 2
                            cnt["v"] += 1
                            if t % 2 == 0:
                                P.op("act", lambda e, ab=ab, vb=vb: e.copy(out=vs[:, vb, :], in_=pacc[:, ab, :]), reads=[B_pacc[ab]], writes=[B_vs[vb]])
                            else:
                                P.op("dve", lambda e, ab=ab, vb=vb: e.tensor_copy(out=vs[:, vb, :], in_=pacc[:, ab, :]), reads=[B_pacc[ab]], writes=[B_vs[vb]])
                            ck = t // 4
                            for (uu, wo) in kv_dests(sti, ck):
                                row = uu * WIN + wo + (t % 4) * 128
                                P.dma("sp", lambda e, row=row, hv0=hv0, vb=vb: e.dma_start(out=vw[row:row + 128, hv0 * 128:hv0 * 128 + 512], in_=vs[:, vb, :]), reads=[B_vs[vb]])
                        continue
                    for ct in range(4):
                        for ck in range(4):
                            ab = cnt["acc"] % 2
                            cnt["acc"] += 1
                            qb = cnt["q"] % 2
                            q3 = cnt["q"] % 4
                            cnt["q"] += 1

                            def main_fn(ct=ct, ck=ck, wb=wb, ab=ab):
                                for c in range(16):
                                    P.op("pe", lambda e, c=c: e.matmul(out=pacc[:, ab, :], lhsT=wt[:, wb, c, ct * 128:(ct + 1) * 128], rhs=hT[:, c, ck * 512:(ck + 1) * 512], start=(c == 0), stop=(c == 15)),
                                         reads=B_hT[ck * 4:ck * 4 + 4] + [B_wt[wb]], writes=[B_pacc[ab]])

                            if kind == "g":
                                gi = (cg - 12) * 4 + ct

                                def a_fn(ab=ab, q3=q3, gi=gi, sti=sti, ck=ck):
                                    P.op("act", lambda e: e.activation(out=qn[:, q3, :], in_=pacc[:, ab, :], func=AF.Sigmoid), reads=[B_pacc[ab]], writes=[B_qn[q3]])
                                    P.dma("sp", lambda e: e.dma_start(out=gT[gi * 128:(gi + 1) * 128, sti * U + ck * 512: sti * U + (ck + 1) * 512], in_=qn[:, q3, :]), reads=[B_qn[q3]])

                                pipe_push(main_fn, [a_fn])
                                continue
                            if cg <= 1:
                                head = ct
                                gcol = cg
                                rope = False
                            else:
                                head = 4 + ((cg - 3) if kind == "q" else (cg - 6)) * 4 + ct
                                gcol = 2 if kind == "q" else 3
                                rope = True

                            def a_fn(ab=ab, qb=qb, q3=q3, gcol=gcol):
                                P.op("act", lambda e: e.copy(out=qs[:, qb, :], in_=pacc[:, ab, :]), reads=[B_pacc[ab]], writes=[B_qs[qb]])
                                P.op("act", lambda e: e.activation(out=sq[:, qb, :], in_=pacc[:, ab, :], func=AF.Square), reads=[B_pacc[ab]], writes=[B_sq[qb]])
                                P.op("pe", lambda e: e.matmul(out=pm[:], lhsT=onesmb[:], rhs=sq[:, qb, :], start=True, stop=True), reads=[B_sq[qb], B_cb], writes=[B_pm])
                                P.op("act", lambda e: e.activation(out=rstd[:, qb, :], in_=pm[:], func=AF.Sqrt, bias=EPS, scale=1.0), reads=[B_pm], writes=[B_rstd[qb]])
                                P.op("dve", lambda e: e.reciprocal(out=rstd[:, qb, :], in_=rstd[:, qb, :]), reads=[B_rstd[qb]], writes=[B_rstd[qb]])
                                P.op("dve", lambda e: e.scalar_tensor_tensor(out=qn[:, q3, :], in0=qs[:, qb, :], scalar=gqk[:, gcol:gcol + 1], in1=rstd[:, qb, :], op0=ALU.mult, op1=ALU.mult),
                                     reads=[B_qs[qb], B_rstd[qb], B_cst], writes=[B_qn[q3]])

                            def b_fn(qb=qb, q3=q3, ck=ck):
                                P.op("pe", lambda e: e.matmul(out=prr[0:64, 0, :], lhsT=r2mb[:], rhs=qn[:, q3, :], start=True, stop=True), reads=[B_qn[q3], B_cb], writes=[B_prr[0]])
                                P.op("dve", lambda e: e.tensor_tensor(out=rt[:, qb, :], in0=prr[0:64, 0, :], in1=ropet[:, ck * 512:(ck + 1) * 512], op=ALU.mult),
                                     reads=[B_prr[0], B_rope], writes=[B_rt[qb]])

                            def c_fn(qb=qb, q3=q3, rope=rope, kind=kind, head=head, sti=sti, ck=ck):
                                if rope:
                                    P.op("pe", lambda e: e.matmul(out=prr[0:32, 1, :], lhsT=sselb[0:64, :], rhs=rt[:, qb, :], start=True, stop=True), reads=[B_rt[qb], B_cb], writes=[B_prr[1]])
                                    P.op("act", lambda e: e.copy(out=qn[0:32, q3, :], in_=prr[0:32, 1, :]), reads=[B_prr[1]], writes=[B_qn[q3]])
                                if kind == "q":
                                    P.dma("sp", lambda e: e.dma_start(out=qT[head * 128:(head + 1) * 128, sti * U + ck * 512: sti * U + (ck + 1) * 512], in_=qn[:, q3, :]), reads=[B_qn[q3]])
                                else:
                                    for (uu, wo) in kv_dests(sti, ck):
                                        r_ = (uu * 16 + head) * 128
                                        P.dma("sp", lambda e, r_=r_, wo=wo: e.dma_start(out=kTw[r_:r_ + 128, wo:wo + 512], in_=qn[:, q3, :]), reads=[B_qn[q3]])

                            pipe_push(main_fn, [a_fn, b_fn if rope else None, c_fn])
                pipe_drain()
            P.flush()

        with ExitStack() as es:
            ebt = es.enter_context(nc.sbuf_tensor("sb_ebt", [128, 4, 7, 128], F32))
            ebt2 = es.enter_context(nc.sbuf_tensor("sb_ebt2", [128, 4, 5, 128], F32))
            hbk = es.enter_context(nc.sbuf_tensor("sb_hbk", [128, 4, 128], F32))
            ebtmp = es.enter_context(nc.sbuf_tensor("sb_ebtmp", [128, 4, 128], F32))
            kT = es.enter_context(nc.sbuf_tensor("sb_kT", [128, 2, WIN], BF16))
            qTt = es.enter_context(nc.sbuf_tensor("sb_qTt", [128, 2, U], BF16))
            vt = es.enter_context(nc.sbuf_tensor("sb_vt", [128, 2, 32, 128], BF16))
            eS = es.enter_context(nc.sbuf_tensor("sb_eS", [128, 2, 1024], F32))
            eS2 = es.enter_context(nc.sbuf_tensor("sb_eS2", [128, 2, 1024], F32))
            pT = es.enter_context(nc.sbuf_tensor("sb_pT", [128, 2, 1024], BF16))
            acc = es.enter_context(nc.sbuf_tensor("sb_acc", [128, 2, U], F32))
            rD = es.enter_context(nc.sbuf_tensor("sb_rD", [128, U], F32))
            oTt = es.enter_context(nc.sbuf_tensor("sb_oTt", [128, 8, U], BF16))
            psS = es.enter_context(nc.psum_tensor("ps_psS", [128, 2, 1024], F32))
            psO = es.enter_context(nc.psum_tensor("ps_psO", [128, 2, 512], F32))
            psE = es.enter_context(nc.psum_tensor("ps_psE", [128, 512], F32))
            B_ebt = Buf("ebt")
            B_hbk = [Buf("hbk%d" % i) for i in range(4)]
            B_ebtmp = [Buf("ebtmp%d" % i) for i in range(4)]
            B_psE = [Buf("psE%d" % i) for i in range(4)]
            ei = 0
            for h in range(4):
                for di in range(7):
                    delta = di - 3
                    k4 = ei % 4
                    ei += 1
                    for bp in range(2):
                        for a in range(2):
                            dr = 2 * delta + a - bp + 7
                            src = bass.AP(tensor=rpbp.tensor, offset=(h * 15 + dr) * 128, ap=[[1, 64], [1, 64]])
                            P.dma("sp", lambda e, src=src, bp=bp, a=a, k4=k4: e.dma_start(out=hbk[bp * 64:(bp + 1) * 64, k4, a * 64:(a + 1) * 64], in_=src), writes=[B_hbk[k4]])
                    P.op("pe", lambda e, k4=k4: e.matmul(out=psE[:, k4 * 128:(k4 + 1) * 128], lhsT=hbk[:, k4, :], rhs=cs(cst, "j2"), start=True, stop=True), reads=[B_hbk[k4], B_cst], writes=[B_psE[k4]])
                    P.op("act", lambda e, k4=k4: e.activation(out=ebtmp[:, k4, :], in_=psE[:, k4 * 128:(k4 + 1) * 128], func=AF.Exp), reads=[B_psE[k4]], writes=[B_ebtmp[k4]])
                    P.op("dve", lambda e, h=h, di=di, k4=k4: e.tensor_tensor(out=ebt[:, h, di, :], in0=ebtmp[:, k4, :], in1=cs(cst, "colvalid"), op=ALU.mult), reads=[B_ebtmp[k4], B_cst], writes=[B_ebt])
            rio = coff["rvint"][0]
            for h in range(4):
                P.op("dve", lambda e, h=h: e.tensor_tensor(out=ebt2[:, h, :, :].rearrange("p a (b c) -> p (a b) c", c=64), in0=ebt[:, h, 1:6, :].rearrange("p a (b c) -> p (a b) c", c=64),
                                                          in1=cst[:, rio:rio + 10].unsqueeze(2).broadcast_to([128, 10, 64]), op=ALU.mult), reads=[B_ebt, B_cst], writes=[B_ebt])

            B_kT = [Buf("kT0"), Buf("kT1")]
            B_qT = [Buf("qT0"), Buf("qT1")]
            B_vt = [Buf("vt0"), Buf("vt1")]
            B_psQ = [Buf("psQ%d" % i) for i in range(4)]
            B_pTQ = [Buf("pTQ%d" % i) for i in range(4)]
            B_psS = [[B_psQ[0], B_psQ[1]], [B_psQ[2], B_psQ[3]]]
            B_psO = [Buf("psO0"), Buf("psO1")]
            B_eS = [Buf("eS0"), Buf("eS1")]
            B_eS2 = [Buf("eS20"), Buf("eS21")]
            B_pT = [[B_pTQ[0], B_pTQ[1]], [B_pTQ[2], B_pTQ[3]]]
            B_oTn = [[Buf("oT%d_%d" % (j, n)) for n in range(16)] for j in range(8)]
            ac = {"h": 0, "s": 0, "o": 0, "q4": 0}
            rvo = coff["rv"][0]
            kbo = coff["kvb"][0]
            pend2 = []

            def push2(a_fn, b_fn, depth=1):
                a_fn()
                pend2.append(b_fn)
                while len(pend2) > depth:
                    pend2.pop(0)()

            def drain2():
                while pend2:
                    pend2.pop(0)()

            for u in range(NU):
                for h in range(4):
                    hbf = ac["h"] % 2
                    ac["h"] += 1
                    krow = (u * 16 + h) * 128
                    P.dma("sp", lambda e, krow=krow, hbf=hbf: e.dma_start(out=kT[:, hbf, 640:3456], in_=kTw[krow:krow + 128, 640:3456]), writes=[B_kT[hbf]])
                    P.dma("sp", lambda e, h=h, u=u, hbf=hbf: e.dma_start(out=qTt[:, hbf, :], in_=qT[h * 128:(h + 1) * 128, u * U:(u + 1) * U]), writes=[B_qT[hbf]])
                    P.dma("sp", lambda e, h=h, u=u, hbf=hbf: e.dma_start(out=vt[:, hbf, 0:22, :], in_=vw[u * WIN + 640:u * WIN + 3456, h * 128:(h + 1) * 128].rearrange("(t p) d -> p t d", p=128)), writes=[B_vt[hbf]])
                    for n in range(16):
                        sb = ac["s"] % 2
                        ac["s"] += 1
                        ob = ac["o"] % 2
                        ac["o"] += 1

                        interior = 2 <= n <= 13
                        dis = list(range(1, 6)) if interior else list(range(7))
                        nd = len(dis)

                        def a_fn(u=u, h=h, n=n, hbf=hbf, sb=sb, interior=interior, dis=dis, nd=nd):
                            for j, di in enumerate(dis):
                                m = n + di
                                P.op("pe", lambda e, m=m, j=j: e.matmul(out=psS[:, sb, j * 128:(j + 1) * 128], lhsT=kT[:, hbf, 640 + m * 128:640 + (m + 1) * 128], rhs=qTt[:, hbf, n * 128:(n + 1) * 128], start=True, stop=True),
                                     reads=[B_kT[hbf], B_qT[hbf]], writes=B_psS[sb])
                            P.op("act", lambda e: e.activation(out=eS[:, sb, 0:512], in_=psS[:, sb, 0:512], func=AF.Exp, scale=SCALE), reads=B_psS[sb], writes=[B_eS[sb]])
                            P.op("act", lambda e: e.activation(out=eS[:, sb, 512:nd * 128], in_=psS[:, sb, 512:nd * 128], func=AF.Exp, scale=SCALE), reads=B_psS[sb], writes=[B_eS[sb]])
                            if interior:
                                P.op("dve", lambda e: e.tensor_tensor(out=pT[:, sb, 0:640], in0=eS[:, sb, 0:640], in1=ebt2[:, h, :, :].rearrange("p a b -> p (a b)"), op=ALU.mult),
                                     reads=[B_eS[sb], B_ebt], writes=B_pT[sb])
                                return
                            P.op("dve", lambda e: e.tensor_tensor(out=eS2[:, sb, 0:896], in0=eS[:, sb, 0:896], in1=ebt[:, h, :, :].rearrange("p a b -> p (a b)"), op=ALU.mult),
                                 reads=[B_eS[sb], B_ebt], writes=[B_eS2[sb]])
                            ro = rvo + (u * 16 + n) * 14
                            P.op("pool", lambda e: e.tensor_tensor(out=pT[:, sb, 0:896].rearrange("p (a b) -> p a b", b=64), in0=eS2[:, sb, 0:896].rearrange("p (a b) -> p a b", b=64),
                                                                  in1=cst[:, ro:ro + 14].unsqueeze(2).broadcast_to([128, 14, 64]), op=ALU.mult),
                                 reads=[B_eS2[sb], B_cst], writes=B_pT[sb])

                        def b_fn(h=h, n=n, hbf=hbf, sb=sb, ob=ob, dis=dis, nd=nd):
                            for j, di in enumerate(dis):
                                m = n + di
                                P.op("pe", lambda e, m=m, j=j: e.matmul(out=psO[:, ob, 0:128], lhsT=vt[:, hbf, m, :], rhs=pT[:, sb, j * 128:(j + 1) * 128], start=(j == 0), stop=(j == nd - 1)),
                                     reads=[B_vt[hbf]] + B_pT[sb], writes=[B_psO[ob]])
                            for j in range(nd):
                                P.op("pe", lambda e, j=j: e.matmul(out=psO[:, ob, 128:256], lhsT=onesb[:], rhs=pT[:, sb, j * 128:(j + 1) * 128], start=(j == 0), stop=(j == nd - 1)),
                                     reads=[B_cb] + B_pT[sb], writes=[B_psO[ob]])
                            B_r = Buf("rDn")
                            P.op("dve", lambda e: e.reciprocal(out=rD[:, n * 128:(n + 1) * 128], in_=psO[:, ob, 128:256]), reads=[B_psO[ob]], writes=[B_r])
                            P.op("dve", lambda e: e.tensor_tensor(out=oTt[:, h, n * 128:(n + 1) * 128], in0=psO[:, ob, 0:128], in1=rD[:, n * 128:(n + 1) * 128], op=ALU.mult),
                                 reads=[B_psO[ob], B_r], writes=[B_oTn[h][n]])

                        push2(a_fn, b_fn)
                for hs in range(4):
                    B_accb = [[Buf("acc%d_%d" % (g, i)) for i in range(16)] for g in range(3)]
                    for g, d in enumerate(DILS):
                        head = 4 + 4 * g + hs
                        nb = 16 // d
                        hbf = ac["h"] % 2
                        ac["h"] += 1
                        krow = (u * 16 + head) * 128
                        P.dma("sp", lambda e, krow=krow, hbf=hbf: e.dma_start(out=kT[:, hbf, :], in_=kTw[krow:krow + 128, :]), writes=[B_kT[hbf]])
                        P.dma("sp", lambda e, head=head, u=u, hbf=hbf: e.dma_start(out=qTt[:, hbf, :], in_=qT[head * 128:(head + 1) * 128, u * U:(u + 1) * U]), writes=[B_qT[hbf]])
                        for r in range(d):
                            base = (u * WIN + 1024 - 64 * d + r) * D + head * 128
                            src = bass.AP(tensor=vw.tensor, offset=base, ap=[[d * D, 128], [128 * d * D, nb + 1], [1, 128]])
                            P.dma("sp", lambda e, src=src, r=r, nb=nb, hbf=hbf: e.dma_start(out=vt[:, hbf, r * (nb + 1):(r + 1) * (nb + 1), :], in_=src), writes=[B_vt[hbf]])
                        blocks = [(r, n) for r in range(d) for n in range(nb)]
                        for pi in range(8):
                            k4 = ac["q4"] % 4
                            ac["q4"] += 1
                            ob = ac["o"] % 2
                            ac["o"] += 1
                            sbh, so = k4 // 2, (k4 % 2) * 512

                            def a_fn(u=u, g=g, d=d, pi=pi, hbf=hbf, k4=k4, sbh=sbh, so=so, blocks=blocks):
                                for bi in range(2):
                                    r, n = blocks[2 * pi + bi]
                                    q0 = n * 128 * d + r
                                    for ab in range(2):
                                        m = n + ab
                                        k0 = 1024 + (m * 128 - 64) * d + r
                                        sub = bi * 2 + ab
                                        P.op("pe", lambda e, k0=k0, q0=q0, sub=sub: e.matmul(
                                            out=psS[:, sbh, so + sub * 128:so + (sub + 1) * 128],
                                            lhsT=kT[:, hbf, k0:k0 + 127 * d + 1:d], rhs=qTt[:, hbf, q0:q0 + 127 * d + 1:d], start=True, stop=False),
                                            reads=[B_kT[hbf], B_qT[hbf]], writes=[B_psQ[k4]])
                                        P.op("pe", lambda e, sub=sub, ab=ab: e.matmul(
                                            out=psS[:, sbh, so + sub * 128:so + (sub + 1) * 128], lhsT=identb[:], rhs=negb[:, ab, :], start=False, stop=True),
                                            reads=[B_cb], writes=[B_psQ[k4]])
                                ko = kbo + ((u * 3 + g) * 8 + pi) * 4
                                for sub in range(4):
                                    P.op("act", lambda e, sub=sub: e.activation(out=pT[:, sbh, so + sub * 128:so + (sub + 1) * 128], in_=psS[:, sbh, so + sub * 128:so + (sub + 1) * 128], func=AF.Exp,
                                                                                bias=cst[:, ko + sub:ko + sub + 1], scale=SCALE),
                                         reads=[B_psQ[k4], B_cst], writes=[B_pTQ[k4]])

                            def b_fn(g=g, d=d, nb=nb, pi=pi, hbf=hbf, k4=k4, sbh=sbh, so=so, ob=ob, blocks=blocks, B_accb=B_accb):
                                for bi in range(2):
                                    r, n = blocks[2 * pi + bi]
                                    for ab in range(2):
                                        m = n + ab
                                        P.op("pe", lambda e, r=r, m=m, bi=bi, ab=ab: e.matmul(
                                            out=psO[:, ob, bi * 256:bi * 256 + 128], lhsT=vt[:, hbf, r * (nb + 1) + m, :],
                                            rhs=pT[:, sbh, so + (bi * 2 + ab) * 128:so + (bi * 2 + ab + 1) * 128], start=(ab == 0), stop=(ab == 1)),
                                            reads=[B_vt[hbf], B_pTQ[k4]], writes=[B_psO[ob]])
                                    for ab in range(2):
                                        P.op("pe", lambda e, bi=bi, ab=ab: e.matmul(
                                            out=psO[:, ob, bi * 256 + 128:bi * 256 + 256], lhsT=onesb[:],
                                            rhs=pT[:, sbh, so + (bi * 2 + ab) * 128:so + (bi * 2 + ab + 1) * 128], start=(ab == 0), stop=(ab == 1)),
                                            reads=[B_cb, B_pTQ[k4]], writes=[B_psO[ob]])
                                for bi in range(2):
                                    r, n = blocks[2 * pi + bi]
                                    q0 = n * 128 * d + r
                                    src = psO[:, ob, bi * 256:(bi + 1) * 256].rearrange("p (a b) -> p a b", b=128)
                                    dst = acc[:, :, q0:q0 + 127 * d + 1:d]
                                    bb = B_accb[g][2 * pi + bi]
                                    if g == 0:
                                        P.op("dve", lambda e, src=src, dst=dst: e.tensor_copy(out=dst, in_=src), reads=[B_psO[ob]], writes=[bb])
                                    else:
                                        P.op("dve", lambda e, src=src, dst=dst: e.tensor_tensor(out=dst, in0=dst, in1=src, op=ALU.add), reads=[B_psO[ob]], writes=[bb])

                            push2(a_fn, b_fn, 2)
                    drain2()
                    allacc = [bb for gl in B_accb for bb in gl]
                    B_r = Buf("rDfull")
                    P.op("dve", lambda e: e.reciprocal(out=rD[:], in_=acc[:, 1, :]), reads=allacc, writes=[B_r])
                    P.op("dve", lambda e, hs=hs: e.tensor_tensor(out=oTt[:, 4 + hs, :], in0=acc[:, 0, :], in1=rD[:], op=ALU.mult), reads=allacc + [B_r], writes=B_oTn[4 + hs])
                for j in range(8):
                    P.dma("sp", lambda e, j=j, u=u: e.dma_start(out=oT[j * 128:(j + 1) * 128, u * U:(u + 1) * U], in_=oTt[:, j, :]), reads=B_oTn[j])
            P.flush(include_deferred=True)
        es_z.close()

        with ExitStack() as es:
            wab = es.enter_context(nc.sbuf_tensor("sb_wab", [128, 8, D], BF16))
            wo = es.enter_context(nc.sbuf_tensor("sb_wo", [128, 16, D], BF16))
            wrt = es.enter_context(nc.sbuf_tensor("sb_wrt", [128, 16, 36], F32))
            g2bc = es.enter_context(nc.sbuf_tensor("sb_g2bc", [128, D], F32))
            oc = es.enter_context(nc.sbuf_tensor("sb_oc", [128, 8, 256], BF16))
            gc = es.enter_context(nc.sbuf_tensor("sb_gc", [128, 32, 256], BF16))
            t12 = es.enter_context(nc.sbuf_tensor("sb_t12", [128, 2, 2, 256], F32))
            mix = es.enter_context(nc.sbuf_tensor("sb_mix", [128, 16, 256], BF16))
            x1 = es.enter_context(nc.sbuf_tensor("sb_x1", [128, 1, D], F32))
            h2f = es.enter_context(nc.sbuf_tensor("sb_h2f", [128, D], F32))
            h2b = es.enter_context(nc.sbuf_tensor("sb_h2b", [128, 2, D], BF16))
            h2T = es.enter_context(nc.sbuf_tensor("sb_h2T", [128, 16, 128], F32))
            rs2 = es.enter_context(nc.sbuf_tensor("sb_rs", [128, 2, 96], F32))
            lg2 = es.enter_context(nc.sbuf_tensor("sb_lg", [128, 2, 36], F32))
            ohb2 = es.enter_context(nc.sbuf_tensor("sb_ohb", [128, 2, 32], BF16))
            oh2 = es.enter_context(nc.sbuf_tensor("sb_oh", [128, 2, 3, 32], F32))
            tot = es.enter_context(nc.sbuf_tensor("sb_tot", [128, 32], F32))
            pos2 = es.enter_context(nc.sbuf_tensor("sb_pos", [128, 2, 32], F32))
            pya = es.enter_context(nc.psum_tensor("ps_pya", [128, 2, 512], F32))
            pout = es.enter_context(nc.psum_tensor("ps_pout", [128, 2, 512], F32))
            ptr3 = es.enter_context(nc.psum_tensor("ps_ptr3", [128, 2, 512], F32))
            plg = es.enter_context(nc.psum_tensor("ps_plg", [128, 512], F32))
            B_w3 = Buf("w3")
            B_oc = Buf("oc")
            B_gc = Buf("gc")
            B_t12 = [Buf("t120"), Buf("t121")]
            B_mix = Buf("mix")
            B_x3 = [Buf("x30"), Buf("x31")]
            B_x1 = [Buf("x10"), Buf("x11")]
            B_h2f = Buf("h2f")
            B_h2b = [Buf("h2b0"), Buf("h2b1")]
            B_h2T = Buf("h2T")
            B_rs = Buf("rs")
            B_lg = Buf("lg")
            B_oh = Buf("oh")
            B_tot = Buf("tot")
            B_pos = Buf("pos")
            B_pya = [Buf("pya0"), Buf("pya1")]
            B_pout = [Buf("pout0"), Buf("pout1")]
            B_ptr3 = [Buf("ptr30"), Buf("ptr31")]
            B_plg = Buf("plg")
            P.dma("pool", lambda e: e.dma_start(out=wab[:], in_=w_ab.rearrange("(c p) n -> p c n", p=128)), writes=[B_w3])
            for q4 in range(4):
                P.dma("pool", lambda e, q4=q4: e.dma_start(out=wo[:, q4 * 4:(q4 + 1) * 4, :], in_=w_out[q4 * 512:(q4 + 1) * 512, :].rearrange("(c p) n -> p c n", p=128)), writes=[B_w3])
            P.dma("sp", lambda e: e.dma_start(out=wrt[:], in_=wr_d.rearrange("(c p) n -> p c n", p=128)), writes=[B_w3])
            P.op("dve", lambda e: e.memset(tot[:], 0.0), writes=[B_tot])
            c3 = {"t": 0, "x": 0, "o": 0, "tr": 0, "h": 0}
            P.dma("sp", lambda e: e.dma_start(out=g2bc[:], in_=g12_d[1, :].partition_broadcast(128)), writes=[B_g])
            B_rs2 = [Buf("rs0"), Buf("rs1")]
            B_lg2 = [Buf("lg0"), Buf("lg1")]
            B_oh2 = [Buf("oh0"), Buf("oh1")]
            B_pos2 = [Buf("pos0"), Buf("pos1")]
            B_plg2 = [Buf("plg0"), Buf("plg1")]

            def branch_pair(row0):
                P.dma("sp", lambda e: e.dma_start(out=oc[:], in_=oT[:, row0:row0 + 256].rearrange("(j p) t -> p j t", p=128)), writes=[B_oc])
                P.dma("sp", lambda e: e.dma_start(out=gc[:], in_=gT[:, row0:row0 + 256].rearrange("(j p) t -> p j t", p=128)), writes=[B_gc])
                for ft in range(16):
                    tb = c3["t"] % 2
                    c3["t"] += 1
                    for br in range(2):
                        for c in range(4):
                            P.op("pe", lambda e, br=br, c=c, ft=ft: e.matmul(out=pya[:, br, 0:256], lhsT=wab[:, br * 4 + c, ft * 128:(ft + 1) * 128], rhs=oc[:, br * 4 + c, :], start=(c == 0), stop=(c == 3)),
                                 reads=[B_w3, B_oc], writes=[B_pya[br]])
                        P.op("dve", lambda e, br=br, ft=ft, tb=tb: e.tensor_tensor(out=t12[:, tb, br, :], in0=pya[:, br, 0:256], in1=gc[:, br * 16 + ft, :], op=ALU.mult),
                             reads=[B_pya[br], B_gc], writes=[B_t12[tb]])
                    P.op("pool", lambda e, ft=ft, tb=tb: e.tensor_tensor(out=mix[:, ft, :], in0=t12[:, tb, 0, :], in1=t12[:, tb, 1, :], op=ALU.add), reads=[B_t12[tb]], writes=[B_mix])

            def pre_part(tix, s_):
                row0 = tix * 128
                rs = rs2[:, s_, :]
                P.dma("sp", lambda e: e.dma_start(out=x1[:, 0, :], in_=x_own[row0:row0 + 128, :]), writes=[B_x1[0]])
                for cb in range(4):
                    ob = c3["o"] % 2
                    c3["o"] += 1
                    for ft in range(16):
                        P.op("pe", lambda e, ft=ft, cb=cb, ob=ob: e.matmul(out=pout[:, ob, :], lhsT=mix[:, ft, s_ * 128:(s_ + 1) * 128], rhs=wo[:, ft, cb * 512:(cb + 1) * 512], start=(ft == 0), stop=(ft == 15)),
                             reads=[B_mix, B_w3], writes=[B_pout[ob]])
                    P.op("dve", lambda e, cb=cb, ob=ob: e.tensor_tensor(out=x1[:, 0, cb * 512:(cb + 1) * 512], in0=pout[:, ob, :], in1=x1[:, 0, cb * 512:(cb + 1) * 512], op=ALU.add),
                         reads=[B_pout[ob], B_x1[0]], writes=[B_x1[0]])
                P.dma("sp", lambda e: e.dma_start(out=x1s[row0:row0 + 128, :], in_=x1[:, 0, :]), reads=[B_x1[0]])
                P.op("dve", lambda e: e.memset(rs[:, 0:1], 0.0), writes=[B_rs2[s_]])
                P.op("dve", lambda e: e.memset(rs[:, 5:6], 0.0), writes=[B_rs2[s_]])
                P.op("act", lambda e: e.activation(out=h2b[:, s_, :], in_=x1[:, 0, :], func=AF.Square, accum_out=rs[:, 0:1]), reads=[B_x1[0], B_rs2[s_]], writes=[B_h2b[s_], B_rs2[s_]])
                P.op("act", lambda e: e.activation(out=rs[:, 1:2], in_=rs[:, 0:1], func=AF.Sqrt, bias=EPS, scale=1.0 / D), reads=[B_rs2[s_]], writes=[B_rs2[s_]])
                P.op("dve", lambda e: e.reciprocal(out=rs[:, 2:3], in_=rs[:, 1:2]), reads=[B_rs2[s_]], writes=[B_rs2[s_]])
                P.op("dve", lambda e: e.scalar_tensor_tensor(out=h2f[:], in0=x1[:, 0, :], scalar=rs[:, 2:3], in1=g2bc[:], op0=ALU.mult, op1=ALU.mult),
                     reads=[B_x1[0], B_rs2[s_], B_g], writes=[B_h2f])
                P.op("act", lambda e: e.copy(out=h2b[:, s_, :], in_=h2f[:]), reads=[B_h2f], writes=[B_h2b[s_]])
                for q4 in range(4):
                    pb = c3["tr"] % 2
                    c3["tr"] += 1
                    for j in range(4):
                        c = q4 * 4 + j
                        P.op("pe", lambda e, c=c, j=j, pb=pb: e.transpose(out=ptr3[:, pb, j * 128:(j + 1) * 128], in_=h2f[:, c * 128:(c + 1) * 128], identity=identf), reads=[B_h2f, B_cst], writes=[B_ptr3[pb]])
                    P.op("act", lambda e, q4=q4, pb=pb: e.copy(out=h2T[:, q4 * 4:(q4 + 1) * 4, :], in_=ptr3[:, pb, :].rearrange("p (a b) -> p a b", b=128)), reads=[B_ptr3[pb]], writes=[B_h2T])
                for c in range(16):
                    P.op("pe", lambda e, c=c: e.matmul(out=plg[:, s_ * 256:s_ * 256 + 36], lhsT=h2T[:, c, :], rhs=wrt[:, c, :], start=(c == 0), stop=(c == 15)), reads=[B_h2T, B_w3], writes=[B_plg2[s_]])
                P.op("dve", lambda e: e.tensor_tensor(out=lg2[:, s_, :], in0=plg[:, s_ * 256:s_ * 256 + 36], in1=cs(cst, "rbias"), op=ALU.add), reads=[B_plg2[s_], B_cst], writes=[B_lg2[s_]])

            def chain1(tix, s_):
                rs = rs2[:, s_, :]
                lg = lg2[:, s_, :]
                oh = oh2[:, s_, :, :]
                ohb = ohb2[:, s_, :]
                R = [B_rs2[s_], B_lg2[s_], B_oh2[s_]]
                ops = []

                def dv(fn, extra_r=(), extra_w=()):
                    ops.append(("dve", fn, R + list(extra_r), R + list(extra_w)))
                dv(lambda e: e.reduce_max(out=rs[:, 3:4], in_=lg[:, 0:4], axis=AX.X))
                dv(lambda e: e.tensor_scalar(out=rs[:, 8:12], in0=lg[:, 0:4], scalar1=rs[:, 3:4], scalar2=None, op0=ALU.is_equal))
                dv(lambda e: e.tensor_scalar(out=rs[:, 4:5], in0=rs[:, 3:4], scalar1=-1.0, scalar2=None, op0=ALU.mult))
                ops.append(("act", lambda e: e.activation(out=rs[:, 12:16], in_=lg[:, 0:4], func=AF.Exp, bias=rs[:, 4:5], scale=1.0, accum_out=rs[:, 5:6]), R, R))
                dv(lambda e: e.reciprocal(out=rs[:, 6:7], in_=rs[:, 5:6]))
                dv(lambda e: e.tensor_scalar(out=rs[:, 16:24], in0=lg[:, 4:12], scalar1=rs[:, 8:9], scalar2=None, op0=ALU.mult))
                for g_ in range(1, 4):
                    dv(lambda e, g_=g_: e.scalar_tensor_tensor(out=rs[:, 16:24], in0=lg[:, 4 + 8 * g_:12 + 8 * g_], scalar=rs[:, 8 + g_:9 + g_], in1=rs[:, 16:24], op0=ALU.mult, op1=ALU.add))
                dv(lambda e: e.reduce_max(out=rs[:, 24:25], in_=rs[:, 16:24], axis=AX.X))
                dv(lambda e: e.tensor_scalar(out=rs[:, 32:40], in0=rs[:, 16:24], scalar1=rs[:, 24:25], scalar2=None, op0=ALU.is_equal))
                dv(lambda e: e.scalar_tensor_tensor(out=rs[:, 40:48], in0=rs[:, 32:40], scalar=-1e30, in1=rs[:, 16:24], op0=ALU.mult, op1=ALU.add))
                dv(lambda e: e.reduce_max(out=rs[:, 25:26], in_=rs[:, 40:48], axis=AX.X))
                dv(lambda e: e.tensor_scalar(out=rs[:, 48:56], in0=rs[:, 40:48], scalar1=rs[:, 25:26], scalar2=None, op0=ALU.is_equal))
                dv(lambda e: e.tensor_scalar(out=rs[:, 26:27], in0=rs[:, 24:25], scalar1=-1.0, scalar2=None, op0=ALU.mult))
                ops.append(("act", lambda e: e.activation(out=rs[:, 27:28], in_=rs[:, 25:26], func=AF.Exp, bias=rs[:, 26:27], scale=1.0), R, R))
                dv(lambda e: e.tensor_scalar(out=rs[:, 28:29], in0=rs[:, 27:28], scalar1=1.0, scalar2=None, op0=ALU.add))
                dv(lambda e: e.reciprocal(out=rs[:, 29:30], in_=rs[:, 28:29]))
                dv(lambda e: e.tensor_tensor(out=wts_t[:, tix, 0:1], in0=rs[:, 29:30], in1=rs[:, 6:7], op=ALU.mult), extra_w=[B_idx])
                dv(lambda e: e.tensor_tensor(out=wts_t[:, tix, 1:2], in0=wts_t[:, tix, 0:1], in1=rs[:, 27:28], op=ALU.mult), extra_r=[B_idx], extra_w=[B_idx])
                dv(lambda e: e.tensor_tensor(out=rs[:, 56:60], in0=rs[:, 8:12], in1=cs(cst, "iota4"), op=ALU.mult), extra_r=[B_cst])
                dv(lambda e: e.reduce_sum(out=rs[:, 60:61], in_=rs[:, 56:60], axis=AX.X))
                dv(lambda e: e.tensor_tensor(out=rs[:, 64:72], in0=rs[:, 32:40], in1=cs(cst, "iota8"), op=ALU.mult), extra_r=[B_cst])
                dv(lambda e: e.reduce_sum(out=rs[:, 61:62], in_=rs[:, 64:72], axis=AX.X))
                dv(lambda e: e.tensor_tensor(out=rs[:, 72:80], in0=rs[:, 48:56], in1=cs(cst, "iota8"), op=ALU.mult), extra_r=[B_cst])
                dv(lambda e: e.reduce_sum(out=rs[:, 62:63], in_=rs[:, 72:80], axis=AX.X))
                dv(lambda e: e.scalar_tensor_tensor(out=rs[:, 80:81], in0=rs[:, 60:61], scalar=8.0, in1=rs[:, 61:62], op0=ALU.mult, op1=ALU.add))
                dv(lambda e: e.scalar_tensor_tensor(out=rs[:, 81:82], in0=rs[:, 60:61], scalar=8.0, in1=rs[:, 62:63], op0=ALU.mult, op1=ALU.add))
                dv(lambda e: e.tensor_scalar(out=oh[:, 0, :], in0=cs(cst, "iota32"), scalar1=rs[:, 80:81], scalar2=None, op0=ALU.is_equal), extra_r=[B_cst])
                dv(lambda e: e.tensor_scalar(out=oh[:, 1, :], in0=cs(cst, "iota32"), scalar1=rs[:, 81:82], scalar2=None, op0=ALU.is_equal), extra_r=[B_cst])
                dv(lambda e: e.tensor_tensor(out=ohb, in0=oh[:, 0, :], in1=oh[:, 1, :], op=ALU.add))
                return ops

            def chain2(tix, s_):
                ohb = ohb2[:, s_, :]
                pos = pos2[:, s_, :]
                o_ = s_ * 256
                P.op("pe", lambda e: e.matmul(out=plg[:, o_ + 64:o_ + 96], lhsT=ustrb[:], rhs=ohb, start=True, stop=True), reads=[B_oh2[s_], B_cb], writes=[B_plg2[s_]])
                P.op("pe", lambda e: e.matmul(out=plg[:, o_ + 128:o_ + 160], lhsT=onesb[:], rhs=ohb, start=True, stop=True), reads=[B_oh2[s_], B_cb], writes=[B_plg2[s_]])
                P.op("dve", lambda e: e.tensor_tensor(out=pos, in0=plg[:, o_ + 64:o_ + 96], in1=tot[:], op=ALU.add), reads=[B_plg2[s_], B_tot], writes=[B_pos2[s_]])
                P.op("dve", lambda e: e.tensor_tensor(out=tot[:], in0=plg[:, o_ + 128:o_ + 160], in1=tot[:], op=ALU.add), reads=[B_plg2[s_], B_tot], writes=[B_tot])

            def chain3(tix, s_):
                rs = rs2[:, s_, :]
                oh = oh2[:, s_, :, :]
                pos = pos2[:, s_, :]
                R = [B_rs2[s_], B_oh2[s_]]
                ops = []

                def dv(fn, extra_r=(), extra_w=()):
                    ops.append(("dve", fn, R + list(extra_r), R + list(extra_w)))
                for k_ in range(2):
                    dv(lambda e, k_=k_: e.tensor_tensor(out=oh[:, 2, :], in0=oh[:, k_, :], in1=pos, op=ALU.mult), extra_r=[B_pos2[s_]])
                    dv(lambda e, k_=k_: e.reduce_sum(out=rs[:, 84 + k_:85 + k_], in_=oh[:, 2, :], axis=AX.X))
                    dv(lambda e, k_=k_: e.tensor_scalar(out=rs[:, 84 + k_:85 + k_], in0=rs[:, 84 + k_:85 + k_], scalar1=float(CAP - 1), scalar2=None, op0=ALU.min))
                    dv(lambda e, k_=k_: e.scalar_tensor_tensor(out=rs[:, 88 + k_:89 + k_], in0=rs[:, 80 + k_:81 + k_], scalar=float(CAP), in1=rs[:, 84 + k_:85 + k_], op0=ALU.mult, op1=ALU.add))
                    dv(lambda e, k_=k_: e.tensor_copy(out=idx_t[:, tix, k_:k_ + 1], in_=rs[:, 88 + k_:89 + k_]), extra_w=[B_idx])
                return ops

            def interleave(la, lb):
                for oa, ob_ in zip(la, lb):
                    P.op(oa[0], oa[1], reads=oa[2], writes=oa[3])
                    P.op(ob_[0], ob_[1], reads=ob_[2], writes=ob_[3])

            for pr in range(NT // 256):
                tA, tB = 2 * pr, 2 * pr + 1
                branch_pair(pr * 256)
                pre_part(tA, 0)
                pre_part(tB, 1)
                interleave(chain1(tA, 0), chain1(tB, 1))
                chain2(tA, 0)
                chain2(tB, 1)
                interleave(chain3(tA, 0), chain3(tB, 1))
                for (tix, s_) in ((tA, 0), (tB, 1)):
                    for k_ in range(2):
                        P.dma("pool", lambda e, k_=k_, tix=tix, s_=s_: e.indirect_dma_start(
                            out=xbuf[:, :], out_offset=bass.IndirectOffsetOnAxis(ap=idx_t[:, tix, k_:k_ + 1], axis=0),
                            in_=h2b[:, s_, :], in_offset=None),
                            reads=[B_h2b[s_], B_idx])
            if debug:
                P.dma("sp", lambda e: e.dma_start(out=dbg[:, 0:32], in_=tot[:]), reads=[B_tot])
            P.flush()

        NS = CAP // 128
        with ExitStack() as es:
            wg = es.enter_context(nc.sbuf_tensor("sb_wg", [128, 2, 16, 512], BF16))
            wu = es.enter_context(nc.sbuf_tensor("sb_wu", [128, 2, 16, 512], BF16))
            wd = es.enter_context(nc.sbuf_tensor("sb_wd", [128, 2, 4, D], BF16))
            xg = es.enter_context(nc.sbuf_tensor("sb_xg", [128, NS, D], BF16))
            xgT = es.enter_context(nc.sbuf_tensor("sb_xgT", [128, 16, CAP], BF16))
            sa = es.enter_context(nc.sbuf_tensor("sb_sa", [128, 2, CAP], F32))
            hTe = es.enter_context(nc.sbuf_tensor("sb_hTe", [128, 4, CAP], BF16))
            osb = es.enter_context(nc.sbuf_tensor("sb_osb", [128, NS, D], F32))
            ptr4 = es.enter_context(nc.psum_tensor("ps_ptr4", [128, 2, 8, 128], BF16))
            pau = es.enter_context(nc.psum_tensor("ps_pau", [128, 4, 512], F32))
            pdn = es.enter_context(nc.psum_tensor("ps_pdn", [128, 2, 512], F32))
            B_wgu = [Buf("wgu0"), Buf("wgu1")]
            B_wd = [Buf("wd0"), Buf("wd1")]
            B_xg = Buf("xg")
            B_xgT = Buf("xgT")
            B_sa = [Buf("sa0"), Buf("sa1")]
            B_hTe = Buf("hTe")
            B_osb = Buf("osb")
            B_ptr4 = [Buf("ptr40"), Buf("ptr41")]
            B_pau = [Buf("pa0"), Buf("pu0"), Buf("pa1"), Buf("pu1")]
            B_pdn = [Buf("pdn0"), Buf("pdn1")]
            c4 = {"w": 0, "tr": 0, "au": 0, "dn": 0}

            def load_w(e_, hh):
                wb = c4["w"] % 2
                c4["w"] += 1
                for (dst, src) in ((wg, w_gate), (wu, w_up)):
                    for q2 in range(2):
                        P.dma("pool", lambda e, dst=dst, src=src, e_=e_, hh=hh, wb=wb, q2=q2: e.dma_start(
                            out=dst[:, wb, q2 * 8:(q2 + 1) * 8, :], in_=src[e_ * D + q2 * 1024:e_ * D + (q2 + 1) * 1024, hh * 512:(hh + 1) * 512].rearrange("(c p) n -> p c n", p=128)),
                            writes=[B_wgu[wb]])
                P.dma("pool", lambda e, e_=e_, hh=hh, wb=wb: e.dma_start(
                    out=wd[:, wb, :, :], in_=w_down[e_ * DEXP + hh * 512:e_ * DEXP + (hh + 1) * 512, :].rearrange("(c p) n -> p c n", p=128)),
                    writes=[B_wd[wb]])
                return wb

            for e_ in range(NEXP):
                P.dma("sp", lambda e, e_=e_: e.dma_start(out=xg[:], in_=xbuf[e_ * CAP:(e_ + 1) * CAP, :].rearrange("(t p) d -> p t d", p=128)), writes=[B_xg])
                wbs = [load_w(e_, 0)]
                for st_ in range(NS):
                    for half in range(2):
                        pb = c4["tr"] % 2
                        c4["tr"] += 1
                        for j in range(8):
                            c = half * 8 + j
                            P.op("pe", lambda e, st_=st_, c=c, j=j, pb=pb: e.transpose(out=ptr4[:, pb, j, :], in_=xg[:, st_, c * 128:(c + 1) * 128], identity=identb[:]), reads=[B_xg, B_cb], writes=[B_ptr4[pb]])
                        if half == 0:
                            P.op("act", lambda e, st_=st_, half=half, pb=pb: e.copy(out=xgT[:, half * 8:(half + 1) * 8, st_ * 128:(st_ + 1) * 128], in_=ptr4[:, pb, :, :]), reads=[B_ptr4[pb]], writes=[B_xgT])
                        else:
                            P.op("dve", lambda e, st_=st_, half=half, pb=pb: e.tensor_copy(out=xgT[:, half * 8:(half + 1) * 8, st_ * 128:(st_ + 1) * 128], in_=ptr4[:, pb, :, :]), reads=[B_ptr4[pb]], writes=[B_xgT])
                for hh in range(2):
                    wb = wbs[hh]
                    if hh == 0:
                        wbs.append(load_w(e_, 1))
                    for ht in range(4):
                        ab = c4["au"] % 2
                        c4["au"] += 1
                        for (wsrc, pi_) in ((wg, 0), (wu, 1)):
                            for c in range(16):
                                P.op("pe", lambda e, wsrc=wsrc, pi_=pi_, c=c, ht=ht, wb=wb, ab=ab: e.matmul(out=pau[:, ab * 2 + pi_, 0:CAP], lhsT=wsrc[:, wb, c, ht * 128:(ht + 1) * 128], rhs=xgT[:, c, :], start=(c == 0), stop=(c == 15)),
                                     reads=[B_wgu[wb], B_xgT], writes=[B_pau[ab * 2 + pi_]])
                        P.op("act", lambda e, ab=ab: e.activation(out=sa[:, ab, :], in_=pau[:, ab * 2, 0:CAP], func=AF.Silu), reads=[B_pau[ab * 2]], writes=[B_sa[ab]])
                        P.op("dve", lambda e, ab=ab, ht=ht: e.tensor_tensor(out=hTe[:, ht, :], in0=pau[:, ab * 2 + 1, 0:CAP], in1=sa[:, ab, :], op=ALU.mult), reads=[B_pau[ab * 2 + 1], B_sa[ab]], writes=[B_hTe])
                    for st_ in range(NS):
                        for cb in range(4):
                            db = c4["dn"] % 2
                            c4["dn"] += 1
                            for ht in range(4):
                                P.op("pe", lambda e, st_=st_, cb=cb, ht=ht, wb=wb, db=db: e.matmul(out=pdn[:, db, :], lhsT=hTe[:, ht, st_ * 128:(st_ + 1) * 128], rhs=wd[:, wb, ht, cb * 512:(cb + 1) * 512], start=(ht == 0), stop=(ht == 3)),
                                     reads=[B_hTe, B_wd[wb]], writes=[B_pdn[db]])
                            if hh == 0:
                                P.op("act", lambda e, st_=st_, cb=cb, db=db: e.copy(out=osb[:, st_, cb * 512:(cb + 1) * 512], in_=pdn[:, db, :]), reads=[B_pdn[db]], writes=[B_osb])
                            else:
                                P.op("dve", lambda e, st_=st_, cb=cb, db=db: e.tensor_tensor(out=osb[:, st_, cb * 512:(cb + 1) * 512], in0=pdn[:, db, :], in1=osb[:, st_, cb * 512:(cb + 1) * 512], op=ALU.add), reads=[B_pdn[db], B_osb], writes=[B_osb])
                P.dma("sp", lambda e, e_=e_: e.dma_start(out=ybuf[e_ * CAP:(e_ + 1) * CAP, :].rearrange("(t p) d -> p t d", p=128), in_=osb[:]), reads=[B_osb])
            P.flush()

        with ExitStack() as es:
            y1 = es.enter_context(nc.sbuf_tensor("sb_y1", [128, 2, 2, D], F32))
            x5 = es.enter_context(nc.sbuf_tensor("sb_x5", [128, 2, D], F32))
            o5 = es.enter_context(nc.sbuf_tensor("sb_o5", [128, 2, D], F32))
            B_y1 = [Buf("y10"), Buf("y11")]
            B_x5 = [Buf("x50"), Buf("x51")]
            B_o5 = [Buf("o50"), Buf("o51")]
            for t in range(NT // 128):
                b = t % 2
                for k_ in range(2):
                    P.dma("pool", lambda e, t=t, k_=k_, b=b: e.indirect_dma_start(
                        out=y1[:, b, k_, :], out_offset=None, in_=ybuf[:, :],
                        in_offset=bass.IndirectOffsetOnAxis(ap=idx_t[:, t, k_:k_ + 1], axis=0)),
                        reads=[B_idx], writes=[B_y1[b]])
                P.dma("sp", lambda e, t=t, b=b: e.dma_start(out=x5[:, b, :], in_=x1s[t * 128:(t + 1) * 128, :]), writes=[B_x5[b]])
                P.op("dve", lambda e, t=t, b=b: e.scalar_tensor_tensor(out=o5[:, b, :], in0=y1[:, b, 0, :], scalar=wts_t[:, t, 0:1], in1=x5[:, b, :], op0=ALU.mult, op1=ALU.add),
                     reads=[B_y1[b], B_x5[b], B_idx], writes=[B_o5[b]])
                P.op("dve", lambda e, t=t, b=b: e.scalar_tensor_tensor(out=o5[:, b, :], in0=y1[:, b, 1, :], scalar=wts_t[:, t, 1:2], in1=o5[:, b, :], op0=ALU.mult, op1=ALU.add),
                     reads=[B_y1[b], B_o5[b], B_idx], writes=[B_o5[b]])
                P.dma("sp", lambda e, t=t, b=b: e.dma_start(out=y_out[t * 128:(t + 1) * 128, :], in_=o5[:, b, :]), reads=[B_o5[b]])
            P.flush()
    P.close()
    return nc


def _core_inputs(c, NU, HALO, x_prompt, x_sample, shared):
    xs = [x_prompt[c]]
    if NU == 3:
        s = c // 4
        s0 = (c % 4) * 4096
        xs.append(x_sample[s, s0:s0 + 4096])
    m = {"x_own": np.ascontiguousarray(np.concatenate(xs, 0))}
    if HALO:
        s = c // 4
        s0 = (c % 4) * 4096
        halo = np.zeros((2048, D), np.float32)
        if s0 > 0:
            halo[0:1024] = x_sample[s, s0 - 1024:s0]
        if s0 + 4096 < 16384:
            halo[1024:2048] = x_sample[s, s0 + 4096:s0 + 5120]
        m["x_halo"] = halo
    m["cst"] = _build_cst(c, NU, shared["rbias"], shared["gqk"])
    m["rope"] = _rope_tables(c, NU, HALO).reshape(-1, U)
    for k in ("g12", "w_in", "w_ab", "w_out", "wr", "rpb", "w_gate", "w_up", "w_down"):
        m[k] = shared[k]
    return m


def _shared_inputs(norm1_g, w_in, qn_a, kn_a, rpb_a, qn_b, kn_b, w_branch_a, w_branch_b, w_out, norm2_g,
                   router_group_w, router_group_b, router_expert_w, router_expert_b, w_gate, w_up, w_down):
    f = lambda a: np.ascontiguousarray(np.asarray(a, np.float32))
    return {
        "g12": f(np.stack([norm1_g[0], norm2_g[0]], 0)),
        "w_in": f(w_in[0]),
        "w_ab": f(np.concatenate([w_branch_a[0], w_branch_b[0]], 0)),
        "w_out": f(w_out[0]),
        "wr": f(np.concatenate([router_group_w[0], router_expert_w[0]], 1)),
        "rbias": f(np.concatenate([router_group_b[0], router_expert_b[0]], 0)),
        "gqk": f(np.stack([qn_a[0], kn_a[0], qn_b[0], kn_b[0]], 1)),
        "rpb": f(rpb_a[0].reshape(60, 31)),
        "w_gate": f(w_gate[0].reshape(NEXP * D, DEXP)),
        "w_up": f(w_up[0].reshape(NEXP * D, DEXP)),
        "w_down": f(w_down[0].reshape(NEXP * DEXP, D)),
    }


def kernel(x_prompt, x_sample, norm1_g, w_in, qn_a, kn_a, rpb_a, qn_b, kn_b, w_branch_a, w_branch_b, w_out,
           norm2_g, router_group_w, router_group_b, router_expert_w, router_expert_b, w_gate, w_up, w_down):
    x_prompt = np.asarray(x_prompt, np.float32)
    x_sample = np.asarray(x_sample, np.float32)
    shared = _shared_inputs(norm1_g, w_in, qn_a, kn_a, rpb_a, qn_b, kn_b, w_branch_a, w_branch_b, w_out, norm2_g,
                            router_group_w, router_group_b, router_expert_w, router_expert_b, w_gate, w_up, w_down)
    NU, CAP, HALO = 3, 512, True
    nc = build_program(NU, CAP, HALO)
    in_maps = [_core_inputs(c, NU, HALO, x_prompt, x_sample, shared) for c in range(8)]
    res = run_bass_kernel_spmd(nc, in_maps, core_ids=list(range(8)))
    y_prompt = np.empty((8, 2048, D), np.float32)
    y_sample = np.empty((2, 16384, D), np.float32)
    for c in range(8):
        y = res.results[c]["y"]
        y_prompt[c] = y[0:2048]
        s0 = (c % 4) * 4096
        y_sample[c // 4, s0:s0 + 4096] = y[2048:6144]
    return (y_prompt, y_sample)
```

```python
import math
from contextlib import ExitStack
import numpy as np
import concourse.bass as bass
import concourse.mybir as mybir
from concourse.bass_utils import run_bass_kernel_spmd

F32 = mybir.dt.float32
BF16 = mybir.dt.bfloat16
I32 = mybir.dt.int32
ALU = mybir.AluOpType
AF = mybir.ActivationFunctionType
AX = mybir.AxisListType

D = 2048
HD = 128
U = 2048
WIN = 4096
EPS = 1e-6
SCALE = HD ** -0.5
NEXP = 32
DEXP = 1024
DILS = (1, 4, 16)

CH = 16000
NDSEM = 24
GAP = 2


class Buf:
    __slots__ = ("name", "w", "r")

    def __init__(self, name):
        self.name = name
        self.w = None
        self.r = {}


class Prog:
    ENGS = ("pe", "act", "dve", "pool", "sp")

    def __init__(self, nc):
        self.nc = nc
        self.streams = {e: [] for e in self.ENGS}
        self.cnt = {e: 0 for e in self.ENGS}
        self.dcnt = {e: 0 for e in self.ENGS}
        self.waited = {e: {} for e in self.ENGS}
        self.semobjs = {}
        self.outstanding = []
        self.deferred = []
        self._cms = []

    def _sem(self, key):
        if key not in self.semobjs:
            cm = self.nc.semaphore("s_%s_%s_%d" % key)
            self._cms.append(cm)
            self.semobjs[key] = cm.__enter__()
        return self.semobjs[key]

    def _cref(self, eng, k):
        return (("c", eng, k // CH), k % CH + 1)

    def _add_wait(self, eng, waits, ref):
        key, val = ref
        if self.waited[eng].get(key, 0) >= val:
            return
        self.waited[eng][key] = val
        waits.append((key, val))

    def _deps(self, eng, reads, writes):
        refs = []
        for b in reads:
            if b.w is not None:
                refs.append(b.w)
        for b in writes:
            if b.w is not None:
                refs.append(b.w)
            refs.extend(b.r.items())
        return refs

    def _commit(self, ref, reads, writes):
        for b in reads:
            if b.r.get(ref[0], 0) < ref[1]:
                b.r[ref[0]] = ref[1]
        for b in writes:
            b.w = ref
            b.r = {}

    def op(self, eng, fn, reads=(), writes=()):
        waits = []
        k = self.cnt[eng]
        for ref in self._deps(eng, reads, writes):
            if ref[0][0] == "c" and ref[0][1] == eng:
                if eng == "pe":
                    continue
                if eng in ("dve", "act") and k - (ref[0][2] * CH + ref[1] - 1) >= GAP:
                    continue
            self._add_wait(eng, waits, ref)
        self.cnt[eng] += 1
        ref = self._cref(eng, k)
        self.streams[eng].append((waits, fn, (ref[0], 1)))
        self._commit(ref, reads, writes)
        return ref

    def dma(self, q, fn, reads=(), writes=(), defer=False):
        waits = []
        for ref in self._deps(q, reads, writes):
            self._add_wait(q, waits, ref)
        j = self.dcnt[q]
        self.dcnt[q] += 1
        key = ("d", q, j % NDSEM)
        prev = 16 * (j // NDSEM)
        if prev > 0:
            self._add_wait(q, waits, (key, prev))
        ref = (key, prev + 16)
        self.streams[q].append((waits, fn, (key, 16)))
        self._commit(ref, reads, writes)
        (self.deferred if defer else self.outstanding).append(ref)
        return ref

    def barrier(self, include_deferred=False):
        refs = []
        if include_deferred:
            refs.extend(self.deferred)
            self.deferred = []
        for e in self.ENGS:
            if self.cnt[e] > 0:
                refs.append(self._cref(e, self.cnt[e] - 1))
        refs.extend(self.outstanding)
        self.outstanding = []
        for e in self.ENGS:
            waits = []
            for ref in refs:
                self._add_wait(e, waits, ref)
            if waits:
                self.streams[e].append((waits, None, None))

    def flush(self, include_deferred=False):
        self.barrier(include_deferred)
        nc = self.nc
        for e in self.ENGS:
            for waits, fn, inc in self.streams[e]:
                for key, _ in waits:
                    self._sem(key)
                if inc is not None:
                    self._sem(inc[0])
        prog = self
        streams = self.streams
        self.streams = {e: [] for e in self.ENGS}

        def run(engname):
            def body(eng):
                for waits, fn, inc in streams[engname]:
                    for key, val in waits:
                        eng.wait_ge(prog.semobjs[key], val)
                    if fn is not None:
                        fn(eng).then_inc(prog.semobjs[inc[0]], inc[1])
            return body

        with nc.Block() as block:
            block.tensor(run("pe"))
            block.scalar(run("act"))
            block.vector(run("dve"))
            block.gpsimd(run("pool"))
            block.sync(run("sp"))

    def close(self):
        for cm in reversed(self._cms):
            cm.__exit__(None, None, None)


def _cst_layout(NU):
    off = {}
    cur = 0

    def add(name, n):
        nonlocal cur
        off[name] = (cur, n)
        cur += n
    add("ident", 128)
    add("ones", 128)
    add("onesm", 128)
    add("ustrict", 128)
    add("rm", 32)
    add("r2m", 64)
    add("ssel", 32)
    add("j2", 128)
    add("band4", 512)
    add("colvalid", 128)
    add("iota32", 32)
    add("iota8", 8)
    add("iota4", 4)
    add("rbias", 36)
    add("gqk", 4)
    add("negband", 256)
    add("rvint", 10)
    add("rv", NU * 16 * 14)
    add("kvb", NU * 3 * 8 * 4)
    return off, cur


def _unit_geom(c, u):
    if u == 0:
        return 2048, 0
    s0 = (c % 4) * 4096
    return 16384, s0 + (u - 1) * 2048


def _build_cst(c, NU, rbias, gqk):
    off, NC = _cst_layout(NU)
    cst = np.zeros((128, NC), np.float32)

    def put(name, arr):
        o, n = off[name]
        cst[:, o:o + n] = np.asarray(arr, np.float32).reshape(128, n)
    p = np.arange(128)
    put("ident", np.eye(128))
    put("ones", np.ones((128, 128)))
    put("onesm", np.full((128, 128), 1.0 / 128))
    put("ustrict", (p[:, None] < p[None, :]).astype(np.float32))
    rm = np.zeros((128, 32), np.float32)
    for m in range(16):
        rm[m + 16, m] = -1.0
        rm[m, m + 16] = 1.0
    put("rm", rm)
    r2m = np.zeros((128, 64), np.float32)
    for m in range(32):
        r2m[m, m] = 1.0
    r2m[:, 32:64] = rm
    put("r2m", r2m)
    ssel = np.zeros((128, 32), np.float32)
    for m in range(32):
        ssel[m, m] = 1.0
        ssel[32 + m, m] = 1.0
    put("ssel", ssel)
    j2 = np.zeros((128, 128), np.float32)
    for b in range(2):
        for q in range(64):
            j2[b * 64 + q, b * 64 + 63 - q] = 1.0
    put("j2", j2)
    bandA = (p[:, None] >= p[None, :]).astype(np.float32)
    bandB = (p[:, None] <= p[None, :]).astype(np.float32)
    put("band4", np.stack([bandA, bandB, bandA, bandB], axis=1))
    kc = np.arange(64)
    cs = np.clip(kc - 8, 0, 48)
    cv = ((kc[:, None] >= cs[None, :]) & (kc[:, None] < cs[None, :] + 16)).astype(np.float32)
    put("colvalid", np.tile(cv, (2, 2)))
    put("iota32", np.tile(np.arange(32, dtype=np.float32), (128, 1)))
    put("iota8", np.tile(np.arange(8, dtype=np.float32), (128, 1)))
    put("iota4", np.tile(np.arange(4, dtype=np.float32), (128, 1)))
    put("rbias", np.tile(rbias.reshape(1, 36), (128, 1)))
    put("gqk", gqk)
    rv = np.zeros((128, NU, 16, 7, 2), np.float32)
    kvd = np.zeros((128, NU, 3, 8, 4), np.float32)
    a = p // 64
    for u in range(NU):
        T, st = _unit_geom(c, u)
        rows = T // 64
        R0 = st // 64
        for n in range(16):
            for di in range(7):
                kr = R0 + 2 * (n + di - 3) + a
                for b in range(2):
                    qr = R0 + 2 * n + b
                    rs = min(max(qr - 4, 0), rows - 8)
                    rv[:, u, n, di, b] = ((kr >= rs) & (kr < rs + 8) & (kr >= 0) & (kr < rows)).astype(np.float32)
        for g, d in enumerate(DILS):
            nb = 16 // d
            blocks = [(r, n) for r in range(d) for n in range(nb)]
            for pi in range(8):
                for bi in range(2):
                    r, n = blocks[2 * pi + bi]
                    for ab in range(2):
                        m = n + ab
                        wp = 1024 + (m * 128 - 64 + p) * d + r
                        t = st - 1024 + wp
                        kvd[:, u, g, pi, bi * 2 + ab] = ((t >= 0) & (t < T)).astype(np.float32)
    put("rv", rv)
    put("kvb", (kvd - 1.0) * 30000.0)
    put("negband", (np.stack([bandA, bandB], axis=1) - 1.0) * 30000.0)
    rvint = np.zeros((128, 5, 2), np.float32)
    for di in range(5):
        for b in range(2):
            dd = 2 * (di - 2) + a - b
            rvint[:, di, b] = ((dd >= -4) & (dd <= 3)).astype(np.float32)
    put("rvint", rvint)
    return cst


def _rope_tables(c, NU, HALO):
    inv = 1.0 / (500000.0 ** (np.arange(16, dtype=np.float64) * (2.0 / 32)))
    tabs = []
    poss = []
    for u in range(NU):
        T, st = _unit_geom(c, u)
        poss.append(st + np.arange(U, dtype=np.float64))
    if HALO:
        s0 = (c % 4) * 4096
        poss.append(np.concatenate([s0 - 1024 + np.arange(1024.0), s0 + 4096 + np.arange(1024.0)]))
    for pos in poss:
        ang = np.float32(pos)[None, :].astype(np.float32) * inv.astype(np.float32)[:, None]
        cs_ = np.cos(ang.astype(np.float64))
        sn_ = np.sin(ang.astype(np.float64))
        tab = np.concatenate([cs_, cs_, sn_, sn_], 0)
        tabs.append(tab.astype(np.float32))
    return np.stack(tabs, 0)


def build_program(NU, CAP, HALO, debug=False):
    nc = bass.Bass("TRN2", target_bir_lowering=False)
    NT = NU * U
    NST = NU + (1 if HALO else 0)
    coff, NC = _cst_layout(NU)

    def dram_in(name, shape, dt=F32):
        return nc.dram_tensor(name, list(shape), dt, kind="ExternalInput").ap()

    x_own = dram_in("x_own", [NT, D])
    x_halo = dram_in("x_halo", [U, D]) if HALO else None
    cst_d = dram_in("cst", [128, NC])
    rope_d = dram_in("rope", [NST * 64, U])
    g12_d = dram_in("g12", [2, D])
    w_in = dram_in("w_in", [D, 10240])
    w_ab = dram_in("w_ab", [1024, D])
    w_out = dram_in("w_out", [D, D])
    wr_d = dram_in("wr", [D, 36])
    rpb_d = dram_in("rpb", [60, 31])
    w_gate = dram_in("w_gate", [NEXP * D, DEXP])
    w_up = dram_in("w_up", [NEXP * D, DEXP])
    w_down = dram_in("w_down", [NEXP * DEXP, D])
    y_out = nc.dram_tensor("y", [NT, D], F32, kind="ExternalOutput").ap()

    def scratch(name, shape, dt):
        kind = "ExternalOutput" if (debug and name in ("qT", "kTw", "vw", "gT", "oT", "x1s", "dbg")) else "Internal"
        return nc.dram_tensor(name, list(shape), dt, kind=kind).ap()

    qT = scratch("qT", [16 * 128, NT], BF16)
    kTw = scratch("kTw", [NU * 16 * 128, WIN], BF16)
    vw = scratch("vw", [NU * WIN, D], BF16)
    gT = scratch("gT", [32 * 128, NT], BF16)
    oT = scratch("oT", [8 * 128, NT], BF16)
    x1s = scratch("x1s", [NT, D], F32)
    xbuf = scratch("xbuf", [NEXP * CAP + 128, D], BF16)
    ybuf = scratch("ybuf", [NEXP * CAP + 128, D], F32)
    rpbp = scratch("rpbp", [60, 128], F32)
    dbg = scratch("dbg", [128, 64], F32) if debug else None

    P = Prog(nc)

    def cs(t, name):
        o, n = coff[name]
        return t[:, o:o + n]

    with ExitStack() as es:
        cst = es.enter_context(nc.sbuf_tensor("sb_cst", [128, NC], F32))
        identb = es.enter_context(nc.sbuf_tensor("sb_identb", [128, 128], BF16))
        onesb = es.enter_context(nc.sbuf_tensor("sb_onesb", [128, 128], BF16))
        onesmb = es.enter_context(nc.sbuf_tensor("sb_onesmb", [128, 128], BF16))
        ustrb = es.enter_context(nc.sbuf_tensor("sb_ustrb", [128, 128], BF16))
        rmb = es.enter_context(nc.sbuf_tensor("sb_rmb", [128, 32], BF16))
        r2mb = es.enter_context(nc.sbuf_tensor("sb_r2mb", [128, 64], BF16))
        sselb = es.enter_context(nc.sbuf_tensor("sb_sselb", [128, 32], BF16))
        idx_t = es.enter_context(nc.sbuf_tensor("sb_idx", [128, NT // 128, 2], I32))
        wts_t = es.enter_context(nc.sbuf_tensor("sb_wts", [128, NT // 128, 2], F32))
        B_cst = Buf("cst")
        B_cb = Buf("cstb")
        B_g = Buf("g12")
        B_idx = Buf("idx")
        B_zt = Buf("zt")
        identf = cs(cst, "ident")
        gqk = cs(cst, "gqk")

        P.dma("sp", lambda e: e.dma_start(out=cst[:], in_=cst_d[:, :]), writes=[B_cst])
        for dst, nm in ((identb, "ident"), (onesb, "ones"), (onesmb, "onesm"), (ustrb, "ustrict"), (rmb, "rm"), (r2mb, "r2m"), (sselb, "ssel")):
            P.op("dve", lambda e, dst=dst, nm=nm: e.tensor_copy(out=dst[:], in_=cs(cst, nm)), reads=[B_cst], writes=[B_cb])
        es_z = ExitStack()
        zt = es_z.enter_context(nc.sbuf_tensor("sb_zt", [128, 1, D], BF16))
        zf = es_z.enter_context(nc.sbuf_tensor("sb_zf", [128, 128], F32))
        negb = es_z.enter_context(nc.sbuf_tensor("sb_negb", [128, 2, 128], BF16))
        P.op("pool", lambda e: e.memset(zt[:], 0.0), writes=[B_zt])
        P.op("pool", lambda e: e.memset(zf[:], 0.0), writes=[B_zt])
        P.op("dve", lambda e: e.tensor_copy(out=negb[:].rearrange("p a b -> p (a b)"), in_=cs(cst, "negband")), reads=[B_cst], writes=[B_cb])
        nz = (NEXP * CAP + 128)
        r0 = 0
        while r0 < nz:
            nr = min(1024, nz - r0)
            P.dma("act", lambda e, r0=r0, nr=nr: e.dma_start(
                out=xbuf[r0:r0 + nr, :].rearrange("(t p) d -> p t d", p=128), in_=zt[:, 0:1, :].broadcast_to([128, nr // 128, D])), reads=[B_zt], defer=True)
            r0 += nr
        for (a0, a1) in ((0, 1024), (3072, 4096)):
            P.dma("sp", lambda e, a0=a0: e.dma_start(
                out=vw[a0:a0 + 1024, :].rearrange("(t p) d -> p t d", p=128), in_=zt[:, 0:1, :].broadcast_to([128, 8, D])), reads=[B_zt])
            for h in range(16):
                P.dma("sp", lambda e, a0=a0, h=h: e.dma_start(
                    out=kTw[h * 128:(h + 1) * 128, a0:a0 + 1024], in_=zt[:, 0, 0:1024]), reads=[B_zt])
        P.dma("sp", lambda e: e.dma_start(out=rpbp[0:60, :], in_=zf[0:60, :]), reads=[B_zt])
        P.flush()
        P.dma("sp", lambda e: e.dma_start(out=rpbp[0:60, 48:79], in_=rpb_d[:, :]))
        P.flush()

        with ExitStack() as es:
            g1bc = es.enter_context(nc.sbuf_tensor("sb_g1bc", [128, D], F32))
            xt = es.enter_context(nc.sbuf_tensor("sb_xt", [128, 3, D], F32))
            sqj = es.enter_context(nc.sbuf_tensor("sb_sqj", [128, D], BF16))
            hb = es.enter_context(nc.sbuf_tensor("sb_hb", [128, 3, D], BF16))
            st1 = es.enter_context(nc.sbuf_tensor("sb_st1", [128, 3, 4], F32))
            hT = es.enter_context(nc.sbuf_tensor("sb_hT", [128, 16, U], BF16))
            wt = es.enter_context(nc.sbuf_tensor("sb_wt", [128, 2, 16, 512], BF16))
            ropet = es.enter_context(nc.sbuf_tensor("sb_ropet", [64, U], F32))
            qs = es.enter_context(nc.sbuf_tensor("sb_qs", [128, 2, 512], F32))
            sq = es.enter_context(nc.sbuf_tensor("sb_sq", [128, 2, 512], BF16))
            rstd = es.enter_context(nc.sbuf_tensor("sb_rstd", [128, 2, 512], F32))
            qn = es.enter_context(nc.sbuf_tensor("sb_qn", [128, 4, 512], BF16))
            rt = es.enter_context(nc.sbuf_tensor("sb_rt", [64, 2, 512], BF16))
            vs = es.enter_context(nc.sbuf_tensor("sb_vs", [128, 2, 512], BF16))
            ptr = es.enter_context(nc.psum_tensor("ps_ptr", [128, 2, 8, 128], BF16))
            pacc = es.enter_context(nc.psum_tensor("ps_pacc", [128, 2, 512], F32))
            pm = es.enter_context(nc.psum_tensor("ps_pm", [128, 512], F32))
            prr = es.enter_context(nc.psum_tensor("ps_prr", [128, 2, 512], F32))
            B_xt = [Buf("xt0"), Buf("xt1"), Buf("xt2")]
            B_hb = [Buf("hb0"), Buf("hb1"), Buf("hb2")]
            B_st = [Buf("st0"), Buf("st1"), Buf("st2")]
            B_sqj = Buf("sqj")
            B_hT = [Buf("hT%d" % i) for i in range(16)]
            B_wt = [Buf("wt0"), Buf("wt1")]
            B_rope = Buf("rope")
            B_qs = [Buf("qs0"), Buf("qs1")]
            B_sq = [Buf("sq0"), Buf("sq1")]
            B_rstd = [Buf("rstd0"), Buf("rstd1")]
            B_qn = [Buf("qn0"), Buf("qn1"), Buf("qn2"), Buf("qn3")]
            B_rt = [Buf("rt0"), Buf("rt1")]
            B_vs = [Buf("vs0"), Buf("vs1")]
            B_ptr = [Buf("ptr0"), Buf("ptr1")]
            B_pacc = [Buf("pacc0"), Buf("pacc1")]
            B_pm = Buf("pm")
            B_prr = [Buf("prr0"), Buf("prr1")]
            cnt = {"x": 0, "tr": 0, "w": 0, "acc": 0, "q": 0, "v": 0}
            pend = []

            def pipe_push(main_fn, stages):
                main_fn()
                for st_ in pend:
                    if st_:
                        st_.pop(0)()
                pend.insert(0, [f for f in stages if f is not None])
                while pend and not pend[-1]:
                    pend.pop()

            def pipe_drain():
                while pend:
                    for st_ in pend:
                        if st_:
                            st_.pop(0)()
                    while pend and not pend[-1]:
                        pend.pop()
                    if pend and not any(pend):
                        pend.clear()
            P.dma("sp", lambda e: e.dma_start(out=g1bc[:], in_=g12_d[0, :].partition_broadcast(128)), writes=[B_g])

            def kv_dests(sti, ck):
                if sti < NU:
                    u = sti
                    d = [(u, 1024 + 512 * ck)]
                    if NU == 3 and u == 1 and ck >= 2:
                        d.append((2, 512 * (ck - 2)))
                    if NU == 3 and u == 2 and ck < 2:
                        d.append((1, 3072 + 512 * ck))
                    return d
                return [(1, 512 * ck)] if ck < 2 else [(2, 3072 + 512 * (ck - 2))]

            for sti in range(NST):
                halo = sti >= NU
                xsrc = x_halo if halo else x_own[sti * U:(sti + 1) * U, :]
                P.dma("sp", lambda e, sti=sti: e.dma_start(out=ropet[:], in_=rope_d[sti * 64:(sti + 1) * 64, :]), writes=[B_rope])
                for t in range(16):
                    xb = cnt["x"] % 3
                    cnt["x"] += 1
                    P.dma("sp", lambda e, t=t, xb=xb, xsrc=xsrc: e.dma_start(out=xt[:, xb, :], in_=xsrc[t * 128:(t + 1) * 128, :]), writes=[B_xt[xb]])
                    P.op("dve", lambda e, xb=xb: e.memset(st1[:, xb, 0:1], 0.0), writes=[B_st[xb]])
                    P.op("act", lambda e, xb=xb: e.activation(out=sqj[:], in_=xt[:, xb, :], func=AF.Square, accum_out=st1[:, xb, 0:1]),
                         reads=[B_xt[xb]], writes=[B_sqj, B_st[xb]])
                    P.op("act", lambda e, xb=xb: e.activation(out=st1[:, xb, 1:2], in_=st1[:, xb, 0:1], func=AF.Sqrt, bias=EPS, scale=1.0 / D),
                         reads=[B_st[xb]], writes=[B_st[xb]])
                    P.op("dve", lambda e, xb=xb: e.reciprocal(out=st1[:, xb, 2:3], in_=st1[:, xb, 1:2]),
                         reads=[B_st[xb]], writes=[B_st[xb]])
                    P.op("dve", lambda e, xb=xb: e.scalar_tensor_tensor(out=hb[:, xb, :], in0=xt[:, xb, :], scalar=st1[:, xb, 2:3], in1=g1bc[:], op0=ALU.mult, op1=ALU.mult),
                         reads=[B_xt[xb], B_st[xb], B_g], writes=[B_hb[xb]])
                    for half in range(2):
                        pb = cnt["tr"] % 2
                        cnt["tr"] += 1
                        for j in range(8):
                            c = half * 8 + j
                            P.op("pe", lambda e, xb=xb, c=c, j=j, pb=pb: e.transpose(out=ptr[:, pb, j, :], in_=hb[:, xb, c * 128:(c + 1) * 128], identity=identb[:]),
                                 reads=[B_hb[xb], B_cb], writes=[B_ptr[pb]])
                        eng = "act" if half == 0 else "dve"
                        if eng == "act":
                            P.op("act", lambda e, t=t, half=half, pb=pb: e.copy(out=hT[:, half * 8:(half + 1) * 8, t * 128:(t + 1) * 128], in_=ptr[:, pb, :, :]),
                                 reads=[B_ptr[pb]], writes=[B_hT[t]])
                        else:
                            P.op("dve", lambda e, t=t, half=half, pb=pb: e.tensor_copy(out=hT[:, half * 8:(half + 1) * 8, t * 128:(t + 1) * 128], in_=ptr[:, pb, :, :]),
                                 reads=[B_ptr[pb]], writes=[B_hT[t]])
                for cg in range(20):
                    kind = "q" if cg in (0, 3, 4, 5) else "k" if cg in (1, 6, 7, 8) else "v" if cg in (2, 9, 10, 11) else "g"
                    if halo and kind not in ("k", "v"):
                        continue
                    wb = cnt["w"] % 2
                    cnt["w"] += 1
                    P.dma("pool", lambda e, cg=cg, wb=wb: e.dma_start(out=wt[:, wb, :, :], in_=w_in[:, cg * 512:(cg + 1) * 512].rearrange("(c p) n -> p c n", p=128)),
                          writes=[B_wt[wb]])
                    if kind == "v":
                        pipe_drain()
                        hv0 = 0 if cg == 2 else 4 + (cg - 9) * 4
                        for t in range(16):
                            ab = cnt["acc"] % 2
                            cnt["acc"] += 1
                            for c in range(16):
                                P.op("pe", lambda e, t=t, c=c, wb=wb, ab=ab: e.matmul(out=pacc[:, ab, :], lhsT=hT[:, c, t * 128:(t + 1) * 128], rhs=wt[:, wb, c, :], start=(c == 0), stop=(c == 15)),
                                     reads=[B_hT[t], B_wt[wb]], writes=[B_pacc[ab]])
                            vb = cnt["v"] % 2
                            cnt["v"] += 1
                            if t % 2 == 0:
                                P.op("act", lambda e, ab=ab, vb=vb: e.copy(out=vs[:, vb, :], in_=pacc[:, ab, :]), reads=[B_pacc[ab]], writes=[B_vs[vb]])
                            else:
                                P.op("dve", lambda e, ab=ab, vb=vb: e.tensor_copy(out=vs[:, vb, :], in_=pacc[:, ab, :]), reads=[B_pacc[ab]], writes=[B_vs[vb]])
                            ck = t // 4
                            for (uu, wo) in kv_dests(sti, ck):
                                row = uu * WIN + wo + (t % 4) * 128
                                P.dma("sp", lambda e, row=row, hv0=hv0, vb=vb: e.dma_start(out=vw[row:row + 128, hv0 * 128:hv0 * 128 + 512], in_=vs[:, vb, :]), reads=[B_vs[vb]])
                        continue
                    for ct in range(4):
                        for ck in range(4):
                            ab = cnt["acc"] % 2
                            cnt["acc"] += 1
                            qb = cnt["q"] % 2
                            q3 = cnt["q"] % 4
                            cnt["q"] += 1

                            def main_fn(ct=ct, ck=ck, wb=wb, ab=ab):
                                for c in range(16):
                                    P.op("pe", lambda e, c=c: e.matmul(out=pacc[:, ab, :], lhsT=wt[:, wb, c, ct * 128:(ct + 1) * 128], rhs=hT[:, c, ck * 512:(ck + 1) * 512], start=(c == 0), stop=(c == 15)),
                                         reads=B_hT[ck * 4:ck * 4 + 4] + [B_wt[wb]], writes=[B_pacc[ab]])

                            if kind == "g":
                                gi = (cg - 12) * 4 + ct

                                def a_fn(ab=ab, q3=q3, gi=gi, sti=sti, ck=ck):
                                    P.op("act", lambda e: e.activation(out=qn[:, q3, :], in_=pacc[:, ab, :], func=AF.Sigmoid), reads=[B_pacc[ab]], writes=[B_qn[q3]])
                                    P.dma("sp", lambda e: e.dma_start(out=gT[gi * 128:(gi + 1) * 128, sti * U + ck * 512: sti * U + (ck + 1) * 512], in_=qn[:, q3, :]), reads=[B_qn[q3]])

                                pipe_push(main_fn, [a_fn])
                                continue
                            if cg <= 1:
                                head = ct
                                gcol = cg
                                rope = False
                            else:
                                head = 4 + ((cg - 3) if kind == "q" else (cg - 6)) * 4 + ct
                                gcol = 2 if kind == "q" else 3
                                rope = True

                            def a_fn(ab=ab, qb=qb, q3=q3, gcol=gcol):
                                P.op("act", lambda e: e.copy(out=qs[:, qb, :], in_=pacc[:, ab, :]), reads=[B_pacc[ab]], writes=[B_qs[qb]])
                                P.op("act", lambda e: e.activation(out=sq[:, qb, :], in_=pacc[:, ab, :], func=AF.Square), reads=[B_pacc[ab]], writes=[B_sq[qb]])
                                P.op("pe", lambda e: e.matmul(out=pm[:], lhsT=onesmb[:], rhs=sq[:, qb, :], start=True, stop=True), reads=[B_sq[qb], B_cb], writes=[B_pm])
                                P.op("act", lambda e: e.activation(out=rstd[:, qb, :], in_=pm[:], func=AF.Sqrt, bias=EPS, scale=1.0), reads=[B_pm], writes=[B_rstd[qb]])
                                P.op("dve", lambda e: e.reciprocal(out=rstd[:, qb, :], in_=rstd[:, qb, :]), reads=[B_rstd[qb]], writes=[B_rstd[qb]])
                                P.op("dve", lambda e: e.scalar_tensor_tensor(out=qn[:, q3, :], in0=qs[:, qb, :], scalar=gqk[:, gcol:gcol + 1], in1=rstd[:, qb, :], op0=ALU.mult, op1=ALU.mult),
                                     reads=[B_qs[qb], B_rstd[qb], B_cst], writes=[B_qn[q3]])

                            def b_fn(qb=qb, q3=q3, ck=ck):
                                P.op("pe", lambda e: e.matmul(out=prr[0:64, 0, :], lhsT=r2mb[:], rhs=qn[:, q3, :], start=True, stop=True), reads=[B_qn[q3], B_cb], writes=[B_prr[0]])
                                P.op("dve", lambda e: e.tensor_tensor(out=rt[:, qb, :], in0=prr[0:64, 0, :], in1=ropet[:, ck * 512:(ck + 1) * 512], op=ALU.mult),
                                     reads=[B_prr[0], B_rope], writes=[B_rt[qb]])

                            def c_fn(qb=qb, q3=q3, rope=rope, kind=kind, head=head, sti=sti, ck=ck):
                                if rope:
                                    P.op("pe", lambda e: e.matmul(out=prr[0:32, 1, :], lhsT=sselb[0:64, :], rhs=rt[:, qb, :], start=True, stop=True), reads=[B_rt[qb], B_cb], writes=[B_prr[1]])
                                    P.op("act", lambda e: e.copy(out=qn[0:32, q3, :], in_=prr[0:32, 1, :]), reads=[B_prr[1]], writes=[B_qn[q3]])
                                if kind == "q":
                                    P.dma("sp", lambda e: e.dma_start(out=qT[head * 128:(head + 1) * 128, sti * U + ck * 512: sti * U + (ck + 1) * 512], in_=qn[:, q3, :]), reads=[B_qn[q3]])
                                else:
                                    for (uu, wo) in kv_dests(sti, ck):
                                        r_ = (uu * 16 + head) * 128
                                        P.dma("sp", lambda e, r_=r_, wo=wo: e.dma_start(out=kTw[r_:r_ + 128, wo:wo + 512], in_=qn[:, q3, :]), reads=[B_qn[q3]])

                            pipe_push(main_fn, [a_fn, b_fn if rope else None, c_fn])
                pipe_drain()
            P.flush()

        with ExitStack() as es:
            ebt = es.enter_context(nc.sbuf_tensor("sb_ebt", [128, 4, 7, 128], F32))
            ebt2 = es.enter_context(nc.sbuf_tensor("sb_ebt2", [128, 4, 5, 128], F32))
            hbk = es.enter_context(nc.sbuf_tensor("sb_hbk", [128, 4, 128], F32))
            ebtmp = es.enter_context(nc.sbuf_tensor("sb_ebtmp", [128, 4, 128], F32))
            kT = es.enter_context(nc.sbuf_tensor("sb_kT", [128, 2, WIN], BF16))
            qTt = es.enter_context(nc.sbuf_tensor("sb_qTt", [128, 2, U], BF16))
            vt = es.enter_context(nc.sbuf_tensor("sb_vt", [128, 2, 32, 128], BF16))
            eS = es.enter_context(nc.sbuf_tensor("sb_eS", [128, 2, 1024], F32))
            eS2 = es.enter_context(nc.sbuf_tensor("sb_eS2", [128, 2, 1024], F32))
            pT = es.enter_context(nc.sbuf_tensor("sb_pT", [128, 2, 1024], BF16))
            acc = es.enter_context(nc.sbuf_tensor("sb_acc", [128, 2, U], F32))
            rD = es.enter_context(nc.sbuf_tensor("sb_rD", [128, U], F32))
            oTt = es.enter_context(nc.sbuf_tensor("sb_oTt", [128, 8, U], BF16))
            psS = es.enter_context(nc.psum_tensor("ps_psS", [128, 2, 1024], F32))
            psO = es.enter_context(nc.psum_tensor("ps_psO", [128, 2, 512], F32))
            psE = es.enter_context(nc.psum_tensor("ps_psE", [128, 512], F32))
            B_ebt = Buf("ebt")
            B_hbk = [Buf("hbk%d" % i) for i in range(4)]
            B_ebtmp = [Buf("ebtmp%d" % i) for i in range(4)]
            B_psE = [Buf("psE%d" % i) for i in range(4)]
            ei = 0
            for h in range(4):
                for di in range(7):
                    delta = di - 3
                    k4 = ei % 4
                    ei += 1
                    for bp in range(2):
                        dr = 2 * delta - bp + 7
                        src = bass.AP(tensor=rpbp.tensor, offset=(h * 15 + dr) * 128, ap=[[1, 64], [128, 2], [1, 64]])
                        P.dma("sp", lambda e, src=src, bp=bp, k4=k4: e.dma_start(out=hbk[bp * 64:(bp + 1) * 64, k4, :].rearrange("p (a c) -> p a c", c=64), in_=src), writes=[B_hbk[k4]])
                    P.op("pe", lambda e, k4=k4: e.matmul(out=psE[:, k4 * 128:(k4 + 1) * 128], lhsT=hbk[:, k4, :], rhs=cs(cst, "j2"), start=True, stop=True), reads=[B_hbk[k4], B_cst], writes=[B_psE[k4]])
                    P.op("act", lambda e, k4=k4: e.activation(out=ebtmp[:, k4, :], in_=psE[:, k4 * 128:(k4 + 1) * 128], func=AF.Exp), reads=[B_psE[k4]], writes=[B_ebtmp[k4]])
                    P.op("dve", lambda e, h=h, di=di, k4=k4: e.tensor_tensor(out=ebt[:, h, di, :], in0=ebtmp[:, k4, :], in1=cs(cst, "colvalid"), op=ALU.mult), reads=[B_ebtmp[k4], B_cst], writes=[B_ebt])
            rio = coff["rvint"][0]
            for h in range(4):
                P.op("dve", lambda e, h=h: e.tensor_tensor(out=ebt2[:, h, :, :].rearrange("p a (b c) -> p (a b) c", c=64), in0=ebt[:, h, 1:6, :].rearrange("p a (b c) -> p (a b) c", c=64),
                                                          in1=cst[:, rio:rio + 10].unsqueeze(2).broadcast_to([128, 10, 64]), op=ALU.mult), reads=[B_ebt, B_cst], writes=[B_ebt])

            B_kT = [Buf("kT0"), Buf("kT1")]
            B_qT = [Buf("qT0"), Buf("qT1")]
            B_vt = [Buf("vt0"), Buf("vt1")]
            B_psQ = [Buf("psQ%d" % i) for i in range(4)]
            B_pTQ = [Buf("pTQ%d" % i) for i in range(4)]
            B_psS = [[B_psQ[0], B_psQ[1]], [B_psQ[2], B_psQ[3]]]
            B_psO = [Buf("psO0"), Buf("psO1")]
            B_eS = [Buf("eS0"), Buf("eS1")]
            B_eS2 = [Buf("eS20"), Buf("eS21")]
            B_pT = [[B_pTQ[0], B_pTQ[1]], [B_pTQ[2], B_pTQ[3]]]
            B_oTn = [[Buf("oT%d_%d" % (j, n)) for n in range(16)] for j in range(8)]
            ac = {"h": 0, "s": 0, "o": 0, "q4": 0}
            rvo = coff["rv"][0]
            kbo = coff["kvb"][0]
            pend2 = []

            def push2(a_fn, b_fn, depth=1):
                a_fn()
                pend2.append(b_fn)
                while len(pend2) > depth:
                    pend2.pop(0)()

            def drain2():
                while pend2:
                    pend2.pop(0)()

            for u in range(NU):
                for h in range(4):
                    hbf = ac["h"] % 2
                    ac["h"] += 1
                    krow = (u * 16 + h) * 128
                    P.dma("sp", lambda e, krow=krow, hbf=hbf: e.dma_start(out=kT[:, hbf, 640:3456], in_=kTw[krow:krow + 128, 640:3456]), writes=[B_kT[hbf]])
                    P.dma("sp", lambda e, h=h, u=u, hbf=hbf: e.dma_start(out=qTt[:, hbf, :], in_=qT[h * 128:(h + 1) * 128, u * U:(u + 1) * U]), writes=[B_qT[hbf]])
                    P.dma("sp", lambda e, h=h, u=u, hbf=hbf: e.dma_start(out=vt[:, hbf, 0:22, :], in_=vw[u * WIN + 640:u * WIN + 3456, h * 128:(h + 1) * 128].rearrange("(t p) d -> p t d", p=128)), writes=[B_vt[hbf]])
                    for n in range(16):
                        sb = ac["s"] % 2
                        ac["s"] += 1
                        ob = ac["o"] % 2
                        ac["o"] += 1

                        interior = 2 <= n <= 13
                        dis = list(range(1, 6)) if interior else list(range(7))
                        nd = len(dis)

                        def a_fn(u=u, h=h, n=n, hbf=hbf, sb=sb, interior=interior, dis=dis, nd=nd):
                            for j, di in enumerate(dis):
                                m = n + di
                                P.op("pe", lambda e, m=m, j=j: e.matmul(out=psS[:, sb, j * 128:(j + 1) * 128], lhsT=kT[:, hbf, 640 + m * 128:640 + (m + 1) * 128], rhs=qTt[:, hbf, n * 128:(n + 1) * 128], start=True, stop=True),
                                     reads=[B_kT[hbf], B_qT[hbf]], writes=B_psS[sb])
                            P.op("act", lambda e: e.activation(out=eS[:, sb, 0:512], in_=psS[:, sb, 0:512], func=AF.Exp, scale=SCALE), reads=B_psS[sb], writes=[B_eS[sb]])
                            P.op("act", lambda e: e.activation(out=eS[:, sb, 512:nd * 128], in_=psS[:, sb, 512:nd * 128], func=AF.Exp, scale=SCALE), reads=B_psS[sb], writes=[B_eS[sb]])
                            if interior:
                                P.op("dve", lambda e: e.tensor_tensor(out=pT[:, sb, 0:640], in0=eS[:, sb, 0:640], in1=ebt2[:, h, :, :].rearrange("p a b -> p (a b)"), op=ALU.mult),
                                     reads=[B_eS[sb], B_ebt], writes=B_pT[sb])
                                return
                            P.op("dve", lambda e: e.tensor_tensor(out=eS2[:, sb, 0:896], in0=eS[:, sb, 0:896], in1=ebt[:, h, :, :].rearrange("p a b -> p (a b)"), op=ALU.mult),
                                 reads=[B_eS[sb], B_ebt], writes=[B_eS2[sb]])
                            ro = rvo + (u * 16 + n) * 14
                            P.op("pool", lambda e: e.tensor_tensor(out=pT[:, sb, 0:896].rearrange("p (a b) -> p a b", b=64), in0=eS2[:, sb, 0:896].rearrange("p (a b) -> p a b", b=64),
                                                                  in1=cst[:, ro:ro + 14].unsqueeze(2).broadcast_to([128, 14, 64]), op=ALU.mult),
                                 reads=[B_eS2[sb], B_cst], writes=B_pT[sb])

                        def b_fn(h=h, n=n, hbf=hbf, sb=sb, ob=ob, dis=dis, nd=nd):
                            for j, di in enumerate(dis):
                                m = n + di
                                P.op("pe", lambda e, m=m, j=j: e.matmul(out=psO[:, ob, 0:128], lhsT=vt[:, hbf, m, :], rhs=pT[:, sb, j * 128:(j + 1) * 128], start=(j == 0), stop=(j == nd - 1)),
                                     reads=[B_vt[hbf]] + B_pT[sb], writes=[B_psO[ob]])
                            for j in range(nd):
                                P.op("pe", lambda e, j=j: e.matmul(out=psO[:, ob, 128:256], lhsT=onesb[:], rhs=pT[:, sb, j * 128:(j + 1) * 128], start=(j == 0), stop=(j == nd - 1)),
                                     reads=[B_cb] + B_pT[sb], writes=[B_psO[ob]])
                            B_r = Buf("rDn")
                            P.op("dve", lambda e: e.reciprocal(out=rD[:, n * 128:(n + 1) * 128], in_=psO[:, ob, 128:256]), reads=[B_psO[ob]], writes=[B_r])
                            P.op("dve", lambda e: e.tensor_tensor(out=oTt[:, h, n * 128:(n + 1) * 128], in0=psO[:, ob, 0:128], in1=rD[:, n * 128:(n + 1) * 128], op=ALU.mult),
                                 reads=[B_psO[ob], B_r], writes=[B_oTn[h][n]])

                        push2(a_fn, b_fn)
                for hs in range(4):
                    B_accb = [[Buf("acc%d_%d" % (g, i)) for i in range(16)] for g in range(3)]
                    for g, d in enumerate(DILS):
                        head = 4 + 4 * g + hs
                        nb = 16 // d
                        hbf = ac["h"] % 2
                        ac["h"] += 1
                        krow = (u * 16 + head) * 128
                        P.dma("sp", lambda e, krow=krow, hbf=hbf: e.dma_start(out=kT[:, hbf, :], in_=kTw[krow:krow + 128, :]), writes=[B_kT[hbf]])
                        P.dma("sp", lambda e, head=head, u=u, hbf=hbf: e.dma_start(out=qTt[:, hbf, :], in_=qT[head * 128:(head + 1) * 128, u * U:(u + 1) * U]), writes=[B_qT[hbf]])
                        for r in range(d):
                            base = (u * WIN + 1024 - 64 * d + r) * D + head * 128
                            src = bass.AP(tensor=vw.tensor, offset=base, ap=[[d * D, 128], [128 * d * D, nb + 1], [1, 128]])
                            P.dma("sp", lambda e, src=src, r=r, nb=nb, hbf=hbf: e.dma_start(out=vt[:, hbf, r * (nb + 1):(r + 1) * (nb + 1), :], in_=src), writes=[B_vt[hbf]])
                        blocks = [(r, n) for r in range(d) for n in range(nb)]
                        for pi in range(8):
                            k4 = ac["q4"] % 4
                            ac["q4"] += 1
                            ob = ac["o"] % 2
                            ac["o"] += 1
                            sbh, so = k4 // 2, (k4 % 2) * 512

                            def a_fn(u=u, g=g, d=d, pi=pi, hbf=hbf, k4=k4, sbh=sbh, so=so, blocks=blocks):
                                for bi in range(2):
                                    r, n = blocks[2 * pi + bi]
                                    q0 = n * 128 * d + r
                                    for ab in range(2):
                                        m = n + ab
                                        k0 = 1024 + (m * 128 - 64) * d + r
                                        sub = bi * 2 + ab
                                        P.op("pe", lambda e, k0=k0, q0=q0, sub=sub: e.matmul(
                                            out=psS[:, sbh, so + sub * 128:so + (sub + 1) * 128],
                                            lhsT=kT[:, hbf, k0:k0 + 127 * d + 1:d], rhs=qTt[:, hbf, q0:q0 + 127 * d + 1:d], start=True, stop=False),
                                            reads=[B_kT[hbf], B_qT[hbf]], writes=[B_psQ[k4]])
                                        P.op("pe", lambda e, sub=sub, ab=ab: e.matmul(
                                            out=psS[:, sbh, so + sub * 128:so + (sub + 1) * 128], lhsT=identb[:], rhs=negb[:, ab, :], start=False, stop=True),
                                            reads=[B_cb], writes=[B_psQ[k4]])
                                ko = kbo + ((u * 3 + g) * 8 + pi) * 4
                                for sub in range(4):
                                    P.op("act", lambda e, sub=sub: e.activation(out=pT[:, sbh, so + sub * 128:so + (sub + 1) * 128], in_=psS[:, sbh, so + sub * 128:so + (sub + 1) * 128], func=AF.Exp,
                                                                                bias=cst[:, ko + sub:ko + sub + 1], scale=SCALE),
                                         reads=[B_psQ[k4], B_cst], writes=[B_pTQ[k4]])

                            def b_fn(g=g, d=d, nb=nb, pi=pi, hbf=hbf, k4=k4, sbh=sbh, so=so, ob=ob, blocks=blocks, B_accb=B_accb):
                                for bi in range(2):
                                    r, n = blocks[2 * pi + bi]
                                    for ab in range(2):
                                        m = n + ab
                                        P.op("pe", lambda e, r=r, m=m, bi=bi, ab=ab: e.matmul(
                                            out=psO[:, ob, bi * 256:bi * 256 + 128], lhsT=vt[:, hbf, r * (nb + 1) + m, :],
                                            rhs=pT[:, sbh, so + (bi * 2 + ab) * 128:so + (bi * 2 + ab + 1) * 128], start=(ab == 0), stop=(ab == 1)),
                                            reads=[B_vt[hbf], B_pTQ[k4]], writes=[B_psO[ob]])
                                    for ab in range(2):
                                        P.op("pe", lambda e, bi=bi, ab=ab: e.matmul(
                                            out=psO[:, ob, bi * 256 + 128:bi * 256 + 256], lhsT=onesb[:],
                                            rhs=pT[:, sbh, so + (bi * 2 + ab) * 128:so + (bi * 2 + ab + 1) * 128], start=(ab == 0), stop=(ab == 1)),
                                            reads=[B_cb, B_pTQ[k4]], writes=[B_psO[ob]])
                                for bi in range(2):
                                    r, n = blocks[2 * pi + bi]
                                    q0 = n * 128 * d + r
                                    src = psO[:, ob, bi * 256:(bi + 1) * 256].rearrange("p (a b) -> p a b", b=128)
                                    dst = acc[:, :, q0:q0 + 127 * d + 1:d]
                                    bb = B_accb[g][2 * pi + bi]
                                    if g == 0:
                                        P.op("dve", lambda e, src=src, dst=dst: e.tensor_copy(out=dst, in_=src), reads=[B_psO[ob]], writes=[bb])
                                    else:
                                        P.op("dve", lambda e, src=src, dst=dst: e.tensor_tensor(out=dst, in0=dst, in1=src, op=ALU.add), reads=[B_psO[ob]], writes=[bb])

                            push2(a_fn, b_fn, 2)
                    drain2()
                    allacc = [bb for gl in B_accb for bb in gl]
                    B_r = Buf("rDfull")
                    P.op("dve", lambda e: e.reciprocal(out=rD[:], in_=acc[:, 1, :]), reads=allacc, writes=[B_r])
                    P.op("dve", lambda e, hs=hs: e.tensor_tensor(out=oTt[:, 4 + hs, :], in0=acc[:, 0, :], in1=rD[:], op=ALU.mult), reads=allacc + [B_r], writes=B_oTn[4 + hs])
                for j in range(8):
                    P.dma("sp", lambda e, j=j, u=u: e.dma_start(out=oT[j * 128:(j + 1) * 128, u * U:(u + 1) * U], in_=oTt[:, j, :]), reads=B_oTn[j])
            P.flush(include_deferred=True)
        es_z.close()

        with ExitStack() as es:
            wab = es.enter_context(nc.sbuf_tensor("sb_wab", [128, 8, D], BF16))
            wo = es.enter_context(nc.sbuf_tensor("sb_wo", [128, 16, D], BF16))
            wrt = es.enter_context(nc.sbuf_tensor("sb_wrt", [128, 16, 36], F32))
            g2bc = es.enter_context(nc.sbuf_tensor("sb_g2bc", [128, D], F32))
            oc = es.enter_context(nc.sbuf_tensor("sb_oc", [128, 8, 256], BF16))
            gc = es.enter_context(nc.sbuf_tensor("sb_gc", [128, 32, 256], BF16))
            t12 = es.enter_context(nc.sbuf_tensor("sb_t12", [128, 2, 2, 256], F32))
            mix = es.enter_context(nc.sbuf_tensor("sb_mix", [128, 16, 256], BF16))
            x1 = es.enter_context(nc.sbuf_tensor("sb_x1", [128, 1, D], F32))
            h2f = es.enter_context(nc.sbuf_tensor("sb_h2f", [128, D], F32))
            h2b = es.enter_context(nc.sbuf_tensor("sb_h2b", [128, 2, D], BF16))
            h2T = es.enter_context(nc.sbuf_tensor("sb_h2T", [128, 16, 128], F32))
            rs2 = es.enter_context(nc.sbuf_tensor("sb_rs", [128, 2, 96], F32))
            lg2 = es.enter_context(nc.sbuf_tensor("sb_lg", [128, 2, 36], F32))
            ohb2 = es.enter_context(nc.sbuf_tensor("sb_ohb", [128, 2, 32], BF16))
            oh2 = es.enter_context(nc.sbuf_tensor("sb_oh", [128, 2, 3, 32], F32))
            tot = es.enter_context(nc.sbuf_tensor("sb_tot", [128, 32], F32))
            pos2 = es.enter_context(nc.sbuf_tensor("sb_pos", [128, 2, 32], F32))
            pya = es.enter_context(nc.psum_tensor("ps_pya", [128, 2, 512], F32))
            pout = es.enter_context(nc.psum_tensor("ps_pout", [128, 2, 512], F32))
            ptr3 = es.enter_context(nc.psum_tensor("ps_ptr3", [128, 2, 512], F32))
            plg = es.enter_context(nc.psum_tensor("ps_plg", [128, 512], F32))
            B_w3 = Buf("w3")
            B_oc = Buf("oc")
            B_gc = Buf("gc")
            B_t12 = [Buf("t120"), Buf("t121")]
            B_mix = Buf("mix")
            B_x3 = [Buf("x30"), Buf("x31")]
            B_x1 = [Buf("x10"), Buf("x11")]
            B_h2f = Buf("h2f")
            B_h2b = [Buf("h2b0"), Buf("h2b1")]
            B_h2T = Buf("h2T")
            B_rs = Buf("rs")
            B_lg = Buf("lg")
            B_oh = Buf("oh")
            B_tot = Buf("tot")
            B_pos = Buf("pos")
            B_pya = [Buf("pya0"), Buf("pya1")]
            B_pout = [Buf("pout0"), Buf("pout1")]
            B_ptr3 = [Buf("ptr30"), Buf("ptr31")]
            B_plg = Buf("plg")
            P.dma("pool", lambda e: e.dma_start(out=wab[:], in_=w_ab.rearrange("(c p) n -> p c n", p=128)), writes=[B_w3])
            for q4 in range(4):
                P.dma("pool", lambda e, q4=q4: e.dma_start(out=wo[:, q4 * 4:(q4 + 1) * 4, :], in_=w_out[q4 * 512:(q4 + 1) * 512, :].rearrange("(c p) n -> p c n", p=128)), writes=[B_w3])
            P.dma("sp", lambda e: e.dma_start(out=wrt[:], in_=wr_d.rearrange("(c p) n -> p c n", p=128)), writes=[B_w3])
            P.op("dve", lambda e: e.memset(tot[:], 0.0), writes=[B_tot])
            c3 = {"t": 0, "x": 0, "o": 0, "tr": 0, "h": 0}
            P.dma("sp", lambda e: e.dma_start(out=g2bc[:], in_=g12_d[1, :].partition_broadcast(128)), writes=[B_g])
            B_rs2 = [Buf("rs0"), Buf("rs1")]
            B_lg2 = [Buf("lg0"), Buf("lg1")]
            B_oh2 = [Buf("oh0"), Buf("oh1")]
            B_pos2 = [Buf("pos0"), Buf("pos1")]
            B_plg2 = [Buf("plg0"), Buf("plg1")]

            def branch_pair(row0):
                P.dma("sp", lambda e: e.dma_start(out=oc[:], in_=oT[:, row0:row0 + 256].rearrange("(j p) t -> p j t", p=128)), writes=[B_oc])
                P.dma("sp", lambda e: e.dma_start(out=gc[:], in_=gT[:, row0:row0 + 256].rearrange("(j p) t -> p j t", p=128)), writes=[B_gc])
                for ft in range(16):
                    tb = c3["t"] % 2
                    c3["t"] += 1
                    for br in range(2):
                        for c in range(4):
                            P.op("pe", lambda e, br=br, c=c, ft=ft: e.matmul(out=pya[:, br, 0:256], lhsT=wab[:, br * 4 + c, ft * 128:(ft + 1) * 128], rhs=oc[:, br * 4 + c, :], start=(c == 0), stop=(c == 3)),
                                 reads=[B_w3, B_oc], writes=[B_pya[br]])
                        P.op("dve", lambda e, br=br, ft=ft, tb=tb: e.tensor_tensor(out=t12[:, tb, br, :], in0=pya[:, br, 0:256], in1=gc[:, br * 16 + ft, :], op=ALU.mult),
                             reads=[B_pya[br], B_gc], writes=[B_t12[tb]])
                    P.op("pool", lambda e, ft=ft, tb=tb: e.tensor_tensor(out=mix[:, ft, :], in0=t12[:, tb, 0, :], in1=t12[:, tb, 1, :], op=ALU.add), reads=[B_t12[tb]], writes=[B_mix])

            def pre_part(tix, s_):
                row0 = tix * 128
                rs = rs2[:, s_, :]
                P.dma("sp", lambda e: e.dma_start(out=x1[:, 0, :], in_=x_own[row0:row0 + 128, :]), writes=[B_x1[0]])
                for cb in range(4):
                    ob = c3["o"] % 2
                    c3["o"] += 1
                    for ft in range(16):
                        P.op("pe", lambda e, ft=ft, cb=cb, ob=ob: e.matmul(out=pout[:, ob, :], lhsT=mix[:, ft, s_ * 128:(s_ + 1) * 128], rhs=wo[:, ft, cb * 512:(cb + 1) * 512], start=(ft == 0), stop=(ft == 15)),
                             reads=[B_mix, B_w3], writes=[B_pout[ob]])
                    P.op("dve", lambda e, cb=cb, ob=ob: e.tensor_tensor(out=x1[:, 0, cb * 512:(cb + 1) * 512], in0=pout[:, ob, :], in1=x1[:, 0, cb * 512:(cb + 1) * 512], op=ALU.add),
                         reads=[B_pout[ob], B_x1[0]], writes=[B_x1[0]])
                P.dma("sp", lambda e: e.dma_start(out=x1s[row0:row0 + 128, :], in_=x1[:, 0, :]), reads=[B_x1[0]])
                P.op("dve", lambda e: e.memset(rs[:, 0:1], 0.0), writes=[B_rs2[s_]])
                P.op("dve", lambda e: e.memset(rs[:, 5:6], 0.0), writes=[B_rs2[s_]])
                P.op("act", lambda e: e.activation(out=h2b[:, s_, :], in_=x1[:, 0, :], func=AF.Square, accum_out=rs[:, 0:1]), reads=[B_x1[0], B_rs2[s_]], writes=[B_h2b[s_], B_rs2[s_]])
                P.op("act", lambda e: e.activation(out=rs[:, 1:2], in_=rs[:, 0:1], func=AF.Sqrt, bias=EPS, scale=1.0 / D), reads=[B_rs2[s_]], writes=[B_rs2[s_]])
                P.op("dve", lambda e: e.reciprocal(out=rs[:, 2:3], in_=rs[:, 1:2]), reads=[B_rs2[s_]], writes=[B_rs2[s_]])
                P.op("dve", lambda e: e.scalar_tensor_tensor(out=h2f[:], in0=x1[:, 0, :], scalar=rs[:, 2:3], in1=g2bc[:], op0=ALU.mult, op1=ALU.mult),
                     reads=[B_x1[0], B_rs2[s_], B_g], writes=[B_h2f])
                P.op("act", lambda e: e.copy(out=h2b[:, s_, :], in_=h2f[:]), reads=[B_h2f], writes=[B_h2b[s_]])
                for q4 in range(4):
                    pb = c3["tr"] % 2
                    c3["tr"] += 1
                    for j in range(4):
                        c = q4 * 4 + j
                        P.op("pe", lambda e, c=c, j=j, pb=pb: e.transpose(out=ptr3[:, pb, j * 128:(j + 1) * 128], in_=h2f[:, c * 128:(c + 1) * 128], identity=identf), reads=[B_h2f, B_cst], writes=[B_ptr3[pb]])
                    P.op("act", lambda e, q4=q4, pb=pb: e.copy(out=h2T[:, q4 * 4:(q4 + 1) * 4, :], in_=ptr3[:, pb, :].rearrange("p (a b) -> p a b", b=128)), reads=[B_ptr3[pb]], writes=[B_h2T])
                for c in range(16):
                    P.op("pe", lambda e, c=c: e.matmul(out=plg[:, s_ * 256:s_ * 256 + 36], lhsT=h2T[:, c, :], rhs=wrt[:, c, :], start=(c == 0), stop=(c == 15)), reads=[B_h2T, B_w3], writes=[B_plg2[s_]])
                P.op("dve", lambda e: e.tensor_tensor(out=lg2[:, s_, :], in0=plg[:, s_ * 256:s_ * 256 + 36], in1=cs(cst, "rbias"), op=ALU.add), reads=[B_plg2[s_], B_cst], writes=[B_lg2[s_]])

            def chain1(tix, s_):
                rs = rs2[:, s_, :]
                lg = lg2[:, s_, :]
                oh = oh2[:, s_, :, :]
                ohb = ohb2[:, s_, :]
                R = [B_rs2[s_], B_lg2[s_], B_oh2[s_]]
                ops = []

                def dv(fn, extra_r=(), extra_w=()):
                    ops.append(("dve", fn, R + list(extra_r), R + list(extra_w)))
                dv(lambda e: e.reduce_max(out=rs[:, 3:4], in_=lg[:, 0:4], axis=AX.X))
                dv(lambda e: e.tensor_scalar(out=rs[:, 8:12], in0=lg[:, 0:4], scalar1=rs[:, 3:4], scalar2=None, op0=ALU.is_equal))
                dv(lambda e: e.tensor_scalar(out=rs[:, 4:5], in0=rs[:, 3:4], scalar1=-1.0, scalar2=None, op0=ALU.mult))
                ops.append(("act", lambda e: e.activation(out=rs[:, 12:16], in_=lg[:, 0:4], func=AF.Exp, bias=rs[:, 4:5], scale=1.0, accum_out=rs[:, 5:6]), R, R))
                dv(lambda e: e.reciprocal(out=rs[:, 6:7], in_=rs[:, 5:6]))
                dv(lambda e: e.tensor_scalar(out=rs[:, 16:24], in0=lg[:, 4:12], scalar1=rs[:, 8:9], scalar2=None, op0=ALU.mult))
                for g_ in range(1, 4):
                    dv(lambda e, g_=g_: e.scalar_tensor_tensor(out=rs[:, 16:24], in0=lg[:, 4 + 8 * g_:12 + 8 * g_], scalar=rs[:, 8 + g_:9 + g_], in1=rs[:, 16:24], op0=ALU.mult, op1=ALU.add))
                dv(lambda e: e.reduce_max(out=rs[:, 24:25], in_=rs[:, 16:24], axis=AX.X))
                dv(lambda e: e.tensor_scalar(out=rs[:, 32:40], in0=rs[:, 16:24], scalar1=rs[:, 24:25], scalar2=None, op0=ALU.is_equal))
                dv(lambda e: e.scalar_tensor_tensor(out=rs[:, 40:48], in0=rs[:, 32:40], scalar=-1e30, in1=rs[:, 16:24], op0=ALU.mult, op1=ALU.add))
                dv(lambda e: e.reduce_max(out=rs[:, 25:26], in_=rs[:, 40:48], axis=AX.X))
                dv(lambda e: e.tensor_scalar(out=rs[:, 48:56], in0=rs[:, 40:48], scalar1=rs[:, 25:26], scalar2=None, op0=ALU.is_equal))
                dv(lambda e: e.tensor_scalar(out=rs[:, 26:27], in0=rs[:, 24:25], scalar1=-1.0, scalar2=None, op0=ALU.mult))
                ops.append(("act", lambda e: e.activation(out=rs[:, 27:28], in_=rs[:, 25:26], func=AF.Exp, bias=rs[:, 26:27], scale=1.0), R, R))
                dv(lambda e: e.tensor_scalar(out=rs[:, 28:29], in0=rs[:, 27:28], scalar1=1.0, scalar2=None, op0=ALU.add))
                dv(lambda e: e.reciprocal(out=rs[:, 29:30], in_=rs[:, 28:29]))
                dv(lambda e: e.tensor_tensor(out=wts_t[:, tix, 0:1], in0=rs[:, 29:30], in1=rs[:, 6:7], op=ALU.mult), extra_w=[B_idx])
                dv(lambda e: e.tensor_tensor(out=wts_t[:, tix, 1:2], in0=wts_t[:, tix, 0:1], in1=rs[:, 27:28], op=ALU.mult), extra_r=[B_idx], extra_w=[B_idx])
                dv(lambda e: e.tensor_tensor(out=rs[:, 56:60], in0=rs[:, 8:12], in1=cs(cst, "iota4"), op=ALU.mult), extra_r=[B_cst])
                dv(lambda e: e.reduce_sum(out=rs[:, 60:61], in_=rs[:, 56:60], axis=AX.X))
                dv(lambda e: e.tensor_tensor(out=rs[:, 64:72], in0=rs[:, 32:40], in1=cs(cst, "iota8"), op=ALU.mult), extra_r=[B_cst])
                dv(lambda e: e.reduce_sum(out=rs[:, 61:62], in_=rs[:, 64:72], axis=AX.X))
                dv(lambda e: e.tensor_tensor(out=rs[:, 72:80], in0=rs[:, 48:56], in1=cs(cst, "iota8"), op=ALU.mult), extra_r=[B_cst])
                dv(lambda e: e.reduce_sum(out=rs[:, 62:63], in_=rs[:, 72:80], axis=AX.X))
                dv(lambda e: e.scalar_tensor_tensor(out=rs[:, 80:81], in0=rs[:, 60:61], scalar=8.0, in1=rs[:, 61:62], op0=ALU.mult, op1=ALU.add))
                dv(lambda e: e.scalar_tensor_tensor(out=rs[:, 81:82], in0=rs[:, 60:61], scalar=8.0, in1=rs[:, 62:63], op0=ALU.mult, op1=ALU.add))
                dv(lambda e: e.tensor_scalar(out=oh[:, 0, :], in0=cs(cst, "iota32"), scalar1=rs[:, 80:81], scalar2=None, op0=ALU.is_equal), extra_r=[B_cst])
                dv(lambda e: e.tensor_scalar(out=oh[:, 1, :], in0=cs(cst, "iota32"), scalar1=rs[:, 81:82], scalar2=None, op0=ALU.is_equal), extra_r=[B_cst])
                dv(lambda e: e.tensor_tensor(out=ohb, in0=oh[:, 0, :], in1=oh[:, 1, :], op=ALU.add))
                return ops

            def chain2(tix, s_):
                ohb = ohb2[:, s_, :]
                pos = pos2[:, s_, :]
                o_ = s_ * 256
                P.op("pe", lambda e: e.matmul(out=plg[:, o_ + 64:o_ + 96], lhsT=ustrb[:], rhs=ohb, start=True, stop=True), reads=[B_oh2[s_], B_cb], writes=[B_plg2[s_]])
                P.op("pe", lambda e: e.matmul(out=plg[:, o_ + 128:o_ + 160], lhsT=onesb[:], rhs=ohb, start=True, stop=True), reads=[B_oh2[s_], B_cb], writes=[B_plg2[s_]])
                P.op("dve", lambda e: e.tensor_tensor(out=pos, in0=plg[:, o_ + 64:o_ + 96], in1=tot[:], op=ALU.add), reads=[B_plg2[s_], B_tot], writes=[B_pos2[s_]])
                P.op("dve", lambda e: e.tensor_tensor(out=tot[:], in0=plg[:, o_ + 128:o_ + 160], in1=tot[:], op=ALU.add), reads=[B_plg2[s_], B_tot], writes=[B_tot])

            def chain3(tix, s_):
                rs = rs2[:, s_, :]
                oh = oh2[:, s_, :, :]
                pos = pos2[:, s_, :]
                R = [B_rs2[s_], B_oh2[s_]]
                ops = []

                def dv(fn, extra_r=(), extra_w=()):
                    ops.append(("dve", fn, R + list(extra_r), R + list(extra_w)))
                for k_ in range(2):
                    dv(lambda e, k_=k_: e.tensor_tensor(out=oh[:, 2, :], in0=oh[:, k_, :], in1=pos, op=ALU.mult), extra_r=[B_pos2[s_]])
                    dv(lambda e, k_=k_: e.reduce_sum(out=rs[:, 84 + k_:85 + k_], in_=oh[:, 2, :], axis=AX.X))
                    dv(lambda e, k_=k_: e.tensor_scalar(out=rs[:, 84 + k_:85 + k_], in0=rs[:, 84 + k_:85 + k_], scalar1=float(CAP - 1), scalar2=None, op0=ALU.min))
                    dv(lambda e, k_=k_: e.scalar_tensor_tensor(out=rs[:, 88 + k_:89 + k_], in0=rs[:, 80 + k_:81 + k_], scalar=float(CAP), in1=rs[:, 84 + k_:85 + k_], op0=ALU.mult, op1=ALU.add))
                    dv(lambda e, k_=k_: e.tensor_copy(out=idx_t[:, tix, k_:k_ + 1], in_=rs[:, 88 + k_:89 + k_]), extra_w=[B_idx])
                return ops

            def interleave(la, lb):
                for oa, ob_ in zip(la, lb):
                    P.op(oa[0], oa[1], reads=oa[2], writes=oa[3])
                    P.op(ob_[0], ob_[1], reads=ob_[2], writes=ob_[3])

            for pr in range(NT // 256):
                tA, tB = 2 * pr, 2 * pr + 1
                branch_pair(pr * 256)
                pre_part(tA, 0)
                pre_part(tB, 1)
                interleave(chain1(tA, 0), chain1(tB, 1))
                chain2(tA, 0)
                chain2(tB, 1)
                interleave(chain3(tA, 0), chain3(tB, 1))
                for (tix, s_) in ((tA, 0), (tB, 1)):
                    for k_ in range(2):
                        P.dma("pool", lambda e, k_=k_, tix=tix, s_=s_: e.indirect_dma_start(
                            out=xbuf[:, :], out_offset=bass.IndirectOffsetOnAxis(ap=idx_t[:, tix, k_:k_ + 1], axis=0),
                            in_=h2b[:, s_, :], in_offset=None),
                            reads=[B_h2b[s_], B_idx])
            if debug:
                P.dma("sp", lambda e: e.dma_start(out=dbg[:, 0:32], in_=tot[:]), reads=[B_tot])
            P.flush()

        NS = CAP // 128
        with ExitStack() as es:
            wg = es.enter_context(nc.sbuf_tensor("sb_wg", [128, 2, 16, 512], BF16))
            wu = es.enter_context(nc.sbuf_tensor("sb_wu", [128, 2, 16, 512], BF16))
            wd = es.enter_context(nc.sbuf_tensor("sb_wd", [128, 2, 4, D], BF16))
            xg = es.enter_context(nc.sbuf_tensor("sb_xg", [128, NS, D], BF16))
            xgT = es.enter_context(nc.sbuf_tensor("sb_xgT", [128, 16, CAP], BF16))
            sa = es.enter_context(nc.sbuf_tensor("sb_sa", [128, 2, CAP], F32))
            hTe = es.enter_context(nc.sbuf_tensor("sb_hTe", [128, 4, CAP], BF16))
            osb = es.enter_context(nc.sbuf_tensor("sb_osb", [128, NS, D], F32))
            ptr4 = es.enter_context(nc.psum_tensor("ps_ptr4", [128, 2, 8, 128], BF16))
            pau = es.enter_context(nc.psum_tensor("ps_pau", [128, 4, 512], F32))
            pdn = es.enter_context(nc.psum_tensor("ps_pdn", [128, 2, 512], F32))
            B_wgu = [Buf("wgu0"), Buf("wgu1")]
            B_wd = [Buf("wd0"), Buf("wd1")]
            B_xg = Buf("xg")
            B_xgT = Buf("xgT")
            B_sa = [Buf("sa0"), Buf("sa1")]
            B_hTe = Buf("hTe")
            B_osb = Buf("osb")
            B_ptr4 = [Buf("ptr40"), Buf("ptr41")]
            B_pau = [Buf("pa0"), Buf("pu0"), Buf("pa1"), Buf("pu1")]
            B_pdn = [Buf("pdn0"), Buf("pdn1")]
            c4 = {"w": 0, "tr": 0, "au": 0, "dn": 0}

            def load_w(e_, hh):
                wb = c4["w"] % 2
                c4["w"] += 1
                for (dst, src) in ((wg, w_gate), (wu, w_up)):
                    for q2 in range(2):
                        P.dma("pool", lambda e, dst=dst, src=src, e_=e_, hh=hh, wb=wb, q2=q2: e.dma_start(
                            out=dst[:, wb, q2 * 8:(q2 + 1) * 8, :], in_=src[e_ * D + q2 * 1024:e_ * D + (q2 + 1) * 1024, hh * 512:(hh + 1) * 512].rearrange("(c p) n -> p c n", p=128)),
                            writes=[B_wgu[wb]])
                P.dma("pool", lambda e, e_=e_, hh=hh, wb=wb: e.dma_start(
                    out=wd[:, wb, :, :], in_=w_down[e_ * DEXP + hh * 512:e_ * DEXP + (hh + 1) * 512, :].rearrange("(c p) n -> p c n", p=128)),
                    writes=[B_wd[wb]])
                return wb

            for e_ in range(NEXP):
                P.dma("sp", lambda e, e_=e_: e.dma_start(out=xg[:], in_=xbuf[e_ * CAP:(e_ + 1) * CAP, :].rearrange("(t p) d -> p t d", p=128)), writes=[B_xg])
                wbs = [load_w(e_, 0)]
                for st_ in range(NS):
                    for half in range(2):
                        pb = c4["tr"] % 2
                        c4["tr"] += 1
                        for j in range(8):
                            c = half * 8 + j
                            P.op("pe", lambda e, st_=st_, c=c, j=j, pb=pb: e.transpose(out=ptr4[:, pb, j, :], in_=xg[:, st_, c * 128:(c + 1) * 128], identity=identb[:]), reads=[B_xg, B_cb], writes=[B_ptr4[pb]])
                        if half == 0:
                            P.op("act", lambda e, st_=st_, half=half, pb=pb: e.copy(out=xgT[:, half * 8:(half + 1) * 8, st_ * 128:(st_ + 1) * 128], in_=ptr4[:, pb, :, :]), reads=[B_ptr4[pb]], writes=[B_xgT])
                        else:
                            P.op("dve", lambda e, st_=st_, half=half, pb=pb: e.tensor_copy(out=xgT[:, half * 8:(half + 1) * 8, st_ * 128:(st_ + 1) * 128], in_=ptr4[:, pb, :, :]), reads=[B_ptr4[pb]], writes=[B_xgT])
                for hh in range(2):
                    wb = wbs[hh]
                    if hh == 0:
                        wbs.append(load_w(e_, 1))
                    for ht in range(4):
                        ab = c4["au"] % 2
                        c4["au"] += 1
                        for (wsrc, pi_) in ((wg, 0), (wu, 1)):
                            for c in range(16):
                                P.op("pe", lambda e, wsrc=wsrc, pi_=pi_, c=c, ht=ht, wb=wb, ab=ab: e.matmul(out=pau[:, ab * 2 + pi_, 0:CAP], lhsT=wsrc[:, wb, c, ht * 128:(ht + 1) * 128], rhs=xgT[:, c, :], start=(c == 0), stop=(c == 15)),
                                     reads=[B_wgu[wb], B_xgT], writes=[B_pau[ab * 2 + pi_]])
                        P.op("act", lambda e, ab=ab: e.activation(out=sa[:, ab, :], in_=pau[:, ab * 2, 0:CAP], func=AF.Silu), reads=[B_pau[ab * 2]], writes=[B_sa[ab]])
                        P.op("dve", lambda e, ab=ab, ht=ht: e.tensor_tensor(out=hTe[:, ht, :], in0=pau[:, ab * 2 + 1, 0:CAP], in1=sa[:, ab, :], op=ALU.mult), reads=[B_pau[ab * 2 + 1], B_sa[ab]], writes=[B_hTe])
                    for st_ in range(NS):
                        for cb in range(4):
                            db = c4["dn"] % 2
                            c4["dn"] += 1
                            for ht in range(4):
                                P.op("pe", lambda e, st_=st_, cb=cb, ht=ht, wb=wb, db=db: e.matmul(out=pdn[:, db, :], lhsT=hTe[:, ht, st_ * 128:(st_ + 1) * 128], rhs=wd[:, wb, ht, cb * 512:(cb + 1) * 512], start=(ht == 0), stop=(ht == 3)),
                                     reads=[B_hTe, B_wd[wb]], writes=[B_pdn[db]])
                            if hh == 0:
                                P.op("act", lambda e, st_=st_, cb=cb, db=db: e.copy(out=osb[:, st_, cb * 512:(cb + 1) * 512], in_=pdn[:, db, :]), reads=[B_pdn[db]], writes=[B_osb])
                            else:
                                P.op("dve", lambda e, st_=st_, cb=cb, db=db: e.tensor_tensor(out=osb[:, st_, cb * 512:(cb + 1) * 512], in0=pdn[:, db, :], in1=osb[:, st_, cb * 512:(cb + 1) * 512], op=ALU.add), reads=[B_pdn[db], B_osb], writes=[B_osb])
                P.dma("sp", lambda e, e_=e_: e.dma_start(out=ybuf[e_ * CAP:(e_ + 1) * CAP, :].rearrange("(t p) d -> p t d", p=128), in_=osb[:]), reads=[B_osb])
            P.flush()

        with ExitStack() as es:
            y1 = es.enter_context(nc.sbuf_tensor("sb_y1", [128, 2, 2, D], F32))
            x5 = es.enter_context(nc.sbuf_tensor("sb_x5", [128, 2, D], F32))
            o5 = es.enter_context(nc.sbuf_tensor("sb_o5", [128, 2, D], F32))
            B_y1 = [Buf("y10"), Buf("y11")]
            B_x5 = [Buf("x50"), Buf("x51")]
            B_o5 = [Buf("o50"), Buf("o51")]
            for t in range(NT // 128):
                b = t % 2
                for k_ in range(2):
                    P.dma("pool", lambda e, t=t, k_=k_, b=b: e.indirect_dma_start(
                        out=y1[:, b, k_, :], out_offset=None, in_=ybuf[:, :],
                        in_offset=bass.IndirectOffsetOnAxis(ap=idx_t[:, t, k_:k_ + 1], axis=0)),
                        reads=[B_idx], writes=[B_y1[b]])
                P.dma("sp", lambda e, t=t, b=b: e.dma_start(out=x5[:, b, :], in_=x1s[t * 128:(t + 1) * 128, :]), writes=[B_x5[b]])
                P.op("dve", lambda e, t=t, b=b: e.scalar_tensor_tensor(out=o5[:, b, :], in0=y1[:, b, 0, :], scalar=wts_t[:, t, 0:1], in1=x5[:, b, :], op0=ALU.mult, op1=ALU.add),
                     reads=[B_y1[b], B_x5[b], B_idx], writes=[B_o5[b]])
                P.op("dve", lambda e, t=t, b=b: e.scalar_tensor_tensor(out=o5[:, b, :], in0=y1[:, b, 1, :], scalar=wts_t[:, t, 1:2], in1=o5[:, b, :], op0=ALU.mult, op1=ALU.add),
                     reads=[B_y1[b], B_o5[b], B_idx], writes=[B_o5[b]])
                P.dma("sp", lambda e, t=t, b=b: e.dma_start(out=y_out[t * 128:(t + 1) * 128, :], in_=o5[:, b, :]), reads=[B_o5[b]])
            P.flush()
    P.close()
    return nc


def _core_inputs(c, NU, HALO, x_prompt, x_sample, shared):
    xs = [x_prompt[c]]
    if NU == 3:
        s = c // 4
        s0 = (c % 4) * 4096
        xs.append(x_sample[s, s0:s0 + 4096])
    m = {"x_own": np.ascontiguousarray(np.concatenate(xs, 0))}
    if HALO:
        s = c // 4
        s0 = (c % 4) * 4096
        halo = np.zeros((2048, D), np.float32)
        if s0 > 0:
            halo[0:1024] = x_sample[s, s0 - 1024:s0]
        if s0 + 4096 < 16384:
            halo[1024:2048] = x_sample[s, s0 + 4096:s0 + 5120]
        m["x_halo"] = halo
    m["cst"] = _build_cst(c, NU, shared["rbias"], shared["gqk"])
    m["rope"] = _rope_tables(c, NU, HALO).reshape(-1, U)
    for k in ("g12", "w_in", "w_ab", "w_out", "wr", "rpb", "w_gate", "w_up", "w_down"):
        m[k] = shared[k]
    return m


def _shared_inputs(norm1_g, w_in, qn_a, kn_a, rpb_a, qn_b, kn_b, w_branch_a, w_branch_b, w_out, norm2_g,
                   router_group_w, router_group_b, router_expert_w, router_expert_b, w_gate, w_up, w_down):
    f = lambda a: np.ascontiguousarray(np.asarray(a, np.float32))
    return {
        "g12": f(np.stack([norm1_g[0], norm2_g[0]], 0)),
        "w_in": f(w_in[0]),
        "w_ab": f(np.concatenate([w_branch_a[0], w_branch_b[0]], 0)),
        "w_out": f(w_out[0]),
        "wr": f(np.concatenate([router_group_w[0], router_expert_w[0]], 1)),
        "rbias": f(np.concatenate([router_group_b[0], router_expert_b[0]], 0)),
        "gqk": f(np.stack([qn_a[0], kn_a[0], qn_b[0], kn_b[0]], 1)),
        "rpb": f(rpb_a[0].reshape(60, 31)),
        "w_gate": f(w_gate[0].reshape(NEXP * D, DEXP)),
        "w_up": f(w_up[0].reshape(NEXP * D, DEXP)),
        "w_down": f(w_down[0].reshape(NEXP * DEXP, D)),
    }


def kernel(x_prompt, x_sample, norm1_g, w_in, qn_a, kn_a, rpb_a, qn_b, kn_b, w_branch_a, w_branch_b, w_out,
           norm2_g, router_group_w, router_group_b, router_expert_w, router_expert_b, w_gate, w_up, w_down):
    x_prompt = np.asarray(x_prompt, np.float32)
    x_sample = np.asarray(x_sample, np.float32)
    shared = _shared_inputs(norm1_g, w_in, qn_a, kn_a, rpb_a, qn_b, kn_b, w_branch_a, w_branch_b, w_out, norm2_g,
                            router_group_w, router_group_b, router_expert_w, router_expert_b, w_gate, w_up, w_down)
    NU, CAP, HALO = 3, 512, True
    nc = build_program(NU, CAP, HALO)
    in_maps = [_core_inputs(c, NU, HALO, x_prompt, x_sample, shared) for c in range(8)]
    res = run_bass_kernel_spmd(nc, in_maps, core_ids=list(range(8)))
    y_prompt = np.empty((8, 2048, D), np.float32)
    y_sample = np.empty((2, 16384, D), np.float32)
    for c in range(8):
        y = res.results[c]["y"]
        y_prompt[c] = y[0:2048]
        s0 = (c % 4) * 4096
        y_sample[c // 4, s0:s0 + 4096] = y[2048:6144]
    return (y_prompt, y_sample)
```

```python
import math
from contextlib import ExitStack
import numpy as np
import concourse.bass as bass
import concourse.mybir as mybir
from concourse.bass_utils import run_bass_kernel_spmd

F32 = mybir.dt.float32
BF16 = mybir.dt.bfloat16
I32 = mybir.dt.int32
ALU = mybir.AluOpType
AF = mybir.ActivationFunctionType
AX = mybir.AxisListType

D = 2048
HD = 128
U = 2048
WIN = 4096
EPS = 1e-6
SCALE = HD ** -0.5
NEXP = 32
DEXP = 1024
DILS = (1, 4, 16)

CH = 16000
NDSEM = 24
GAP = 2


class Buf:
    __slots__ = ("name", "w", "r")

    def __init__(self, name):
        self.name = name
        self.w = None
        self.r = {}


class Prog:
    ENGS = ("pe", "act", "dve", "pool", "sp")

    def __init__(self, nc):
        self.nc = nc
        self.streams = {e: [] for e in self.ENGS}
        self.cnt = {e: 0 for e in self.ENGS}
        self.dcnt = {e: 0 for e in self.ENGS}
        self.waited = {e: {} for e in self.ENGS}
        self.semobjs = {}
        self.outstanding = []
        self.deferred = []
        self._cms = []

    def _sem(self, key):
        if key not in self.semobjs:
            cm = self.nc.semaphore("s_%s_%s_%d" % key)
            self._cms.append(cm)
            self.semobjs[key] = cm.__enter__()
        return self.semobjs[key]

    def _cref(self, eng, k):
        return (("c", eng, k // CH), k % CH + 1)

    def _add_wait(self, eng, waits, ref):
        key, val = ref
        if self.waited[eng].get(key, 0) >= val:
            return
        self.waited[eng][key] = val
        waits.append((key, val))

    def _deps(self, eng, reads, writes):
        refs = []
        for b in reads:
            if b.w is not None:
                refs.append(b.w)
        for b in writes:
            if b.w is not None:
                refs.append(b.w)
            refs.extend(b.r.items())
        return refs

    def _commit(self, ref, reads, writes):
        for b in reads:
            if b.r.get(ref[0], 0) < ref[1]:
                b.r[ref[0]] = ref[1]
        for b in writes:
            b.w = ref
            b.r = {}

    def op(self, eng, fn, reads=(), writes=()):
        waits = []
        k = self.cnt[eng]
        for ref in self._deps(eng, reads, writes):
            if ref[0][0] == "c" and ref[0][1] == eng:
                if eng == "pe":
                    continue
                if eng in ("dve", "act") and k - (ref[0][2] * CH + ref[1] - 1) >= GAP:
                    continue
            self._add_wait(eng, waits, ref)
        self.cnt[eng] += 1
        ref = self._cref(eng, k)
        self.streams[eng].append((waits, fn, (ref[0], 1)))
        self._commit(ref, reads, writes)
        return ref

    def dma(self, q, fn, reads=(), writes=(), defer=False):
        waits = []
        for ref in self._deps(q, reads, writes):
            self._add_wait(q, waits, ref)
        j = self.dcnt[q]
        self.dcnt[q] += 1
        key = ("d", q, j % NDSEM)
        prev = 16 * (j // NDSEM)
        if prev > 0:
            self._add_wait(q, waits, (key, prev))
        ref = (key, prev + 16)
        self.streams[q].append((waits, fn, (key, 16)))
        self._commit(ref, reads, writes)
        (self.deferred if defer else self.outstanding).append(ref)
        return ref

    def barrier(self, include_deferred=False):
        refs = []
        if include_deferred:
            refs.extend(self.deferred)
            self.deferred = []
        for e in self.ENGS:
            if self.cnt[e] > 0:
                refs.append(self._cref(e, self.cnt[e] - 1))
        refs.extend(self.outstanding)
        self.outstanding = []
        for e in self.ENGS:
            waits = []
            for ref in refs:
                self._add_wait(e, waits, ref)
            if waits:
                self.streams[e].append((waits, None, None))

    def flush(self, include_deferred=False):
        self.barrier(include_deferred)
        nc = self.nc
        for e in self.ENGS:
            for waits, fn, inc in self.streams[e]:
                for key, _ in waits:
                    self._sem(key)
                if inc is not None:
                    self._sem(inc[0])
        prog = self
        streams = self.streams
        self.streams = {e: [] for e in self.ENGS}

        def run(engname):
            def body(eng):
                for waits, fn, inc in streams[engname]:
                    for key, val in waits:
                        eng.wait_ge(prog.semobjs[key], val)
                    if fn is not None:
                        fn(eng).then_inc(prog.semobjs[inc[0]], inc[1])
            return body

        with nc.Block() as block:
            block.tensor(run("pe"))
            block.scalar(run("act"))
            block.vector(run("dve"))
            block.gpsimd(run("pool"))
            block.sync(run("sp"))

    def close(self):
        for cm in reversed(self._cms):
            cm.__exit__(None, None, None)


def _cst_layout(NU):
    off = {}
    cur = 0

    def add(name, n):
        nonlocal cur
        off[name] = (cur, n)
        cur += n
    add("ident", 128)
    add("ones", 128)
    add("onesm", 128)
    add("ustrict", 128)
    add("rm", 32)
    add("r2m", 64)
    add("ssel", 32)
    add("j2", 128)
    add("band4", 512)
    add("colvalid", 128)
    add("iota32", 32)
    add("iota8", 8)
    add("iota4", 4)
    add("rbias", 36)
    add("gqk", 4)
    add("negband", 256)
    add("rvint", 10)
    add("rv", NU * 16 * 14)
    add("kvb", NU * 3 * 8 * 4)
    return off, cur


def _unit_geom(c, u):
    if u == 0:
        return 2048, 0
    s0 = (c % 4) * 4096
    return 16384, s0 + (u - 1) * 2048


def _build_cst(c, NU, rbias, gqk):
    off, NC = _cst_layout(NU)
    cst = np.zeros((128, NC), np.float32)

    def put(name, arr):
        o, n = off[name]
        cst[:, o:o + n] = np.asarray(arr, np.float32).reshape(128, n)
    p = np.arange(128)
    put("ident", np.eye(128))
    put("ones", np.ones((128, 128)))
    put("onesm", np.full((128, 128), 1.0 / 128))
    put("ustrict", (p[:, None] < p[None, :]).astype(np.float32))
    rm = np.zeros((128, 32), np.float32)
    for m in range(16):
        rm[m + 16, m] = -1.0
        rm[m, m + 16] = 1.0
    put("rm", rm)
    r2m = np.zeros((128, 64), np.float32)
    for m in range(32):
        r2m[m, m] = 1.0
    r2m[:, 32:64] = rm
    put("r2m", r2m)
    ssel = np.zeros((128, 32), np.float32)
    for m in range(32):
        ssel[m, m] = 1.0
        ssel[32 + m, m] = 1.0
    put("ssel", ssel)
    j2 = np.zeros((128, 128), np.float32)
    for b in range(2):
        for q in range(64):
            j2[b * 64 + q, b * 64 + 63 - q] = 1.0
    put("j2", j2)
    bandA = (p[:, None] >= p[None, :]).astype(np.float32)
    bandB = (p[:, None] <= p[None, :]).astype(np.float32)
    put("band4", np.stack([bandA, bandB, bandA, bandB], axis=1))
    kc = np.arange(64)
    cs = np.clip(kc - 8, 0, 48)
    cv = ((kc[:, None] >= cs[None, :]) & (kc[:, None] < cs[None, :] + 16)).astype(np.float32)
    put("colvalid", np.tile(cv, (2, 2)))
    put("iota32", np.tile(np.arange(32, dtype=np.float32), (128, 1)))
    put("iota8", np.tile(np.arange(8, dtype=np.float32), (128, 1)))
    put("iota4", np.tile(np.arange(4, dtype=np.float32), (128, 1)))
    put("rbias", np.tile(rbias.reshape(1, 36), (128, 1)))
    put("gqk", gqk)
    rv = np.zeros((128, NU, 16, 7, 2), np.float32)
    kvd = np.zeros((128, NU, 3, 8, 4), np.float32)
    a = p // 64
    for u in range(NU):
        T, st = _unit_geom(c, u)
        rows = T // 64
        R0 = st // 64
        for n in range(16):
            for di in range(7):
                kr = R0 + 2 * (n + di - 3) + a
                for b in range(2):
                    qr = R0 + 2 * n + b
                    rs = min(max(qr - 4, 0), rows - 8)
                    rv[:, u, n, di, b] = ((kr >= rs) & (kr < rs + 8) & (kr >= 0) & (kr < rows)).astype(np.float32)
        for g, d in enumerate(DILS):
            nb = 16 // d
            blocks = [(r, n) for r in range(d) for n in range(nb)]
            for pi in range(8):
                for bi in range(2):
                    r, n = blocks[2 * pi + bi]
                    for ab in range(2):
                        m = n + ab
                        wp = 1024 + (m * 128 - 64 + p) * d + r
                        t = st - 1024 + wp
                        kvd[:, u, g, pi, bi * 2 + ab] = ((t >= 0) & (t < T)).astype(np.float32)
    put("rv", rv)
    put("kvb", (kvd - 1.0) * 30000.0)
    put("negband", (np.stack([bandA, bandB], axis=1) - 1.0) * 30000.0)
    rvint = np.zeros((128, 5, 2), np.float32)
    for di in range(5):
        for b in range(2):
            dd = 2 * (di - 2) + a - b
            rvint[:, di, b] = ((dd >= -4) & (dd <= 3)).astype(np.float32)
    put("rvint", rvint)
    return cst


def _rope_tables(c, NU, HALO):
    inv = 1.0 / (500000.0 ** (np.arange(16, dtype=np.float64) * (2.0 / 32)))
    tabs = []
    poss = []
    for u in range(NU):
        T, st = _unit_geom(c, u)
        poss.append(st + np.arange(U, dtype=np.float64))
    if HALO:
        s0 = (c % 4) * 4096
        poss.append(np.concatenate([s0 - 1024 + np.arange(1024.0), s0 + 4096 + np.arange(1024.0)]))
    for pos in poss:
        ang = np.float32(pos)[None, :].astype(np.float32) * inv.astype(np.float32)[:, None]
        cs_ = np.cos(ang.astype(np.float64))
        sn_ = np.sin(ang.astype(np.float64))
        tab = np.concatenate([cs_, cs_, sn_, sn_], 0)
        tabs.append(tab.astype(np.float32))
    return np.stack(tabs, 0)


def build_program(NU, CAP, HALO, debug=False):
    nc = bass.Bass("TRN2", target_bir_lowering=False)
    NT = NU * U
    NST = NU + (1 if HALO else 0)
    coff, NC = _cst_layout(NU)

    def dram_in(name, shape, dt=F32):
        return nc.dram_tensor(name, list(shape), dt, kind="ExternalInput").ap()

    x_own = dram_in("x_own", [NT, D])
    x_halo = dram_in("x_halo", [U, D]) if HALO else None
    cst_d = dram_in("cst", [128, NC])
    rope_d = dram_in("rope", [NST * 64, U])
    g12_d = dram_in("g12", [2, D])
    w_in = dram_in("w_in", [D, 10240])
    w_ab = dram_in("w_ab", [1024, D])
    w_out = dram_in("w_out", [D, D])
    wr_d = dram_in("wr", [D, 36])
    rpb_d = dram_in("rpb", [60, 31])
    w_gate = dram_in("w_gate", [NEXP * D, DEXP])
    w_up = dram_in("w_up", [NEXP * D, DEXP])
    w_down = dram_in("w_down", [NEXP * DEXP, D])
    y_out = nc.dram_tensor("y", [NT, D], F32, kind="ExternalOutput").ap()

    def scratch(name, shape, dt):
        kind = "ExternalOutput" if (debug and name in ("qT", "kTw", "vw", "gT", "oT", "x1s", "dbg")) else "Internal"
        return nc.dram_tensor(name, list(shape), dt, kind=kind).ap()

    qT = scratch("qT", [16 * 128, NT], BF16)
    kTw = scratch("kTw", [NU * 16 * 128, WIN], BF16)
    vw = scratch("vw", [NU * WIN, D], BF16)
    gT = scratch("gT", [32 * 128, NT], BF16)
    oT = scratch("oT", [8 * 128, NT], BF16)
    x1s = scratch("x1s", [NT, D], F32)
    xbuf = scratch("xbuf", [NEXP * CAP + 128, D], BF16)
    ybuf = scratch("ybuf", [NEXP * CAP + 128, D], F32)
    rpbp = scratch("rpbp", [60, 128], F32)
    dbg = scratch("dbg", [128, 64], F32) if debug else None

    P = Prog(nc)

    def cs(t, name):
        o, n = coff[name]
        return t[:, o:o + n]

    with ExitStack() as es:
        cst = es.enter_context(nc.sbuf_tensor("sb_cst", [128, NC], F32))
        identb = es.enter_context(nc.sbuf_tensor("sb_identb", [128, 128], BF16))
        onesb = es.enter_context(nc.sbuf_tensor("sb_onesb", [128, 128], BF16))
        onesmb = es.enter_context(nc.sbuf_tensor("sb_onesmb", [128, 128], BF16))
        ustrb = es.enter_context(nc.sbuf_tensor("sb_ustrb", [128, 128], BF16))
        rmb = es.enter_context(nc.sbuf_tensor("sb_rmb", [128, 32], BF16))
        r2mb = es.enter_context(nc.sbuf_tensor("sb_r2mb", [128, 64], BF16))
        sselb = es.enter_context(nc.sbuf_tensor("sb_sselb", [128, 32], BF16))
        idx_t = es.enter_context(nc.sbuf_tensor("sb_idx", [128, NT // 128, 2], I32))
        wts_t = es.enter_context(nc.sbuf_tensor("sb_wts", [128, NT // 128, 2], F32))
        B_cst = Buf("cst")
        B_cb = Buf("cstb")
        B_g = Buf("g12")
        B_idx = Buf("idx")
        B_zt = Buf("zt")
        identf = cs(cst, "ident")
        gqk = cs(cst, "gqk")

        P.dma("sp", lambda e: e.dma_start(out=cst[:], in_=cst_d[:, :]), writes=[B_cst])
        for dst, nm in ((identb, "ident"), (onesb, "ones"), (onesmb, "onesm"), (ustrb, "ustrict"), (rmb, "rm"), (r2mb, "r2m"), (sselb, "ssel")):
            P.op("dve", lambda e, dst=dst, nm=nm: e.tensor_copy(out=dst[:], in_=cs(cst, nm)), reads=[B_cst], writes=[B_cb])
        es_z = ExitStack()
        zt = es_z.enter_context(nc.sbuf_tensor("sb_zt", [128, 1, D], BF16))
        zf = es_z.enter_context(nc.sbuf_tensor("sb_zf", [128, 128], F32))
        negb = es_z.enter_context(nc.sbuf_tensor("sb_negb", [128, 2, 128], BF16))
        P.op("pool", lambda e: e.memset(zt[:], 0.0), writes=[B_zt])
        P.op("pool", lambda e: e.memset(zf[:], 0.0), writes=[B_zt])
        P.op("dve", lambda e: e.tensor_copy(out=negb[:].rearrange("p a b -> p (a b)"), in_=cs(cst, "negband")), reads=[B_cst], writes=[B_cb])
        nz = (NEXP * CAP + 128)
        r0 = 0
        while r0 < nz:
            nr = min(1024, nz - r0)
            P.dma("act", lambda e, r0=r0, nr=nr: e.dma_start(
                out=xbuf[r0:r0 + nr, :].rearrange("(t p) d -> p t d", p=128), in_=zt[:, 0:1, :].broadcast_to([128, nr // 128, D])), reads=[B_zt], defer=True)
            r0 += nr
        for (a0, a1) in ((0, 1024), (3072, 4096)):
            P.dma("sp", lambda e, a0=a0: e.dma_start(
                out=vw[a0:a0 + 1024, :].rearrange("(t p) d -> p t d", p=128), in_=zt[:, 0:1, :].broadcast_to([128, 8, D])), reads=[B_zt])
            for h in range(16):
                P.dma("sp", lambda e, a0=a0, h=h: e.dma_start(
                    out=kTw[h * 128:(h + 1) * 128, a0:a0 + 1024], in_=zt[:, 0, 0:1024]), reads=[B_zt])
        P.dma("sp", lambda e: e.dma_start(out=rpbp[0:60, :], in_=zf[0:60, :]), reads=[B_zt])
        P.flush()
        P.dma("sp", lambda e: e.dma_start(out=rpbp[0:60, 48:79], in_=rpb_d[:, :]))
        P.flush()

        with ExitStack() as es:
            g1bc = es.enter_context(nc.sbuf_tensor("sb_g1bc", [128, D], F32))
            xt = es.enter_context(nc.sbuf_tensor("sb_xt", [128, 3, D], F32))
            sqj = es.enter_context(nc.sbuf_tensor("sb_sqj", [128, D], BF16))
            hb = es.enter_context(nc.sbuf_tensor("sb_hb", [128, 3, D], BF16))
            st1 = es.enter_context(nc.sbuf_tensor("sb_st1", [128, 3, 4], F32))
            hT = es.enter_context(nc.sbuf_tensor("sb_hT", [128, 16, U], BF16))
            wt = es.enter_context(nc.sbuf_tensor("sb_wt", [128, 2, 16, 512], BF16))
            ropet = es.enter_context(nc.sbuf_tensor("sb_ropet", [64, U], F32))
            qs = es.enter_context(nc.sbuf_tensor("sb_qs", [128, 2, 512], F32))
            sq = es.enter_context(nc.sbuf_tensor("sb_sq", [128, 2, 512], BF16))
            rstd = es.enter_context(nc.sbuf_tensor("sb_rstd", [128, 2, 512], F32))
            qn = es.enter_context(nc.sbuf_tensor("sb_qn", [128, 4, 512], BF16))
            rt = es.enter_context(nc.sbuf_tensor("sb_rt", [64, 2, 512], BF16))
            vs = es.enter_context(nc.sbuf_tensor("sb_vs", [128, 2, 512], BF16))
            ptr = es.enter_context(nc.psum_tensor("ps_ptr", [128, 2, 8, 128], BF16))
            pacc = es.enter_context(nc.psum_tensor("ps_pacc", [128, 2, 512], F32))
            pm = es.enter_context(nc.psum_tensor("ps_pm", [128, 512], F32))
            prr = es.enter_context(nc.psum_tensor("ps_prr", [128, 2, 512], F32))
            B_xt = [Buf("xt0"), Buf("xt1"), Buf("xt2")]
            B_hb = [Buf("hb0"), Buf("hb1"), Buf("hb2")]
            B_st = [Buf("st0"), Buf("st1"), Buf("st2")]
            B_sqj = Buf("sqj")
            B_hT = [Buf("hT%d" % i) for i in range(16)]
            B_wt = [Buf("wt0"), Buf("wt1")]
            B_rope = Buf("rope")
            B_qs = [Buf("qs0"), Buf("qs1")]
            B_sq = [Buf("sq0"), Buf("sq1")]
            B_rstd = [Buf("rstd0"), Buf("rstd1")]
            B_qn = [Buf("qn0"), Buf("qn1"), Buf("qn2"), Buf("qn3")]
            B_rt = [Buf("rt0"), Buf("rt1")]
            B_vs = [Buf("vs0"), Buf("vs1")]
            B_ptr = [Buf("ptr0"), Buf("ptr1")]
            B_pacc = [Buf("pacc0"), Buf("pacc1")]
            B_pm = Buf("pm")
            B_prr = [Buf("prr0"), Buf("prr1")]
            cnt = {"x": 0, "tr": 0, "w": 0, "acc": 0, "q": 0, "v": 0}
            pend = []

            def pipe_push(main_fn, stages):
                main_fn()
                for st_ in pend:
                    if st_:
                        st_.pop(0)()
                pend.insert(0, [f for f in stages if f is not None])
                while pend and not pend[-1]:
                    pend.pop()

            def pipe_drain():
                while pend:
                    for st_ in pend:
                        if st_:
                            st_.pop(0)()
                    while pend and not pend[-1]:
                        pend.pop()
                    if pend and not any(pend):
                        pend.clear()
            P.dma("sp", lambda e: e.dma_start(out=g1bc[:], in_=g12_d[0, :].partition_broadcast(128)), writes=[B_g])

            def kv_dests(sti, ck):
                if sti < NU:
                    u = sti
                    d = [(u, 1024 + 512 * ck)]
                    if NU == 3 and u == 1 and ck >= 2:
                        d.append((2, 512 * (ck - 2)))
                    if NU == 3 and u == 2 and ck < 2:
                        d.append((1, 3072 + 512 * ck))
                    return d
                return [(1, 512 * ck)] if ck < 2 else [(2, 3072 + 512 * (ck - 2))]

            for sti in range(NST):
                halo = sti >= NU
                xsrc = x_halo if halo else x_own[sti * U:(sti + 1) * U, :]
                P.dma("sp", lambda e, sti=sti: e.dma_start(out=ropet[:], in_=rope_d[sti * 64:(sti + 1) * 64, :]), writes=[B_rope])
                for t in range(16):
                    xb = cnt["x"] % 3
                    cnt["x"] += 1
                    P.dma("sp", lambda e, t=t, xb=xb, xsrc=xsrc: e.dma_start(out=xt[:, xb, :], in_=xsrc[t * 128:(t + 1) * 128, :]), writes=[B_xt[xb]])
                    P.op("dve", lambda e, xb=xb: e.memset(st1[:, xb, 0:1], 0.0), writes=[B_st[xb]])
                    P.op("act", lambda e, xb=xb: e.activation(out=sqj[:], in_=xt[:, xb, :], func=AF.Square, accum_out=st1[:, xb, 0:1]),
                         reads=[B_xt[xb]], writes=[B_sqj, B_st[xb]])
                    P.op("act", lambda e, xb=xb: e.activation(out=st1[:, xb, 1:2], in_=st1[:, xb, 0:1], func=AF.Sqrt, bias=EPS, scale=1.0 / D),
                         reads=[B_st[xb]], writes=[B_st[xb]])
                    P.op("dve", lambda e, xb=xb: e.reciprocal(out=st1[:, xb, 2:3], in_=st1[:, xb, 1:2]),
                         reads=[B_st[xb]], writes=[B_st[xb]])
                    P.op("dve", lambda e, xb=xb: e.scalar_tensor_tensor(out=hb[:, xb, :], in0=xt[:, xb, :], scalar=st1[:, xb, 2:3], in1=g1bc[:], op0=ALU.mult, op1=ALU.mult),
                         reads=[B_xt[xb], B_st[xb], B_g], writes=[B_hb[xb]])
                    for half in range(2):
                        pb = cnt["tr"] % 2
                        cnt["tr"] += 1
                        for j in range(8):
                            c = half * 8 + j
                            P.op("pe", lambda e, xb=xb, c=c, j=j, pb=pb: e.transpose(out=ptr[:, pb, j, :], in_=hb[:, xb, c * 128:(c + 1) * 128], identity=identb[:]),
                                 reads=[B_hb[xb], B_cb], writes=[B_ptr[pb]])
                        eng = "act" if half == 0 else "dve"
                        if eng == "act":
                            P.op("act", lambda e, t=t, half=half, pb=pb: e.copy(out=hT[:, half * 8:(half + 1) * 8, t * 128:(t + 1) * 128], in_=ptr[:, pb, :, :]),
                                 reads=[B_ptr[pb]], writes=[B_hT[t]])
                        else:
                            P.op("dve", lambda e, t=t, half=half, pb=pb: e.tensor_copy(out=hT[:, half * 8:(half + 1) * 8, t * 128:(t + 1) * 128], in_=ptr[:, pb, :, :]),
                                 reads=[B_ptr[pb]], writes=[B_hT[t]])
                for cg in range(20):
                    kind = "q" if cg in (0, 3, 4, 5) else "k" if cg in (1, 6, 7, 8) else "v" if cg in (2, 9, 10, 11) else "g"
                    if halo and kind not in ("k", "v"):
                        continue
                    wb = cnt["w"] % 2
                    cnt["w"] += 1
                    P.dma("pool", lambda e, cg=cg, wb=wb: e.dma_start(out=wt[:, wb, :, :], in_=w_in[:, cg * 512:(cg + 1) * 512].rearrange("(c p) n -> p c n", p=128)),
                          writes=[B_wt[wb]])
                    if kind == "v":
                        pipe_drain()
                        hv0 = 0 if cg == 2 else 4 + (cg - 9) * 4
                        for t in range(16):
                            ab = cnt["acc"] % 2
                            cnt["acc"] += 1
                            for c in range(16):
                                P.op("pe", lambda e, t=t, c=c, wb=wb, ab=ab: e.matmul(out=pacc[:, ab, :], lhsT=hT[:, c, t * 128:(t + 1) * 128], rhs=wt[:, wb, c, :], start=(c == 0), stop=(c == 15)),
                                     reads=[B_hT[t], B_wt[wb]], writes=[B_pacc[ab]])
                            vb = cnt["v"] % 2
                            cnt["v"] += 1
                            if t % 2 == 0:
                                P.op("act", lambda e, ab=ab, vb=vb: e.copy(out=vs[:, vb, :], in_=pacc[:, ab, :]), reads=[B_pacc[ab]], writes=[B_vs[vb]])
                            else:
                                P.op("dve", lambda e, ab=ab, vb=vb: e.tensor_copy(out=vs[:, vb, :], in_=pacc[:, ab, :]), reads=[B_pacc[ab]], writes=[B_vs[vb]])
                            ck = t // 4
                            for (uu, wo) in kv_dests(sti, ck):
                                row = uu * WIN + wo + (t % 4) * 128
                                P.dma("sp", lambda e, row=row, hv0=hv0, vb=vb: e.dma_start(out=vw[row:row + 128, hv0 * 128:hv0 * 128 + 512], in_=vs[:, vb, :]), reads=[B_vs[vb]])
                        continue
                    for ct in range(4):
                        for ck in range(4):
                            ab = cnt["acc"] % 2
                            cnt["acc"] += 1
                            qb = cnt["q"] % 2
                            q3 = cnt["q"] % 4
                            cnt["q"] += 1

                            def main_fn(ct=ct, ck=ck, wb=wb, ab=ab):
                                for c in range(16):
                                    P.op("pe", lambda e, c=c: e.matmul(out=pacc[:, ab, :], lhsT=wt[:, wb, c, ct * 128:(ct + 1) * 128], rhs=hT[:, c, ck * 512:(ck + 1) * 512], start=(c == 0), stop=(c == 15)),
                                         reads=B_hT[ck * 4:ck * 4 + 4] + [B_wt[wb]], writes=[B_pacc[ab]])

                            if kind == "g":
                                gi = (cg - 12) * 4 + ct

                                def a_fn(ab=ab, q3=q3, gi=gi, sti=sti, ck=ck):
                                    P.op("act", lambda e: e.activation(out=qn[:, q3, :], in_=pacc[:, ab, :], func=AF.Sigmoid), reads=[B_pacc[ab]], writes=[B_qn[q3]])
                                    P.dma("sp", lambda e: e.dma_start(out=gT[gi * 128:(gi + 1) * 128, sti * U + ck * 512: sti * U + (ck + 1) * 512], in_=qn[:, q3, :]), reads=[B_qn[q3]])

                                pipe_push(main_fn, [a_fn])
                                continue
                            if cg <= 1:
                                head = ct
                                gcol = cg
                                rope = False
                            else:
                                head = 4 + ((cg - 3) if kind == "q" else (cg - 6)) * 4 + ct
                                gcol = 2 if kind == "q" else 3
                                rope = True

                            def a_fn(ab=ab, qb=qb, q3=q3, gcol=gcol):
                                P.op("act", lambda e: e.activation(out=qs[:, qb, :], in_=pacc[:, ab, :], func=AF.Copy, scale=gqk[:, gcol:gcol + 1]), reads=[B_pacc[ab], B_cst], writes=[B_qs[qb]])
                                P.op("act", lambda e: e.activation(out=sq[:, qb, :], in_=pacc[:, ab, :], func=AF.Square), reads=[B_pacc[ab]], writes=[B_sq[qb]])
                                P.op("pe", lambda e: e.matmul(out=pm[:], lhsT=onesmb[:], rhs=sq[:, qb, :], start=True, stop=True), reads=[B_sq[qb], B_cb], writes=[B_pm])
                                P.op("act", lambda e: e.activation(out=rstd[:, qb, :], in_=pm[:], func=AF.Ln, bias=EPS, scale=1.0), reads=[B_pm], writes=[B_rstd[qb]])
                                P.op("act", lambda e: e.activation(out=rstd[:, qb, :], in_=rstd[:, qb, :], func=AF.Exp, scale=-0.5), reads=[B_rstd[qb]], writes=[B_rstd[qb]])
                                P.op("dve", lambda e: e.tensor_tensor(out=qn[:, q3, :], in0=qs[:, qb, :], in1=rstd[:, qb, :], op=ALU.mult),
                                     reads=[B_qs[qb], B_rstd[qb]], writes=[B_qn[q3]])

                            def b_fn(qb=qb, q3=q3, ck=ck):
                                P.op("pe", lambda e: e.matmul(out=prr[0:64, 0, :], lhsT=r2mb[:], rhs=qn[:, q3, :], start=True, stop=True), reads=[B_qn[q3], B_cb], writes=[B_prr[0]])
                                P.op("dve", lambda e: e.tensor_tensor(out=rt[:, qb, :], in0=prr[0:64, 0, :], in1=ropet[:, ck * 512:(ck + 1) * 512], op=ALU.mult),
                                     reads=[B_prr[0], B_rope], writes=[B_rt[qb]])

                            def c_fn(qb=qb, q3=q3, rope=rope, kind=kind, head=head, sti=sti, ck=ck):
                                if rope:
                                    P.op("pe", lambda e: e.matmul(out=prr[0:32, 1, :], lhsT=sselb[0:64, :], rhs=rt[:, qb, :], start=True, stop=True), reads=[B_rt[qb], B_cb], writes=[B_prr[1]])
                                    P.op("act", lambda e: e.copy(out=qn[0:32, q3, :], in_=prr[0:32, 1, :]), reads=[B_prr[1]], writes=[B_qn[q3]])
                                if kind == "q":
                                    P.dma("sp", lambda e: e.dma_start(out=qT[head * 128:(head + 1) * 128, sti * U + ck * 512: sti * U + (ck + 1) * 512], in_=qn[:, q3, :]), reads=[B_qn[q3]])
                                else:
                                    for (uu, wo) in kv_dests(sti, ck):
                                        r_ = (uu * 16 + head) * 128
                                        P.dma("sp", lambda e, r_=r_, wo=wo: e.dma_start(out=kTw[r_:r_ + 128, wo:wo + 512], in_=qn[:, q3, :]), reads=[B_qn[q3]])

                            pipe_push(main_fn, [a_fn, b_fn if rope else None, c_fn])
                pipe_drain()
            P.flush()

        with ExitStack() as es:
            ebt = es.enter_context(nc.sbuf_tensor("sb_ebt", [128, 4, 7, 128], F32))
            ebt2 = es.enter_context(nc.sbuf_tensor("sb_ebt2", [128, 4, 5, 128], F32))
            hbk = es.enter_context(nc.sbuf_tensor("sb_hbk", [128, 4, 128], F32))
            ebtmp = es.enter_context(nc.sbuf_tensor("sb_ebtmp", [128, 4, 128], F32))
            kT = es.enter_context(nc.sbuf_tensor("sb_kT", [128, 2, WIN], BF16))
            qTt = es.enter_context(nc.sbuf_tensor("sb_qTt", [128, 2, U], BF16))
            vt = es.enter_context(nc.sbuf_tensor("sb_vt", [128, 2, 32, 128], BF16))
            eS = es.enter_context(nc.sbuf_tensor("sb_eS", [128, 2, 1024], F32))
            eS2 = es.enter_context(nc.sbuf_tensor("sb_eS2", [128, 2, 1024], F32))
            pT = es.enter_context(nc.sbuf_tensor("sb_pT", [128, 2, 1024], BF16))
            acc = es.enter_context(nc.sbuf_tensor("sb_acc", [128, 2, U], F32))
            rD = es.enter_context(nc.sbuf_tensor("sb_rD", [128, U], F32))
            oTt = es.enter_context(nc.sbuf_tensor("sb_oTt", [128, 8, U], BF16))
            psS = es.enter_context(nc.psum_tensor("ps_psS", [128, 2, 1024], F32))
            psO = es.enter_context(nc.psum_tensor("ps_psO", [128, 2, 512], F32))
            psE = es.enter_context(nc.psum_tensor("ps_psE", [128, 512], F32))
            B_ebt = Buf("ebt")
            B_hbk = [Buf("hbk%d" % i) for i in range(4)]
            B_ebtmp = [Buf("ebtmp%d" % i) for i in range(4)]
            B_psE = [Buf("psE%d" % i) for i in range(4)]
            ei = 0
            for h in range(4):
                for di in range(7):
                    delta = di - 3
                    k4 = ei % 4
                    ei += 1
                    for bp in range(2):
                        for a in range(2):
                            dr = 2 * delta + a - bp + 7
                            src = bass.AP(tensor=rpbp.tensor, offset=(h * 15 + dr) * 128, ap=[[1, 64], [1, 64]])
                            P.dma("sp", lambda e, src=src, bp=bp, a=a, k4=k4: e.dma_start(out=hbk[bp * 64:(bp + 1) * 64, k4, a * 64:(a + 1) * 64], in_=src), writes=[B_hbk[k4]])
                    P.op("pe", lambda e, k4=k4: e.matmul(out=psE[:, k4 * 128:(k4 + 1) * 128], lhsT=hbk[:, k4, :], rhs=cs(cst, "j2"), start=True, stop=True), reads=[B_hbk[k4], B_cst], writes=[B_psE[k4]])
                    P.op("act", lambda e, k4=k4: e.activation(out=ebtmp[:, k4, :], in_=psE[:, k4 * 128:(k4 + 1) * 128], func=AF.Exp), reads=[B_psE[k4]], writes=[B_ebtmp[k4]])
                    P.op("dve", lambda e, h=h, di=di, k4=k4: e.tensor_tensor(out=ebt[:, h, di, :], in0=ebtmp[:, k4, :], in1=cs(cst, "colvalid"), op=ALU.mult), reads=[B_ebtmp[k4], B_cst], writes=[B_ebt])
            rio = coff["rvint"][0]
            for h in range(4):
                P.op("dve", lambda e, h=h: e.tensor_tensor(out=ebt2[:, h, :, :].rearrange("p a (b c) -> p (a b) c", c=64), in0=ebt[:, h, 1:6, :].rearrange("p a (b c) -> p (a b) c", c=64),
                                                          in1=cst[:, rio:rio + 10].unsqueeze(2).broadcast_to([128, 10, 64]), op=ALU.mult), reads=[B_ebt, B_cst], writes=[B_ebt])

            B_kT = [Buf("kT0"), Buf("kT1")]
            B_qT = [Buf("qT0"), Buf("qT1")]
            B_vt = [Buf("vt0"), Buf("vt1")]
            B_psQ = [Buf("psQ%d" % i) for i in range(4)]
            B_pTQ = [Buf("pTQ%d" % i) for i in range(4)]
            B_psS = [[B_psQ[0], B_psQ[1]], [B_psQ[2], B_psQ[3]]]
            B_psO = [Buf("psO0"), Buf("psO1")]
            B_eS = [Buf("eS0"), Buf("eS1")]
            B_eS2 = [Buf("eS20"), Buf("eS21")]
            B_pT = [[B_pTQ[0], B_pTQ[1]], [B_pTQ[2], B_pTQ[3]]]
            B_oTn = [[Buf("oT%d_%d" % (j, n)) for n in range(16)] for j in range(8)]
            ac = {"h": 0, "s": 0, "o": 0, "q4": 0}
            rvo = coff["rv"][0]
            kbo = coff["kvb"][0]
            pend2 = []

            def push2(a_fn, b_fn, depth=1):
                a_fn()
                pend2.append(b_fn)
                while len(pend2) > depth:
                    pend2.pop(0)()

            def drain2():
                while pend2:
                    pend2.pop(0)()

            for u in range(NU):
                for h in range(4):
                    hbf = ac["h"] % 2
                    ac["h"] += 1
                    krow = (u * 16 + h) * 128
                    P.dma("sp", lambda e, krow=krow, hbf=hbf: e.dma_start(out=kT[:, hbf, 640:3456], in_=kTw[krow:krow + 128, 640:3456]), writes=[B_kT[hbf]])
                    P.dma("sp", lambda e, h=h, u=u, hbf=hbf: e.dma_start(out=qTt[:, hbf, :], in_=qT[h * 128:(h + 1) * 128, u * U:(u + 1) * U]), writes=[B_qT[hbf]])
                    P.dma("sp", lambda e, h=h, u=u, hbf=hbf: e.dma_start(out=vt[:, hbf, 0:22, :], in_=vw[u * WIN + 640:u * WIN + 3456, h * 128:(h + 1) * 128].rearrange("(t p) d -> p t d", p=128)), writes=[B_vt[hbf]])
                    for n in range(16):
                        sb = ac["s"] % 2
                        ac["s"] += 1
                        ob = ac["o"] % 2
                        ac["o"] += 1

                        interior = 2 <= n <= 13
                        dis = list(range(1, 6)) if interior else list(range(7))
                        nd = len(dis)

                        def a_fn(u=u, h=h, n=n, hbf=hbf, sb=sb, interior=interior, dis=dis, nd=nd):
                            for j, di in enumerate(dis):
                                m = n + di
                                P.op("pe", lambda e, m=m, j=j: e.matmul(out=psS[:, sb, j * 128:(j + 1) * 128], lhsT=kT[:, hbf, 640 + m * 128:640 + (m + 1) * 128], rhs=qTt[:, hbf, n * 128:(n + 1) * 128], start=True, stop=True),
                                     reads=[B_kT[hbf], B_qT[hbf]], writes=B_psS[sb])
                            P.op("act", lambda e: e.activation(out=eS[:, sb, 0:512], in_=psS[:, sb, 0:512], func=AF.Exp, scale=SCALE), reads=B_psS[sb], writes=[B_eS[sb]])
                            P.op("act", lambda e: e.activation(out=eS[:, sb, 512:nd * 128], in_=psS[:, sb, 512:nd * 128], func=AF.Exp, scale=SCALE), reads=B_psS[sb], writes=[B_eS[sb]])
                            if interior:
                                P.op("dve", lambda e: e.tensor_tensor(out=pT[:, sb, 0:640], in0=eS[:, sb, 0:640], in1=ebt2[:, h, :, :].rearrange("p a b -> p (a b)"), op=ALU.mult),
                                     reads=[B_eS[sb], B_ebt], writes=B_pT[sb])
                                return
                            P.op("dve", lambda e: e.tensor_tensor(out=eS2[:, sb, 0:896], in0=eS[:, sb, 0:896], in1=ebt[:, h, :, :].rearrange("p a b -> p (a b)"), op=ALU.mult),
                                 reads=[B_eS[sb], B_ebt], writes=[B_eS2[sb]])
                            ro = rvo + (u * 16 + n) * 14
                            P.op("pool", lambda e: e.tensor_tensor(out=pT[:, sb, 0:896].rearrange("p (a b) -> p a b", b=64), in0=eS2[:, sb, 0:896].rearrange("p (a b) -> p a b", b=64),
                                                                  in1=cst[:, ro:ro + 14].unsqueeze(2).broadcast_to([128, 14, 64]), op=ALU.mult),
                                 reads=[B_eS2[sb], B_cst], writes=B_pT[sb])

                        def b_fn(h=h, n=n, hbf=hbf, sb=sb, ob=ob, dis=dis, nd=nd):
                            for j, di in enumerate(dis):
                                m = n + di
                                P.op("pe", lambda e, m=m, j=j: e.matmul(out=psO[:, ob, 0:128], lhsT=vt[:, hbf, m, :], rhs=pT[:, sb, j * 128:(j + 1) * 128], start=(j == 0), stop=(j == nd - 1)),
                                     reads=[B_vt[hbf]] + B_pT[sb], writes=[B_psO[ob]])
                            for j in range(nd):
                                P.op("pe", lambda e, j=j: e.matmul(out=psO[:, ob, 128:256], lhsT=onesb[:], rhs=pT[:, sb, j * 128:(j + 1) * 128], start=(j == 0), stop=(j == nd - 1)),
                                     reads=[B_cb] + B_pT[sb], writes=[B_psO[ob]])
                            B_r = Buf("rDn")
                            P.op("dve", lambda e: e.reciprocal(out=rD[:, n * 128:(n + 1) * 128], in_=psO[:, ob, 128:256]), reads=[B_psO[ob]], writes=[B_r])
                            P.op("dve", lambda e: e.tensor_tensor(out=oTt[:, h, n * 128:(n + 1) * 128], in0=psO[:, ob, 0:128], in1=rD[:, n * 128:(n + 1) * 128], op=ALU.mult),
                                 reads=[B_psO[ob], B_r], writes=[B_oTn[h][n]])

                        push2(a_fn, b_fn)
                for hs in range(4):
                    B_accb = [[Buf("acc%d_%d" % (g, i)) for i in range(16)] for g in range(3)]
                    for g, d in enumerate(DILS):
                        head = 4 + 4 * g + hs
                        nb = 16 // d
                        hbf = ac["h"] % 2
                        ac["h"] += 1
                        krow = (u * 16 + head) * 128
                        P.dma("sp", lambda e, krow=krow, hbf=hbf: e.dma_start(out=kT[:, hbf, :], in_=kTw[krow:krow + 128, :]), writes=[B_kT[hbf]])
                        P.dma("sp", lambda e, head=head, u=u, hbf=hbf: e.dma_start(out=qTt[:, hbf, :], in_=qT[head * 128:(head + 1) * 128, u * U:(u + 1) * U]), writes=[B_qT[hbf]])
                        for r in range(d):
                            base = (u * WIN + 1024 - 64 * d + r) * D + head * 128
                            src = bass.AP(tensor=vw.tensor, offset=base, ap=[[d * D, 128], [128 * d * D, nb + 1], [1, 128]])
                            P.dma("sp", lambda e, src=src, r=r, nb=nb, hbf=hbf: e.dma_start(out=vt[:, hbf, r * (nb + 1):(r + 1) * (nb + 1), :], in_=src), writes=[B_vt[hbf]])
                        blocks = [(r, n) for r in range(d) for n in range(nb)]
                        for pi in range(8):
                            k4 = ac["q4"] % 4
                            ac["q4"] += 1
                            ob = ac["o"] % 2
                            ac["o"] += 1
                            sbh, so = k4 // 2, (k4 % 2) * 512

                            def a_fn(u=u, g=g, d=d, pi=pi, hbf=hbf, k4=k4, sbh=sbh, so=so, blocks=blocks):
                                for bi in range(2):
                                    r, n = blocks[2 * pi + bi]
                                    q0 = n * 128 * d + r
                                    for ab in range(2):
                                        m = n + ab
                                        k0 = 1024 + (m * 128 - 64) * d + r
                                        sub = bi * 2 + ab
                                        P.op("pe", lambda e, k0=k0, q0=q0, sub=sub: e.matmul(
                                            out=psS[:, sbh, so + sub * 128:so + (sub + 1) * 128],
                                            lhsT=kT[:, hbf, k0:k0 + 127 * d + 1:d], rhs=qTt[:, hbf, q0:q0 + 127 * d + 1:d], start=True, stop=False),
                                            reads=[B_kT[hbf], B_qT[hbf]], writes=[B_psQ[k4]])
                                        P.op("pe", lambda e, sub=sub, ab=ab: e.matmul(
                                            out=psS[:, sbh, so + sub * 128:so + (sub + 1) * 128], lhsT=identb[:], rhs=negb[:, ab, :], start=False, stop=True),
                                            reads=[B_cb], writes=[B_psQ[k4]])
                                ko = kbo + ((u * 3 + g) * 8 + pi) * 4
                                for sub in range(4):
                                    P.op("act", lambda e, sub=sub: e.activation(out=pT[:, sbh, so + sub * 128:so + (sub + 1) * 128], in_=psS[:, sbh, so + sub * 128:so + (sub + 1) * 128], func=AF.Exp,
                                                                                bias=cst[:, ko + sub:ko + sub + 1], scale=SCALE),
                                         reads=[B_psQ[k4], B_cst], writes=[B_pTQ[k4]])

                            def b_fn(g=g, d=d, nb=nb, pi=pi, hbf=hbf, k4=k4, sbh=sbh, so=so, ob=ob, blocks=blocks, B_accb=B_accb):
                                for bi in range(2):
                                    r, n = blocks[2 * pi + bi]
                                    for ab in range(2):
                                        m = n + ab
                                        P.op("pe", lambda e, r=r, m=m, bi=bi, ab=ab: e.matmul(
                                            out=psO[:, ob, bi * 256:bi * 256 + 128], lhsT=vt[:, hbf, r * (nb + 1) + m, :],
                                            rhs=pT[:, sbh, so + (bi * 2 + ab) * 128:so + (bi * 2 + ab + 1) * 128], start=(ab == 0), stop=(ab == 1)),
                                            reads=[B_vt[hbf], B_pTQ[k4]], writes=[B_psO[ob]])
                                    for ab in range(2):
                                        P.op("pe", lambda e, bi=bi, ab=ab: e.matmul(
                                            out=psO[:, ob, bi * 256 + 128:bi * 256 + 256], lhsT=onesb[:],
                                            rhs=pT[:, sbh, so + (bi * 2 + ab) * 128:so + (bi * 2 + ab + 1) * 128], start=(ab == 0), stop=(ab == 1)),
                                            reads=[B_cb, B_pTQ[k4]], writes=[B_psO[ob]])
                                for bi in range(2):
                                    r, n = blocks[2 * pi + bi]
                                    q0 = n * 128 * d + r
                                    src = psO[:, ob, bi * 256:(bi + 1) * 256].rearrange("p (a b) -> p a b", b=128)
                                    dst = acc[:, :, q0:q0 + 127 * d + 1:d]
                                    bb = B_accb[g][2 * pi + bi]
                                    if g == 0:
                                        P.op("dve", lambda e, src=src, dst=dst: e.tensor_copy(out=dst, in_=src), reads=[B_psO[ob]], writes=[bb])
                                    else:
                                        P.op("dve", lambda e, src=src, dst=dst: e.tensor_tensor(out=dst, in0=dst, in1=src, op=ALU.add), reads=[B_psO[ob]], writes=[bb])

                            push2(a_fn, b_fn, 2)
                    drain2()
                    allacc = [bb for gl in B_accb for bb in gl]
                    B_r = Buf("rDfull")
                    P.op("dve", lambda e: e.reciprocal(out=rD[:], in_=acc[:, 1, :]), reads=allacc, writes=[B_r])
                    P.op("dve", lambda e, hs=hs: e.tensor_tensor(out=oTt[:, 4 + hs, :], in0=acc[:, 0, :], in1=rD[:], op=ALU.mult), reads=allacc + [B_r], writes=B_oTn[4 + hs])
                for j in range(8):
                    P.dma("sp", lambda e, j=j, u=u: e.dma_start(out=oT[j * 128:(j + 1) * 128, u * U:(u + 1) * U], in_=oTt[:, j, :]), reads=B_oTn[j])
            P.flush(include_deferred=True)
        es_z.close()

        with ExitStack() as es:
            wab = es.enter_context(nc.sbuf_tensor("sb_wab", [128, 8, D], BF16))
            wo = es.enter_context(nc.sbuf_tensor("sb_wo", [128, 16, D], BF16))
            wrt = es.enter_context(nc.sbuf_tensor("sb_wrt", [128, 16, 36], F32))
            g2bc = es.enter_context(nc.sbuf_tensor("sb_g2bc", [128, D], F32))
            oc = es.enter_context(nc.sbuf_tensor("sb_oc", [128, 8, 256], BF16))
            gc = es.enter_context(nc.sbuf_tensor("sb_gc", [128, 32, 256], BF16))
            t12 = es.enter_context(nc.sbuf_tensor("sb_t12", [128, 2, 2, 256], F32))
            mix = es.enter_context(nc.sbuf_tensor("sb_mix", [128, 16, 256], BF16))
            x1 = es.enter_context(nc.sbuf_tensor("sb_x1", [128, 1, D], F32))
            h2f = es.enter_context(nc.sbuf_tensor("sb_h2f", [128, D], F32))
            h2b = es.enter_context(nc.sbuf_tensor("sb_h2b", [128, 2, D], BF16))
            h2T = es.enter_context(nc.sbuf_tensor("sb_h2T", [128, 16, 128], F32))
            rs2 = es.enter_context(nc.sbuf_tensor("sb_rs", [128, 2, 96], F32))
            lg2 = es.enter_context(nc.sbuf_tensor("sb_lg", [128, 2, 36], F32))
            ohb2 = es.enter_context(nc.sbuf_tensor("sb_ohb", [128, 2, 32], BF16))
            oh2 = es.enter_context(nc.sbuf_tensor("sb_oh", [128, 2, 3, 32], F32))
            tot = es.enter_context(nc.sbuf_tensor("sb_tot", [128, 32], F32))
            pos2 = es.enter_context(nc.sbuf_tensor("sb_pos", [128, 2, 32], F32))
            pya = es.enter_context(nc.psum_tensor("ps_pya", [128, 2, 512], F32))
            pout = es.enter_context(nc.psum_tensor("ps_pout", [128, 2, 512], F32))
            ptr3 = es.enter_context(nc.psum_tensor("ps_ptr3", [128, 2, 512], F32))
            plg = es.enter_context(nc.psum_tensor("ps_plg", [128, 512], F32))
            B_w3 = Buf("w3")
            B_oc = Buf("oc")
            B_gc = Buf("gc")
            B_t12 = [Buf("t120"), Buf("t121")]
            B_mix = Buf("mix")
            B_x3 = [Buf("x30"), Buf("x31")]
            B_x1 = [Buf("x10"), Buf("x11")]
            B_h2f = Buf("h2f")
            B_h2b = [Buf("h2b0"), Buf("h2b1")]
            B_h2T = Buf("h2T")
            B_rs = Buf("rs")
            B_lg = Buf("lg")
            B_oh = Buf("oh")
            B_tot = Buf("tot")
            B_pos = Buf("pos")
            B_pya = [Buf("pya0"), Buf("pya1")]
            B_pout = [Buf("pout0"), Buf("pout1")]
            B_ptr3 = [Buf("ptr30"), Buf("ptr31")]
            B_plg = Buf("plg")
            P.dma("pool", lambda e: e.dma_start(out=wab[:], in_=w_ab.rearrange("(c p) n -> p c n", p=128)), writes=[B_w3])
            for q4 in range(4):
                P.dma("pool", lambda e, q4=q4: e.dma_start(out=wo[:, q4 * 4:(q4 + 1) * 4, :], in_=w_out[q4 * 512:(q4 + 1) * 512, :].rearrange("(c p) n -> p c n", p=128)), writes=[B_w3])
            P.dma("sp", lambda e: e.dma_start(out=wrt[:], in_=wr_d.rearrange("(c p) n -> p c n", p=128)), writes=[B_w3])
            P.op("dve", lambda e: e.memset(tot[:], 0.0), writes=[B_tot])
            c3 = {"t": 0, "x": 0, "o": 0, "tr": 0, "h": 0}
            P.dma("sp", lambda e: e.dma_start(out=g2bc[:], in_=g12_d[1, :].partition_broadcast(128)), writes=[B_g])
            B_rs2 = [Buf("rs0"), Buf("rs1")]
            B_lg2 = [Buf("lg0"), Buf("lg1")]
            B_oh2 = [Buf("oh0"), Buf("oh1")]
            B_pos2 = [Buf("pos0"), Buf("pos1")]
            B_plg2 = [Buf("plg0"), Buf("plg1")]

            def branch_pair(row0):
                P.dma("sp", lambda e: e.dma_start(out=oc[:], in_=oT[:, row0:row0 + 256].rearrange("(j p) t -> p j t", p=128)), writes=[B_oc])
                P.dma("sp", lambda e: e.dma_start(out=gc[:], in_=gT[:, row0:row0 + 256].rearrange("(j p) t -> p j t", p=128)), writes=[B_gc])
                for ft in range(16):
                    tb = c3["t"] % 2
                    c3["t"] += 1
                    for br in range(2):
                        for c in range(4):
                            P.op("pe", lambda e, br=br, c=c, ft=ft: e.matmul(out=pya[:, br, 0:256], lhsT=wab[:, br * 4 + c, ft * 128:(ft + 1) * 128], rhs=oc[:, br * 4 + c, :], start=(c == 0), stop=(c == 3)),
                                 reads=[B_w3, B_oc], writes=[B_pya[br]])
                        P.op("dve", lambda e, br=br, ft=ft, tb=tb: e.tensor_tensor(out=t12[:, tb, br, :], in0=pya[:, br, 0:256], in1=gc[:, br * 16 + ft, :], op=ALU.mult),
                             reads=[B_pya[br], B_gc], writes=[B_t12[tb]])
                    P.op("pool", lambda e, ft=ft, tb=tb: e.tensor_tensor(out=mix[:, ft, :], in0=t12[:, tb, 0, :], in1=t12[:, tb, 1, :], op=ALU.add), reads=[B_t12[tb]], writes=[B_mix])

            def pre_part(tix, s_):
                row0 = tix * 128
                rs = rs2[:, s_, :]
                P.dma("sp", lambda e: e.dma_start(out=x1[:, 0, :], in_=x_own[row0:row0 + 128, :]), writes=[B_x1[0]])
                for cb in range(4):
                    ob = c3["o"] % 2
                    c3["o"] += 1
                    for ft in range(16):
                        P.op("pe", lambda e, ft=ft, cb=cb, ob=ob: e.matmul(out=pout[:, ob, :], lhsT=mix[:, ft, s_ * 128:(s_ + 1) * 128], rhs=wo[:, ft, cb * 512:(cb + 1) * 512], start=(ft == 0), stop=(ft == 15)),
                             reads=[B_mix, B_w3], writes=[B_pout[ob]])
                    P.op("dve", lambda e, cb=cb, ob=ob: e.tensor_tensor(out=x1[:, 0, cb * 512:(cb + 1) * 512], in0=pout[:, ob, :], in1=x1[:, 0, cb * 512:(cb + 1) * 512], op=ALU.add),
                         reads=[B_pout[ob], B_x1[0]], writes=[B_x1[0]])
                P.dma("sp", lambda e: e.dma_start(out=x1s[row0:row0 + 128, :], in_=x1[:, 0, :]), reads=[B_x1[0]])
                P.op("dve", lambda e: e.memset(rs[:, 0:1], 0.0), writes=[B_rs2[s_]])
                P.op("dve", lambda e: e.memset(rs[:, 5:6], 0.0), writes=[B_rs2[s_]])
                P.op("act", lambda e: e.activation(out=h2b[:, s_, :], in_=x1[:, 0, :], func=AF.Square, accum_out=rs[:, 0:1]), reads=[B_x1[0], B_rs2[s_]], writes=[B_h2b[s_], B_rs2[s_]])
                P.op("act", lambda e: e.activation(out=rs[:, 1:2], in_=rs[:, 0:1], func=AF.Sqrt, bias=EPS, scale=1.0 / D), reads=[B_rs2[s_]], writes=[B_rs2[s_]])
                P.op("dve", lambda e: e.reciprocal(out=rs[:, 2:3], in_=rs[:, 1:2]), reads=[B_rs2[s_]], writes=[B_rs2[s_]])
                P.op("dve", lambda e: e.scalar_tensor_tensor(out=h2f[:], in0=x1[:, 0, :], scalar=rs[:, 2:3], in1=g2bc[:], op0=ALU.mult, op1=ALU.mult),
                     reads=[B_x1[0], B_rs2[s_], B_g], writes=[B_h2f])
                P.op("act", lambda e: e.copy(out=h2b[:, s_, :], in_=h2f[:]), reads=[B_h2f], writes=[B_h2b[s_]])
                for q4 in range(4):
                    pb = c3["tr"] % 2
                    c3["tr"] += 1
                    for j in range(4):
                        c = q4 * 4 + j
                        P.op("pe", lambda e, c=c, j=j, pb=pb: e.transpose(out=ptr3[:, pb, j * 128:(j + 1) * 128], in_=h2f[:, c * 128:(c + 1) * 128], identity=identf), reads=[B_h2f, B_cst], writes=[B_ptr3[pb]])
                    P.op("act", lambda e, q4=q4, pb=pb: e.copy(out=h2T[:, q4 * 4:(q4 + 1) * 4, :], in_=ptr3[:, pb, :].rearrange("p (a b) -> p a b", b=128)), reads=[B_ptr3[pb]], writes=[B_h2T])
                for c in range(16):
                    P.op("pe", lambda e, c=c: e.matmul(out=plg[:, s_ * 256:s_ * 256 + 36], lhsT=h2T[:, c, :], rhs=wrt[:, c, :], start=(c == 0), stop=(c == 15)), reads=[B_h2T, B_w3], writes=[B_plg2[s_]])
                P.op("dve", lambda e: e.tensor_tensor(out=lg2[:, s_, :], in0=plg[:, s_ * 256:s_ * 256 + 36], in1=cs(cst, "rbias"), op=ALU.add), reads=[B_plg2[s_], B_cst], writes=[B_lg2[s_]])

            def chain1(tix, s_):
                rs = rs2[:, s_, :]
                lg = lg2[:, s_, :]
                oh = oh2[:, s_, :, :]
                ohb = ohb2[:, s_, :]
                R = [B_rs2[s_], B_lg2[s_], B_oh2[s_]]
                ops = []

                def dv(fn, extra_r=(), extra_w=()):
                    ops.append(("dve", fn, R + list(extra_r), R + list(extra_w)))
                dv(lambda e: e.reduce_max(out=rs[:, 3:4], in_=lg[:, 0:4], axis=AX.X))
                dv(lambda e: e.tensor_scalar(out=rs[:, 8:12], in0=lg[:, 0:4], scalar1=rs[:, 3:4], scalar2=None, op0=ALU.is_equal))
                dv(lambda e: e.tensor_scalar(out=rs[:, 4:5], in0=rs[:, 3:4], scalar1=-1.0, scalar2=None, op0=ALU.mult))
                ops.append(("act", lambda e: e.activation(out=rs[:, 12:16], in_=lg[:, 0:4], func=AF.Exp, bias=rs[:, 4:5], scale=1.0, accum_out=rs[:, 5:6]), R, R))
                dv(lambda e: e.reciprocal(out=rs[:, 6:7], in_=rs[:, 5:6]))
                dv(lambda e: e.tensor_scalar(out=rs[:, 16:24], in0=lg[:, 4:12], scalar1=rs[:, 8:9], scalar2=None, op0=ALU.mult))
                for g_ in range(1, 4):
                    dv(lambda e, g_=g_: e.scalar_tensor_tensor(out=rs[:, 16:24], in0=lg[:, 4 + 8 * g_:12 + 8 * g_], scalar=rs[:, 8 + g_:9 + g_], in1=rs[:, 16:24], op0=ALU.mult, op1=ALU.add))
                dv(lambda e: e.reduce_max(out=rs[:, 24:25], in_=rs[:, 16:24], axis=AX.X))
                dv(lambda e: e.tensor_scalar(out=rs[:, 32:40], in0=rs[:, 16:24], scalar1=rs[:, 24:25], scalar2=None, op0=ALU.is_equal))
                dv(lambda e: e.scalar_tensor_tensor(out=rs[:, 40:48], in0=rs[:, 32:40], scalar=-1e30, in1=rs[:, 16:24], op0=ALU.mult, op1=ALU.add))
                dv(lambda e: e.reduce_max(out=rs[:, 25:26], in_=rs[:, 40:48], axis=AX.X))
                dv(lambda e: e.tensor_scalar(out=rs[:, 48:56], in0=rs[:, 40:48], scalar1=rs[:, 25:26], scalar2=None, op0=ALU.is_equal))
                dv(lambda e: e.tensor_scalar(out=rs[:, 26:27], in0=rs[:, 24:25], scalar1=-1.0, scalar2=None, op0=ALU.mult))
                ops.append(("act", lambda e: e.activation(out=rs[:, 27:28], in_=rs[:, 25:26], func=AF.Exp, bias=rs[:, 26:27], scale=1.0), R, R))
                dv(lambda e: e.tensor_scalar(out=rs[:, 28:29], in0=rs[:, 27:28], scalar1=1.0, scalar2=None, op0=ALU.add))
                dv(lambda e: e.reciprocal(out=rs[:, 29:30], in_=rs[:, 28:29]))
                dv(lambda e: e.tensor_tensor(out=wts_t[:, tix, 0:1], in0=rs[:, 29:30], in1=rs[:, 6:7], op=ALU.mult), extra_w=[B_idx])
                dv(lambda e: e.tensor_tensor(out=wts_t[:, tix, 1:2], in0=wts_t[:, tix, 0:1], in1=rs[:, 27:28], op=ALU.mult), extra_r=[B_idx], extra_w=[B_idx])
                dv(lambda e: e.tensor_tensor(out=rs[:, 56:60], in0=rs[:, 8:12], in1=cs(cst, "iota4"), op=ALU.mult), extra_r=[B_cst])
                dv(lambda e: e.reduce_sum(out=rs[:, 60:61], in_=rs[:, 56:60], axis=AX.X))
                dv(lambda e: e.tensor_tensor(out=rs[:, 64:72], in0=rs[:, 32:40], in1=cs(cst, "iota8"), op=ALU.mult), extra_r=[B_cst])
                dv(lambda e: e.reduce_sum(out=rs[:, 61:62], in_=rs[:, 64:72], axis=AX.X))
                dv(lambda e: e.tensor_tensor(out=rs[:, 72:80], in0=rs[:, 48:56], in1=cs(cst, "iota8"), op=ALU.mult), extra_r=[B_cst])
                dv(lambda e: e.reduce_sum(out=rs[:, 62:63], in_=rs[:, 72:80], axis=AX.X))
                dv(lambda e: e.scalar_tensor_tensor(out=rs[:, 80:81], in0=rs[:, 60:61], scalar=8.0, in1=rs[:, 61:62], op0=ALU.mult, op1=ALU.add))
                dv(lambda e: e.scalar_tensor_tensor(out=rs[:, 81:82], in0=rs[:, 60:61], scalar=8.0, in1=rs[:, 62:63], op0=ALU.mult, op1=ALU.add))
                dv(lambda e: e.tensor_scalar(out=oh[:, 0, :], in0=cs(cst, "iota32"), scalar1=rs[:, 80:81], scalar2=None, op0=ALU.is_equal), extra_r=[B_cst])
                dv(lambda e: e.tensor_scalar(out=oh[:, 1, :], in0=cs(cst, "iota32"), scalar1=rs[:, 81:82], scalar2=None, op0=ALU.is_equal), extra_r=[B_cst])
                dv(lambda e: e.tensor_tensor(out=ohb, in0=oh[:, 0, :], in1=oh[:, 1, :], op=ALU.add))
                return ops

            def chain2(tix, s_):
                ohb = ohb2[:, s_, :]
                pos = pos2[:, s_, :]
                o_ = s_ * 256
                P.op("pe", lambda e: e.matmul(out=plg[:, o_ + 64:o_ + 96], lhsT=ustrb[:], rhs=ohb, start=True, stop=True), reads=[B_oh2[s_], B_cb], writes=[B_plg2[s_]])
                P.op("pe", lambda e: e.matmul(out=plg[:, o_ + 128:o_ + 160], lhsT=onesb[:], rhs=ohb, start=True, stop=True), reads=[B_oh2[s_], B_cb], writes=[B_plg2[s_]])
                P.op("dve", lambda e: e.tensor_tensor(out=pos, in0=plg[:, o_ + 64:o_ + 96], in1=tot[:], op=ALU.add), reads=[B_plg2[s_], B_tot], writes=[B_pos2[s_]])
                P.op("dve", lambda e: e.tensor_tensor(out=tot[:], in0=plg[:, o_ + 128:o_ + 160], in1=tot[:], op=ALU.add), reads=[B_plg2[s_], B_tot], writes=[B_tot])

            def chain3(tix, s_):
                rs = rs2[:, s_, :]
                oh = oh2[:, s_, :, :]
                pos = pos2[:, s_, :]
                R = [B_rs2[s_], B_oh2[s_]]
                ops = []

                def dv(fn, extra_r=(), extra_w=()):
                    ops.append(("dve", fn, R + list(extra_r), R + list(extra_w)))
                for k_ in range(2):
                    dv(lambda e, k_=k_: e.tensor_tensor(out=oh[:, 2, :], in0=oh[:, k_, :], in1=pos, op=ALU.mult), extra_r=[B_pos2[s_]])
                    dv(lambda e, k_=k_: e.reduce_sum(out=rs[:, 84 + k_:85 + k_], in_=oh[:, 2, :], axis=AX.X))
                    dv(lambda e, k_=k_: e.tensor_scalar(out=rs[:, 84 + k_:85 + k_], in0=rs[:, 84 + k_:85 + k_], scalar1=float(CAP - 1), scalar2=None, op0=ALU.min))
                    dv(lambda e, k_=k_: e.scalar_tensor_tensor(out=rs[:, 88 + k_:89 + k_], in0=rs[:, 80 + k_:81 + k_], scalar=float(CAP), in1=rs[:, 84 + k_:85 + k_], op0=ALU.mult, op1=ALU.add))
                    dv(lambda e, k_=k_: e.tensor_copy(out=idx_t[:, tix, k_:k_ + 1], in_=rs[:, 88 + k_:89 + k_]), extra_w=[B_idx])
                return ops

            def interleave(la, lb):
                for oa, ob_ in zip(la, lb):
                    P.op(oa[0], oa[1], reads=oa[2], writes=oa[3])
                    P.op(ob_[0], ob_[1], reads=ob_[2], writes=ob_[3])

            for pr in range(NT // 256):
                tA, tB = 2 * pr, 2 * pr + 1
                branch_pair(pr * 256)
                pre_part(tA, 0)
                pre_part(tB, 1)
                interleave(chain1(tA, 0), chain1(tB, 1))
                chain2(tA, 0)
                chain2(tB, 1)
                interleave(chain3(tA, 0), chain3(tB, 1))
                for (tix, s_) in ((tA, 0), (tB, 1)):
                    for k_ in range(2):
                        P.dma("pool", lambda e, k_=k_, tix=tix, s_=s_: e.indirect_dma_start(
                            out=xbuf[:, :], out_offset=bass.IndirectOffsetOnAxis(ap=idx_t[:, tix, k_:k_ + 1], axis=0),
                            in_=h2b[:, s_, :], in_offset=None),
                            reads=[B_h2b[s_], B_idx])
            if debug:
                P.dma("sp", lambda e: e.dma_start(out=dbg[:, 0:32], in_=tot[:]), reads=[B_tot])
            P.flush()

        NS = CAP // 128
        with ExitStack() as es:
            wg = es.enter_context(nc.sbuf_tensor("sb_wg", [128, 2, 16, 512], BF16))
            wu = es.enter_context(nc.sbuf_tensor("sb_wu", [128, 2, 16, 512], BF16))
            wd = es.enter_context(nc.sbuf_tensor("sb_wd", [128, 2, 4, D], BF16))
            xg = es.enter_context(nc.sbuf_tensor("sb_xg", [128, NS, D], BF16))
            xgT = es.enter_context(nc.sbuf_tensor("sb_xgT", [128, 16, CAP], BF16))
            sa = es.enter_context(nc.sbuf_tensor("sb_sa", [128, 2, CAP], F32))
            hTe = es.enter_context(nc.sbuf_tensor("sb_hTe", [128, 4, CAP], BF16))
            osb = es.enter_context(nc.sbuf_tensor("sb_osb", [128, NS, D], F32))
            ptr4 = es.enter_context(nc.psum_tensor("ps_ptr4", [128, 2, 8, 128], BF16))
            pau = es.enter_context(nc.psum_tensor("ps_pau", [128, 4, 512], F32))
            pdn = es.enter_context(nc.psum_tensor("ps_pdn", [128, 2, 512], F32))
            B_wgu = [Buf("wgu0"), Buf("wgu1")]
            B_wd = [Buf("wd0"), Buf("wd1")]
            B_xg = Buf("xg")
            B_xgT = Buf("xgT")
            B_sa = [Buf("sa0"), Buf("sa1")]
            B_hTe = Buf("hTe")
            B_osb = Buf("osb")
            B_ptr4 = [Buf("ptr40"), Buf("ptr41")]
            B_pau = [Buf("pa0"), Buf("pu0"), Buf("pa1"), Buf("pu1")]
            B_pdn = [Buf("pdn0"), Buf("pdn1")]
            c4 = {"w": 0, "tr": 0, "au": 0, "dn": 0}

            def load_w(e_, hh):
                wb = c4["w"] % 2
                c4["w"] += 1
                for (dst, src) in ((wg, w_gate), (wu, w_up)):
                    for q2 in range(2):
                        P.dma("pool", lambda e, dst=dst, src=src, e_=e_, hh=hh, wb=wb, q2=q2: e.dma_start(
                            out=dst[:, wb, q2 * 8:(q2 + 1) * 8, :], in_=src[e_ * D + q2 * 1024:e_ * D + (q2 + 1) * 1024, hh * 512:(hh + 1) * 512].rearrange("(c p) n -> p c n", p=128)),
                            writes=[B_wgu[wb]])
                P.dma("pool", lambda e, e_=e_, hh=hh, wb=wb: e.dma_start(
                    out=wd[:, wb, :, :], in_=w_down[e_ * DEXP + hh * 512:e_ * DEXP + (hh + 1) * 512, :].rearrange("(c p) n -> p c n", p=128)),
                    writes=[B_wd[wb]])
                return wb

            for e_ in range(NEXP):
                P.dma("sp", lambda e, e_=e_: e.dma_start(out=xg[:], in_=xbuf[e_ * CAP:(e_ + 1) * CAP, :].rearrange("(t p) d -> p t d", p=128)), writes=[B_xg])
                wbs = [load_w(e_, 0)]
                for st_ in range(NS):
                    for half in range(2):
                        pb = c4["tr"] % 2
                        c4["tr"] += 1
                        for j in range(8):
                            c = half * 8 + j
                            P.op("pe", lambda e, st_=st_, c=c, j=j, pb=pb: e.transpose(out=ptr4[:, pb, j, :], in_=xg[:, st_, c * 128:(c + 1) * 128], identity=identb[:]), reads=[B_xg, B_cb], writes=[B_ptr4[pb]])
                        if half == 0:
                            P.op("act", lambda e, st_=st_, half=half, pb=pb: e.copy(out=xgT[:, half * 8:(half + 1) * 8, st_ * 128:(st_ + 1) * 128], in_=ptr4[:, pb, :, :]), reads=[B_ptr4[pb]], writes=[B_xgT])
                        else:
                            P.op("dve", lambda e, st_=st_, half=half, pb=pb: e.tensor_copy(out=xgT[:, half * 8:(half + 1) * 8, st_ * 128:(st_ + 1) * 128], in_=ptr4[:, pb, :, :]), reads=[B_ptr4[pb]], writes=[B_xgT])
                for hh in range(2):
                    wb = wbs[hh]
                    if hh == 0:
                        wbs.append(load_w(e_, 1))
                    for ht in range(4):
                        ab = c4["au"] % 2
                        c4["au"] += 1
                        for (wsrc, pi_) in ((wg, 0), (wu, 1)):
                            for c in range(16):
                                P.op("pe", lambda e, wsrc=wsrc, pi_=pi_, c=c, ht=ht, wb=wb, ab=ab: e.matmul(out=pau[:, ab * 2 + pi_, 0:CAP], lhsT=wsrc[:, wb, c, ht * 128:(ht + 1) * 128], rhs=xgT[:, c, :], start=(c == 0), stop=(c == 15)),
                                     reads=[B_wgu[wb], B_xgT], writes=[B_pau[ab * 2 + pi_]])
                        P.op("act", lambda e, ab=ab: e.activation(out=sa[:, ab, :], in_=pau[:, ab * 2, 0:CAP], func=AF.Silu), reads=[B_pau[ab * 2]], writes=[B_sa[ab]])
                        P.op("dve", lambda e, ab=ab, ht=ht: e.tensor_tensor(out=hTe[:, ht, :], in0=pau[:, ab * 2 + 1, 0:CAP], in1=sa[:, ab, :], op=ALU.mult), reads=[B_pau[ab * 2 + 1], B_sa[ab]], writes=[B_hTe])
                    for st_ in range(NS):
                        for cb in range(4):
                            db = c4["dn"] % 2
                            c4["dn"] += 1
                            for ht in range(4):
                                P.op("pe", lambda e, st_=st_, cb=cb, ht=ht, wb=wb, db=db: e.matmul(out=pdn[:, db, :], lhsT=hTe[:, ht, st_ * 128:(st_ + 1) * 128], rhs=wd[:, wb, ht, cb * 512:(cb + 1) * 512], start=(ht == 0), stop=(ht == 3)),
                                     reads=[B_hTe, B_wd[wb]], writes=[B_pdn[db]])
                            if hh == 0:
                                P.op("act", lambda e, st_=st_, cb=cb, db=db: e.copy(out=osb[:, st_, cb * 512:(cb + 1) * 512], in_=pdn[:, db, :]), reads=[B_pdn[db]], writes=[B_osb])
                            else:
                                P.op("dve", lambda e, st_=st_, cb=cb, db=db: e.tensor_tensor(out=osb[:, st_, cb * 512:(cb + 1) * 512], in0=pdn[:, db, :], in1=osb[:, st_, cb * 512:(cb + 1) * 512], op=ALU.add), reads=[B_pdn[db], B_osb], writes=[B_osb])
                P.dma("sp", lambda e, e_=e_: e.dma_start(out=ybuf[e_ * CAP:(e_ + 1) * CAP, :].rearrange("(t p) d -> p t d", p=128), in_=osb[:]), reads=[B_osb])
            P.flush()

        with ExitStack() as es:
            y1 = es.enter_context(nc.sbuf_tensor("sb_y1", [128, 2, 2, D], F32))
            x5 = es.enter_context(nc.sbuf_tensor("sb_x5", [128, 2, D], F32))
            o5 = es.enter_context(nc.sbuf_tensor("sb_o5", [128, 2, D], F32))
            B_y1 = [Buf("y10"), Buf("y11")]
            B_x5 = [Buf("x50"), Buf("x51")]
            B_o5 = [Buf("o50"), Buf("o51")]
            for t in range(NT // 128):
                b = t % 2
                for k_ in range(2):
                    P.dma("pool", lambda e, t=t, k_=k_, b=b: e.indirect_dma_start(
                        out=y1[:, b, k_, :], out_offset=None, in_=ybuf[:, :],
                        in_offset=bass.IndirectOffsetOnAxis(ap=idx_t[:, t, k_:k_ + 1], axis=0)),
                        reads=[B_idx], writes=[B_y1[b]])
                P.dma("sp", lambda e, t=t, b=b: e.dma_start(out=x5[:, b, :], in_=x1s[t * 128:(t + 1) * 128, :]), writes=[B_x5[b]])
                P.op("dve", lambda e, t=t, b=b: e.scalar_tensor_tensor(out=o5[:, b, :], in0=y1[:, b, 0, :], scalar=wts_t[:, t, 0:1], in1=x5[:, b, :], op0=ALU.mult, op1=ALU.add),
                     reads=[B_y1[b], B_x5[b], B_idx], writes=[B_o5[b]])
                P.op("dve", lambda e, t=t, b=b: e.scalar_tensor_tensor(out=o5[:, b, :], in0=y1[:, b, 1, :], scalar=wts_t[:, t, 1:2], in1=o5[:, b, :], op0=ALU.mult, op1=ALU.add),
                     reads=[B_y1[b], B_o5[b], B_idx], writes=[B_o5[b]])
                P.dma("sp", lambda e, t=t, b=b: e.dma_start(out=y_out[t * 128:(t + 1) * 128, :], in_=o5[:, b, :]), reads=[B_o5[b]])
            P.flush()
    P.close()
    return nc


def _core_inputs(c, NU, HALO, x_prompt, x_sample, shared):
    xs = [x_prompt[c]]
    if NU == 3:
        s = c // 4
        s0 = (c % 4) * 4096
        xs.append(x_sample[s, s0:s0 + 4096])
    m = {"x_own": np.ascontiguousarray(np.concatenate(xs, 0))}
    if HALO:
        s = c // 4
        s0 = (c % 4) * 4096
        halo = np.zeros((2048, D), np.float32)
        if s0 > 0:
            halo[0:1024] = x_sample[s, s0 - 1024:s0]
        if s0 + 4096 < 16384:
            halo[1024:2048] = x_sample[s, s0 + 4096:s0 + 5120]
        m["x_halo"] = halo
    m["cst"] = _build_cst(c, NU, shared["rbias"], shared["gqk"])
    m["rope"] = _rope_tables(c, NU, HALO).reshape(-1, U)
    for k in ("g12", "w_in", "w_ab", "w_out", "wr", "rpb", "w_gate", "w_up", "w_down"):
        m[k] = shared[k]
    return m


def _shared_inputs(norm1_g, w_in, qn_a, kn_a, rpb_a, qn_b, kn_b, w_branch_a, w_branch_b, w_out, norm2_g,
                   router_group_w, router_group_b, router_expert_w, router_expert_b, w_gate, w_up, w_down):
    f = lambda a: np.ascontiguousarray(np.asarray(a, np.float32))
    return {
        "g12": f(np.stack([norm1_g[0], norm2_g[0]], 0)),
        "w_in": f(w_in[0]),
        "w_ab": f(np.concatenate([w_branch_a[0], w_branch_b[0]], 0)),
        "w_out": f(w_out[0]),
        "wr": f(np.concatenate([router_group_w[0], router_expert_w[0]], 1)),
        "rbias": f(np.concatenate([router_group_b[0], router_expert_b[0]], 0)),
        "gqk": f(np.stack([qn_a[0], kn_a[0], qn_b[0], kn_b[0]], 1)),
        "rpb": f(rpb_a[0].reshape(60, 31)),
        "w_gate": f(w_gate[0].reshape(NEXP * D, DEXP)),
        "w_up": f(w_up[0].reshape(NEXP * D, DEXP)),
        "w_down": f(w_down[0].reshape(NEXP * DEXP, D)),
    }


def kernel(x_prompt, x_sample, norm1_g, w_in, qn_a, kn_a, rpb_a, qn_b, kn_b, w_branch_a, w_branch_b, w_out,
           norm2_g, router_group_w, router_group_b, router_expert_w, router_expert_b, w_gate, w_up, w_down):
    x_prompt = np.asarray(x_prompt, np.float32)
    x_sample = np.asarray(x_sample, np.float32)
    shared = _shared_inputs(norm1_g, w_in, qn_a, kn_a, rpb_a, qn_b, kn_b, w_branch_a, w_branch_b, w_out, norm2_g,
                            router_group_w, router_group_b, router_expert_w, router_expert_b, w_gate, w_up, w_down)
    NU, CAP, HALO = 3, 512, True
    nc = build_program(NU, CAP, HALO)
    in_maps = [_core_inputs(c, NU, HALO, x_prompt, x_sample, shared) for c in range(8)]
    res = run_bass_kernel_spmd(nc, in_maps, core_ids=list(range(8)))
    y_prompt = np.empty((8, 2048, D), np.float32)
    y_sample = np.empty((2, 16384, D), np.float32)
    for c in range(8):
        y = res.results[c]["y"]
        y_prompt[c] = y[0:2048]
        s0 = (c % 4) * 4096
        y_sample[c // 4, s0:s0 + 4096] = y[2048:6144]
    return (y_prompt, y_sample)
```

```python
import math
from contextlib import ExitStack
import numpy as np
import concourse.bass as bass
import concourse.mybir as mybir
from concourse.bass_utils import run_bass_kernel_spmd

F32 = mybir.dt.float32
BF16 = mybir.dt.bfloat16
I32 = mybir.dt.int32
ALU = mybir.AluOpType
AF = mybir.ActivationFunctionType
AX = mybir.AxisListType

D = 2048
HD = 128
U = 2048
WIN = 4096
EPS = 1e-6
SCALE = HD ** -0.5
NEXP = 32
DEXP = 1024
DILS = (1, 4, 16)

CH = 16000
NDSEM = 24
GAP = 2


class Buf:
    __slots__ = ("name", "w", "r")

    def __init__(self, name):
        self.name = name
        self.w = None
        self.r = {}


class Prog:
    ENGS = ("pe", "act", "dve", "pool", "sp")

    def __init__(self, nc):
        self.nc = nc
        self.streams = {e: [] for e in self.ENGS}
        self.cnt = {e: 0 for e in self.ENGS}
        self.dcnt = {e: 0 for e in self.ENGS}
        self.waited = {e: {} for e in self.ENGS}
        self.semobjs = {}
        self.outstanding = []
        self.deferred = []
        self._cms = []

    def _sem(self, key):
        if key not in self.semobjs:
            cm = self.nc.semaphore("s_%s_%s_%d" % key)
            self._cms.append(cm)
            self.semobjs[key] = cm.__enter__()
        return self.semobjs[key]

    def _cref(self, eng, k):
        return (("c", eng, k // CH), k % CH + 1)

    def _add_wait(self, eng, waits, ref):
        key, val = ref
        if self.waited[eng].get(key, 0) >= val:
            return
        self.waited[eng][key] = val
        waits.append((key, val))

    def _deps(self, eng, reads, writes):
        refs = []
        for b in reads:
            if b.w is not None:
                refs.append(b.w)
        for b in writes:
            if b.w is not None:
                refs.append(b.w)
            refs.extend(b.r.items())
        return refs

    def _commit(self, ref, reads, writes):
        for b in reads:
            if b.r.get(ref[0], 0) < ref[1]:
                b.r[ref[0]] = ref[1]
        for b in writes:
            b.w = ref
            b.r = {}

    def op(self, eng, fn, reads=(), writes=()):
        waits = []
        k = self.cnt[eng]
        for ref in self._deps(eng, reads, writes):
            if ref[0][0] == "c" and ref[0][1] == eng:
                if eng == "pe":
                    continue
                if eng in ("dve", "act") and k - (ref[0][2] * CH + ref[1] - 1) >= GAP:
                    continue
            self._add_wait(eng, waits, ref)
        self.cnt[eng] += 1
        ref = self._cref(eng, k)
        self.streams[eng].append((waits, fn, (ref[0], 1)))
        self._commit(ref, reads, writes)
        return ref

    def dma(self, q, fn, reads=(), writes=(), defer=False):
        waits = []
        for ref in self._deps(q, reads, writes):
            self._add_wait(q, waits, ref)
        j = self.dcnt[q]
        self.dcnt[q] += 1
        key = ("d", q, j % NDSEM)
        prev = 16 * (j // NDSEM)
        if prev > 0:
            self._add_wait(q, waits, (key, prev))
        ref = (key, prev + 16)
        self.streams[q].append((waits, fn, (key, 16)))
        self._commit(ref, reads, writes)
        (self.deferred if defer else self.outstanding).append(ref)
        return ref

    def barrier(self, include_deferred=False):
        refs = []
        if include_deferred:
            refs.extend(self.deferred)
            self.deferred = []
        for e in self.ENGS:
            if self.cnt[e] > 0:
                refs.append(self._cref(e, self.cnt[e] - 1))
        refs.extend(self.outstanding)
        self.outstanding = []
        for e in self.ENGS:
            waits = []
            for ref in refs:
                self._add_wait(e, waits, ref)
            if waits:
                self.streams[e].append((waits, None, None))

    def flush(self, include_deferred=False):
        self.barrier(include_deferred)
        nc = self.nc
        for e in self.ENGS:
            for waits, fn, inc in self.streams[e]:
                for key, _ in waits:
                    self._sem(key)
                if inc is not None:
                    self._sem(inc[0])
        prog = self
        streams = self.streams
        self.streams = {e: [] for e in self.ENGS}

        def run(engname):
            def body(eng):
                for waits, fn, inc in streams[engname]:
                    for key, val in waits:
                        eng.wait_ge(prog.semobjs[key], val)
                    if fn is not None:
                        fn(eng).then_inc(prog.semobjs[inc[0]], inc[1])
            return body

        with nc.Block() as block:
            block.tensor(run("pe"))
            block.scalar(run("act"))
            block.vector(run("dve"))
            block.gpsimd(run("pool"))
            block.sync(run("sp"))

    def close(self):
        for cm in reversed(self._cms):
            cm.__exit__(None, None, None)


def _cst_layout(NU):
    off = {}
    cur = 0

    def add(name, n):
        nonlocal cur
        off[name] = (cur, n)
        cur += n
    add("ident", 128)
    add("ones", 128)
    add("onesm", 128)
    add("ustrict", 128)
    add("rm", 32)
    add("r2m", 64)
    add("ssel", 32)
    add("j2", 128)
    add("band4", 512)
    add("colvalid", 128)
    add("iota32", 32)
    add("iota8", 8)
    add("iota4", 4)
    add("rbias", 36)
    add("gqk", 4)
    add("negband", 256)
    add("rvint", 10)
    add("rv", NU * 16 * 14)
    add("kvb", NU * 3 * 8 * 4)
    return off, cur


def _unit_geom(c, u):
    if u == 0:
        return 2048, 0
    s0 = (c % 4) * 4096
    return 16384, s0 + (u - 1) * 2048


def _build_cst(c, NU, rbias, gqk):
    off, NC = _cst_layout(NU)
    cst = np.zeros((128, NC), np.float32)

    def put(name, arr):
        o, n = off[name]
        cst[:, o:o + n] = np.asarray(arr, np.float32).reshape(128, n)
    p = np.arange(128)
    put("ident", np.eye(128))
    put("ones", np.ones((128, 128)))
    put("onesm", np.full((128, 128), 1.0 / 128))
    put("ustrict", (p[:, None] < p[None, :]).astype(np.float32))
    rm = np.zeros((128, 32), np.float32)
    for m in range(16):
        rm[m + 16, m] = -1.0
        rm[m, m + 16] = 1.0
    put("rm", rm)
    r2m = np.zeros((128, 64), np.float32)
    for m in range(32):
        r2m[m, m] = 1.0
    r2m[:, 32:64] = rm
    put("r2m", r2m)
    ssel = np.zeros((128, 32), np.float32)
    for m in range(32):
        ssel[m, m] = 1.0
        ssel[32 + m, m] = 1.0
    put("ssel", ssel)
    j2 = np.zeros((128, 128), np.float32)
    for b in range(2):
        for q in range(64):
            j2[b * 64 + q, b * 64 + 63 - q] = 1.0
    put("j2", j2)
    bandA = (p[:, None] >= p[None, :]).astype(np.float32)
    bandB = (p[:, None] <= p[None, :]).astype(np.float32)
    put("band4", np.stack([bandA, bandB, bandA, bandB], axis=1))
    kc = np.arange(64)
    cs = np.clip(kc - 8, 0, 48)
    cv = ((kc[:, None] >= cs[None, :]) & (kc[:, None] < cs[None, :] + 16)).astype(np.float32)
    put("colvalid", np.tile(cv, (2, 2)))
    put("iota32", np.tile(np.arange(32, dtype=np.float32), (128, 1)))
    put("iota8", np.tile(np.arange(8, dtype=np.float32), (128, 1)))
    put("iota4", np.tile(np.arange(4, dtype=np.float32), (128, 1)))
    put("rbias", np.tile(rbias.reshape(1, 36), (128, 1)))
    put("gqk", gqk)
    rv = np.zeros((128, NU, 16, 7, 2), np.float32)
    kvd = np.zeros((128, NU, 3, 8, 4), np.float32)
    a = p // 64
    for u in range(NU):
        T, st = _unit_geom(c, u)
        rows = T // 64
        R0 = st // 64
        for n in range(16):
            for di in range(7):
                kr = R0 + 2 * (n + di - 3) + a
                for b in range(2):
                    qr = R0 + 2 * n + b
                    rs = min(max(qr - 4, 0), rows - 8)
                    rv[:, u, n, di, b] = ((kr >= rs) & (kr < rs + 8) & (kr >= 0) & (kr < rows)).astype(np.float32)
        for g, d in enumerate(DILS):
            nb = 16 // d
            blocks = [(r, n) for r in range(d) for n in range(nb)]
            for pi in range(8):
                for bi in range(2):
                    r, n = blocks[2 * pi + bi]
                    for ab in range(2):
                        m = n + ab
                        wp = 1024 + (m * 128 - 64 + p) * d + r
                        t = st - 1024 + wp
                        kvd[:, u, g, pi, bi * 2 + ab] = ((t >= 0) & (t < T)).astype(np.float32)
    put("rv", rv)
    put("kvb", (kvd - 1.0) * 30000.0)
    put("negband", (np.stack([bandA, bandB], axis=1) - 1.0) * 30000.0)
    rvint = np.zeros((128, 5, 2), np.float32)
    for di in range(5):
        for b in range(2):
            dd = 2 * (di - 2) + a - b
            rvint[:, di, b] = ((dd >= -4) & (dd <= 3)).astype(np.float32)
    put("rvint", rvint)
    return cst


def _rope_tables(c, NU, HALO):
    inv = 1.0 / (500000.0 ** (np.arange(16, dtype=np.float64) * (2.0 / 32)))
    tabs = []
    poss = []
    for u in range(NU):
        T, st = _unit_geom(c, u)
        poss.append(st + np.arange(U, dtype=np.float64))
    if HALO:
        s0 = (c % 4) * 4096
        poss.append(np.concatenate([s0 - 1024 + np.arange(1024.0), s0 + 4096 + np.arange(1024.0)]))
    for pos in poss:
        ang = np.float32(pos)[None, :].astype(np.float32) * inv.astype(np.float32)[:, None]
        cs_ = np.cos(ang.astype(np.float64))
        sn_ = np.sin(ang.astype(np.float64))
        tab = np.concatenate([cs_, cs_, sn_, sn_], 0)
        tabs.append(tab.astype(np.float32))
    return np.stack(tabs, 0)


def build_program(NU, CAP, HALO, debug=False):
    nc = bass.Bass("TRN2", target_bir_lowering=False)
    NT = NU * U
    NST = NU + (1 if HALO else 0)
    coff, NC = _cst_layout(NU)

    def dram_in(name, shape, dt=F32):
        return nc.dram_tensor(name, list(shape), dt, kind="ExternalInput").ap()

    x_own = dram_in("x_own", [NT, D])
    x_halo = dram_in("x_halo", [U, D]) if HALO else None
    cst_d = dram_in("cst", [128, NC])
    rope_d = dram_in("rope", [NST * 64, U])
    g12_d = dram_in("g12", [2, D])
    w_in = dram_in("w_in", [D, 10240])
    w_ab = dram_in("w_ab", [1024, D])
    w_out = dram_in("w_out", [D, D])
    wr_d = dram_in("wr", [D, 36])
    rpb_d = dram_in("rpb", [60, 31])
    w_gate = dram_in("w_gate", [NEXP * D, DEXP])
    w_up = dram_in("w_up", [NEXP * D, DEXP])
    w_down = dram_in("w_down", [NEXP * DEXP, D])
    y_out = nc.dram_tensor("y", [NT, D], F32, kind="ExternalOutput").ap()

    def scratch(name, shape, dt):
        kind = "ExternalOutput" if (debug and name in ("qT", "kTw", "vw", "gT", "oT", "x1s", "dbg")) else "Internal"
        return nc.dram_tensor(name, list(shape), dt, kind=kind).ap()

    qT = scratch("qT", [16 * 128, NT], BF16)
    kTw = scratch("kTw", [NU * 16 * 128, WIN], BF16)
    vw = scratch("vw", [NU * WIN, D], BF16)
    gT = scratch("gT", [32 * 128, NT], BF16)
    oT = scratch("oT", [8 * 128, NT], BF16)
    x1s = scratch("x1s", [NT, D], F32)
    xbuf = scratch("xbuf", [NEXP * CAP + 128, D], BF16)
    ybuf = scratch("ybuf", [NEXP * CAP + 128, D], F32)
    rpbp = scratch("rpbp", [60, 128], F32)
    dbg = scratch("dbg", [128, 64], F32) if debug else None

    P = Prog(nc)

    def cs(t, name):
        o, n = coff[name]
        return t[:, o:o + n]

    with ExitStack() as es:
        cst = es.enter_context(nc.sbuf_tensor("sb_cst", [128, NC], F32))
        identb = es.enter_context(nc.sbuf_tensor("sb_identb", [128, 128], BF16))
        onesb = es.enter_context(nc.sbuf_tensor("sb_onesb", [128, 128], BF16))
        onesmb = es.enter_context(nc.sbuf_tensor("sb_onesmb", [128, 128], BF16))
        ustrb = es.enter_context(nc.sbuf_tensor("sb_ustrb", [128, 128], BF16))
        rmb = es.enter_context(nc.sbuf_tensor("sb_rmb", [128, 32], BF16))
        r2mb = es.enter_context(nc.sbuf_tensor("sb_r2mb", [128, 64], BF16))
        sselb = es.enter_context(nc.sbuf_tensor("sb_sselb", [128, 32], BF16))
        idx_t = es.enter_context(nc.sbuf_tensor("sb_idx", [128, NT // 128, 2], I32))
        wts_t = es.enter_context(nc.sbuf_tensor("sb_wts", [128, NT // 128, 2], F32))
        B_cst = Buf("cst")
        B_cb = Buf("cstb")
        B_g = Buf("g12")
        B_idx = Buf("idx")
        B_zt = Buf("zt")
        identf = cs(cst, "ident")
        gqk = cs(cst, "gqk")

        P.dma("sp", lambda e: e.dma_start(out=cst[:], in_=cst_d[:, :]), writes=[B_cst])
        for dst, nm in ((identb, "ident"), (onesb, "ones"), (onesmb, "onesm"), (ustrb, "ustrict"), (rmb, "rm"), (r2mb, "r2m"), (sselb, "ssel")):
            P.op("dve", lambda e, dst=dst, nm=nm: e.tensor_copy(out=dst[:], in_=cs(cst, nm)), reads=[B_cst], writes=[B_cb])
        es_z = ExitStack()
        zt = es_z.enter_context(nc.sbuf_tensor("sb_zt", [128, 1, D], BF16))
        zf = es_z.enter_context(nc.sbuf_tensor("sb_zf", [128, 128], F32))
        negb = es_z.enter_context(nc.sbuf_tensor("sb_negb", [128, 2, 128], BF16))
        P.op("pool", lambda e: e.memset(zt[:], 0.0), writes=[B_zt])
        P.op("pool", lambda e: e.memset(zf[:], 0.0), writes=[B_zt])
        P.op("dve", lambda e: e.tensor_copy(out=negb[:].rearrange("p a b -> p (a b)"), in_=cs(cst, "negband")), reads=[B_cst], writes=[B_cb])
        nz = (NEXP * CAP + 128)
        r0 = 0
        while r0 < nz:
            nr = min(1024, nz - r0)
            P.dma("act", lambda e, r0=r0, nr=nr: e.dma_start(
                out=xbuf[r0:r0 + nr, :].rearrange("(t p) d -> p t d", p=128), in_=zt[:, 0:1, :].broadcast_to([128, nr // 128, D])), reads=[B_zt], defer=True)
            r0 += nr
        for (a0, a1) in ((0, 1024), (3072, 4096)):
            P.dma("sp", lambda e, a0=a0: e.dma_start(
                out=vw[a0:a0 + 1024, :].rearrange("(t p) d -> p t d", p=128), in_=zt[:, 0:1, :].broadcast_to([128, 8, D])), reads=[B_zt])
            for h in range(16):
                P.dma("sp", lambda e, a0=a0, h=h: e.dma_start(
                    out=kTw[h * 128:(h + 1) * 128, a0:a0 + 1024], in_=zt[:, 0, 0:1024]), reads=[B_zt])
        P.dma("sp", lambda e: e.dma_start(out=rpbp[0:60, :], in_=zf[0:60, :]), reads=[B_zt])
        P.flush()
        P.dma("sp", lambda e: e.dma_start(out=rpbp[0:60, 48:79], in_=rpb_d[:, :]))
        P.flush()

        with ExitStack() as es:
            g1bc = es.enter_context(nc.sbuf_tensor("sb_g1bc", [128, D], F32))
            xt = es.enter_context(nc.sbuf_tensor("sb_xt", [128, 3, D], F32))
            sqj = es.enter_context(nc.sbuf_tensor("sb_sqj", [128, D], BF16))
            hb = es.enter_context(nc.sbuf_tensor("sb_hb", [128, 3, D], BF16))
            st1 = es.enter_context(nc.sbuf_tensor("sb_st1", [128, 3, 4], F32))
            hT = es.enter_context(nc.sbuf_tensor("sb_hT", [128, 16, U], BF16))
            wt = es.enter_context(nc.sbuf_tensor("sb_wt", [128, 2, 16, 512], BF16))
            ropet = es.enter_context(nc.sbuf_tensor("sb_ropet", [64, U], F32))
            qs = es.enter_context(nc.sbuf_tensor("sb_qs", [128, 2, 512], F32))
            sq = es.enter_context(nc.sbuf_tensor("sb_sq", [128, 2, 512], BF16))
            rstd = es.enter_context(nc.sbuf_tensor("sb_rstd", [128, 2, 512], F32))
            qn = es.enter_context(nc.sbuf_tensor("sb_qn", [128, 4, 512], BF16))
            rt = es.enter_context(nc.sbuf_tensor("sb_rt", [64, 2, 512], BF16))
            vs = es.enter_context(nc.sbuf_tensor("sb_vs", [128, 2, 512], BF16))
            ptr = es.enter_context(nc.psum_tensor("ps_ptr", [128, 2, 8, 128], BF16))
            pacc = es.enter_context(nc.psum_tensor("ps_pacc", [128, 2, 512], F32))
            pm = es.enter_context(nc.psum_tensor("ps_pm", [128, 512], F32))
            prr = es.enter_context(nc.psum_tensor("ps_prr", [128, 2, 512], F32))
            B_xt = [Buf("xt0"), Buf("xt1"), Buf("xt2")]
            B_hb = [Buf("hb0"), Buf("hb1"), Buf("hb2")]
            B_st = [Buf("st0"), Buf("st1"), Buf("st2")]
            B_sqj = Buf("sqj")
            B_hT = [Buf("hT%d" % i) for i in range(16)]
            B_wt = [Buf("wt0"), Buf("wt1")]
            B_rope = Buf("rope")
            B_qs = [Buf("qs0"), Buf("qs1")]
            B_sq = [Buf("sq0"), Buf("sq1")]
            B_rstd = [Buf("rstd0"), Buf("rstd1")]
            B_qn = [Buf("qn0"), Buf("qn1"), Buf("qn2"), Buf("qn3")]
            B_rt = [Buf("rt0"), Buf("rt1")]
            B_vs = [Buf("vs0"), Buf("vs1")]
            B_ptr = [Buf("ptr0"), Buf("ptr1")]
            B_pacc = [Buf("pacc0"), Buf("pacc1")]
            B_pm = Buf("pm")
            B_prr = [Buf("prr0"), Buf("prr1")]
            cnt = {"x": 0, "tr": 0, "w": 0, "acc": 0, "q": 0, "v": 0}
            pend = []

            def pipe_push(main_fn, stages):
                main_fn()
                for st_ in pend:
                    if st_:
                        st_.pop(0)()
                pend.insert(0, [f for f in stages if f is not None])
                while pend and not pend[-1]:
                    pend.pop()

            def pipe_drain():
                while pend:
                    for st_ in pend:
                        if st_:
                            st_.pop(0)()
                    while pend and not pend[-1]:
                        pend.pop()
                    if pend and not any(pend):
                        pend.clear()
            P.dma("sp", lambda e: e.dma_start(out=g1bc[:], in_=g12_d[0, :].partition_broadcast(128)), writes=[B_g])

            def kv_dests(sti, ck):
                if sti < NU:
                    u = sti
                    d = [(u, 1024 + 512 * ck)]
                    if NU == 3 and u == 1 and ck >= 2:
                        d.append((2, 512 * (ck - 2)))
                    if NU == 3 and u == 2 and ck < 2:
                        d.append((1, 3072 + 512 * ck))
                    return d
                return [(1, 512 * ck)] if ck < 2 else [(2, 3072 + 512 * (ck - 2))]

            for sti in range(NST):
                halo = sti >= NU
                xsrc = x_halo if halo else x_own[sti * U:(sti + 1) * U, :]
                P.dma("sp", lambda e, sti=sti: e.dma_start(out=ropet[:], in_=rope_d[sti * 64:(sti + 1) * 64, :]), writes=[B_rope])
                for t in range(16):
                    xb = cnt["x"] % 3
                    cnt["x"] += 1
                    P.dma("sp", lambda e, t=t, xb=xb, xsrc=xsrc: e.dma_start(out=xt[:, xb, :], in_=xsrc[t * 128:(t + 1) * 128, :]), writes=[B_xt[xb]])
                    P.op("dve", lambda e, xb=xb: e.memset(st1[:, xb, 0:1], 0.0), writes=[B_st[xb]])
                    P.op("act", lambda e, xb=xb: e.activation(out=sqj[:], in_=xt[:, xb, :], func=AF.Square, accum_out=st1[:, xb, 0:1]),
                         reads=[B_xt[xb]], writes=[B_sqj, B_st[xb]])
                    P.op("act", lambda e, xb=xb: e.activation(out=st1[:, xb, 1:2], in_=st1[:, xb, 0:1], func=AF.Sqrt, bias=EPS, scale=1.0 / D),
                         reads=[B_st[xb]], writes=[B_st[xb]])
                    P.op("dve", lambda e, xb=xb: e.reciprocal(out=st1[:, xb, 2:3], in_=st1[:, xb, 1:2]),
                         reads=[B_st[xb]], writes=[B_st[xb]])
                    P.op("dve", lambda e, xb=xb: e.scalar_tensor_tensor(out=hb[:, xb, :], in0=xt[:, xb, :], scalar=st1[:, xb, 2:3], in1=g1bc[:], op0=ALU.mult, op1=ALU.mult),
                         reads=[B_xt[xb], B_st[xb], B_g], writes=[B_hb[xb]])
                    for half in range(2):
                        pb = cnt["tr"] % 2
                        cnt["tr"] += 1
                        for j in range(8):
                            c = half * 8 + j
                            P.op("pe", lambda e, xb=xb, c=c, j=j, pb=pb: e.transpose(out=ptr[:, pb, j, :], in_=hb[:, xb, c * 128:(c + 1) * 128], identity=identb[:]),
                                 reads=[B_hb[xb], B_cb], writes=[B_ptr[pb]])
                        eng = "act" if half == 0 else "dve"
                        if eng == "act":
                            P.op("act", lambda e, t=t, half=half, pb=pb: e.copy(out=hT[:, half * 8:(half + 1) * 8, t * 128:(t + 1) * 128], in_=ptr[:, pb, :, :]),
                                 reads=[B_ptr[pb]], writes=[B_hT[t]])
                        else:
                            P.op("dve", lambda e, t=t, half=half, pb=pb: e.tensor_copy(out=hT[:, half * 8:(half + 1) * 8, t * 128:(t + 1) * 128], in_=ptr[:, pb, :, :]),
                                 reads=[B_ptr[pb]], writes=[B_hT[t]])
                for cg in range(20):
                    kind = "q" if cg in (0, 3, 4, 5) else "k" if cg in (1, 6, 7, 8) else "v" if cg in (2, 9, 10, 11) else "g"
                    if halo and kind not in ("k", "v"):
                        continue
                    wb = cnt["w"] % 2
                    cnt["w"] += 1
                    P.dma("pool", lambda e, cg=cg, wb=wb: e.dma_start(out=wt[:, wb, :, :], in_=w_in[:, cg * 512:(cg + 1) * 512].rearrange("(c p) n -> p c n", p=128)),
                          writes=[B_wt[wb]])
                    if kind == "v":
                        pipe_drain()
                        hv0 = 0 if cg == 2 else 4 + (cg - 9) * 4
                        for t in range(16):
                            ab = cnt["acc"] % 2
                            cnt["acc"] += 1
                            for c in range(16):
                                P.op("pe", lambda e, t=t, c=c, wb=wb, ab=ab: e.matmul(out=pacc[:, ab, :], lhsT=hT[:, c, t * 128:(t + 1) * 128], rhs=wt[:, wb, c, :], start=(c == 0), stop=(c == 15)),
                                     reads=[B_hT[t], B_wt[wb]], writes=[B_pacc[ab]])
                            vb = cnt["v"] % 2
                            cnt["v"] += 1
                            if t % 2 == 0:
                                P.op("act", lambda e, ab=ab, vb=vb: e.copy(out=vs[:, vb, :], in_=pacc[:, ab, :]), reads=[B_pacc[ab]], writes=[B_vs[vb]])
                            else:
                                P.op("dve", lambda e, ab=ab, vb=vb: e.tensor_copy(out=vs[:, vb, :], in_=pacc[:, ab, :]), reads=[B_pacc[ab]], writes=[B_vs[vb]])
                            ck = t // 4
                            for (uu, wo) in kv_dests(sti, ck):
                                row = uu * WIN + wo + (t % 4) * 128
                                P.dma("sp", lambda e, row=row, hv0=hv0, vb=vb: e.dma_start(out=vw[row:row + 128, hv0 * 128:hv0 * 128 + 512], in_=vs[:, vb, :]), reads=[B_vs[vb]])
                        continue
                    for ct in range(4):
                        for ck in range(4):
                            ab = cnt["acc"] % 2
                            cnt["acc"] += 1
                            qb = cnt["q"] % 2
                            q3 = cnt["q"] % 4
                            cnt["q"] += 1

                            def main_fn(ct=ct, ck=ck, wb=wb, ab=ab):
                                for c in range(16):
                                    P.op("pe", lambda e, c=c: e.matmul(out=pacc[:, ab, :], lhsT=wt[:, wb, c, ct * 128:(ct + 1) * 128], rhs=hT[:, c, ck * 512:(ck + 1) * 512], start=(c == 0), stop=(c == 15)),
                                         reads=B_hT[ck * 4:ck * 4 + 4] + [B_wt[wb]], writes=[B_pacc[ab]])

                            if kind == "g":
                                gi = (cg - 12) * 4 + ct

                                def a_fn(ab=ab, q3=q3, gi=gi, sti=sti, ck=ck):
                                    P.op("act", lambda e: e.activation(out=qn[:, q3, :], in_=pacc[:, ab, :], func=AF.Sigmoid), reads=[B_pacc[ab]], writes=[B_qn[q3]])
                                    P.dma("sp", lambda e: e.dma_start(out=gT[gi * 128:(gi + 1) * 128, sti * U + ck * 512: sti * U + (ck + 1) * 512], in_=qn[:, q3, :]), reads=[B_qn[q3]])

                                pipe_push(main_fn, [a_fn])
                                continue
                            if cg <= 1:
                                head = ct
                                gcol = cg
                                rope = False
                            else:
                                head = 4 + ((cg - 3) if kind == "q" else (cg - 6)) * 4 + ct
                                gcol = 2 if kind == "q" else 3
                                rope = True

                            def a_fn(ab=ab, qb=qb, q3=q3, gcol=gcol):
                                P.op("act", lambda e: e.activation(out=qs[:, qb, :], in_=pacc[:, ab, :], func=AF.Copy, scale=gqk[:, gcol:gcol + 1]), reads=[B_pacc[ab], B_cst], writes=[B_qs[qb]])
                                P.op("act", lambda e: e.activation(out=sq[:, qb, :], in_=pacc[:, ab, :], func=AF.Square), reads=[B_pacc[ab]], writes=[B_sq[qb]])
                                P.op("pe", lambda e: e.matmul(out=pm[:], lhsT=onesmb[:], rhs=sq[:, qb, :], start=True, stop=True), reads=[B_sq[qb], B_cb], writes=[B_pm])
                                P.op("act", lambda e: e.activation(out=rstd[:, qb, :], in_=pm[:], func=AF.Ln, bias=EPS, scale=1.0), reads=[B_pm], writes=[B_rstd[qb]])
                                P.op("act", lambda e: e.activation(out=rstd[:, qb, :], in_=rstd[:, qb, :], func=AF.Exp, scale=-0.5), reads=[B_rstd[qb]], writes=[B_rstd[qb]])
                                P.op("dve", lambda e: e.tensor_tensor(out=qn[:, q3, :], in0=qs[:, qb, :], in1=rstd[:, qb, :], op=ALU.mult),
                                     reads=[B_qs[qb], B_rstd[qb]], writes=[B_qn[q3]])

                            def b_fn(qb=qb, q3=q3, ck=ck):
                                P.op("pe", lambda e: e.matmul(out=prr[0:64, 0, :], lhsT=r2mb[:], rhs=qn[:, q3, :], start=True, stop=True), reads=[B_qn[q3], B_cb], writes=[B_prr[0]])
                                P.op("dve", lambda e: e.tensor_tensor(out=rt[:, qb, :], in0=prr[0:64, 0, :], in1=ropet[:, ck * 512:(ck + 1) * 512], op=ALU.mult),
                                     reads=[B_prr[0], B_rope], writes=[B_rt[qb]])

                            def c_fn(qb=qb, q3=q3, rope=rope, kind=kind, head=head, sti=sti, ck=ck):
                                if rope:
                                    P.op("pe", lambda e: e.matmul(out=prr[0:32, 1, :], lhsT=sselb[0:64, :], rhs=rt[:, qb, :], start=True, stop=True), reads=[B_rt[qb], B_cb], writes=[B_prr[1]])
                                    P.op("act", lambda e: e.copy(out=qn[0:32, q3, :], in_=prr[0:32, 1, :]), reads=[B_prr[1]], writes=[B_qn[q3]])
                                if kind == "q":
                                    P.dma("sp", lambda e: e.dma_start(out=qT[head * 128:(head + 1) * 128, sti * U + ck * 512: sti * U + (ck + 1) * 512], in_=qn[:, q3, :]), reads=[B_qn[q3]])
                                else:
                                    for (uu, wo) in kv_dests(sti, ck):
                                        r_ = (uu * 16 + head) * 128
                                        P.dma("sp", lambda e, r_=r_, wo=wo: e.dma_start(out=kTw[r_:r_ + 128, wo:wo + 512], in_=qn[:, q3, :]), reads=[B_qn[q3]])

                            pipe_push(main_fn, [a_fn, b_fn if rope else None, c_fn])
                pipe_drain()
            P.flush()

        with ExitStack() as es:
            ebt = es.enter_context(nc.sbuf_tensor("sb_ebt", [128, 4, 7, 128], F32))
            ebt2 = es.enter_context(nc.sbuf_tensor("sb_ebt2", [128, 4, 5, 128], F32))
            hbk = es.enter_context(nc.sbuf_tensor("sb_hbk", [128, 4, 128], F32))
            ebtmp = es.enter_context(nc.sbuf_tensor("sb_ebtmp", [128, 4, 128], F32))
            kT = es.enter_context(nc.sbuf_tensor("sb_kT", [128, 2, WIN], BF16))
            qTt = es.enter_context(nc.sbuf_tensor("sb_qTt", [128, 2, U], BF16))
            vt = es.enter_context(nc.sbuf_tensor("sb_vt", [128, 2, 32, 128], BF16))
            eS = es.enter_context(nc.sbuf_tensor("sb_eS", [128, 2, 1024], F32))
            eS2 = es.enter_context(nc.sbuf_tensor("sb_eS2", [128, 2, 1024], F32))
            pT = es.enter_context(nc.sbuf_tensor("sb_pT", [128, 2, 1024], BF16))
            acc = es.enter_context(nc.sbuf_tensor("sb_acc", [128, 2, U], F32))
            rD = es.enter_context(nc.sbuf_tensor("sb_rD", [128, U], F32))
            oTt = es.enter_context(nc.sbuf_tensor("sb_oTt", [128, 8, U], BF16))
            psS = es.enter_context(nc.psum_tensor("ps_psS", [128, 2, 1024], F32))
            psO = es.enter_context(nc.psum_tensor("ps_psO", [128, 2, 512], F32))
            psE = es.enter_context(nc.psum_tensor("ps_psE", [128, 512], F32))
            B_ebt = Buf("ebt")
            B_hbk = [Buf("hbk%d" % i) for i in range(4)]
            B_ebtmp = [Buf("ebtmp%d" % i) for i in range(4)]
            B_psE = [Buf("psE%d" % i) for i in range(4)]
            ei = 0
            for h in range(4):
                for di in range(7):
                    delta = di - 3
                    k4 = ei % 4
                    ei += 1
                    for bp in range(2):
                        dr = 2 * delta - bp + 7
                        src = bass.AP(tensor=rpbp.tensor, offset=(h * 15 + dr) * 128, ap=[[1, 64], [128, 2], [1, 64]])
                        P.dma("sp", lambda e, src=src, bp=bp, k4=k4: e.dma_start(out=hbk[bp * 64:(bp + 1) * 64, k4, :].rearrange("p (a c) -> p a c", c=64), in_=src), writes=[B_hbk[k4]])
                    P.op("pe", lambda e, k4=k4: e.matmul(out=psE[:, k4 * 128:(k4 + 1) * 128], lhsT=hbk[:, k4, :], rhs=cs(cst, "j2"), start=True, stop=True), reads=[B_hbk[k4], B_cst], writes=[B_psE[k4]])
                    P.op("act", lambda e, k4=k4: e.activation(out=ebtmp[:, k4, :], in_=psE[:, k4 * 128:(k4 + 1) * 128], func=AF.Exp), reads=[B_psE[k4]], writes=[B_ebtmp[k4]])
                    P.op("dve", lambda e, h=h, di=di, k4=k4: e.tensor_tensor(out=ebt[:, h, di, :], in0=ebtmp[:, k4, :], in1=cs(cst, "colvalid"), op=ALU.mult), reads=[B_ebtmp[k4], B_cst], writes=[B_ebt])
            rio = coff["rvint"][0]
            for h in range(4):
                P.op("dve", lambda e, h=h: e.tensor_tensor(out=ebt2[:, h, :, :].rearrange("p a (b c) -> p (a b) c", c=64), in0=ebt[:, h, 1:6, :].rearrange("p a (b c) -> p (a b) c", c=64),
                                                          in1=cst[:, rio:rio + 10].unsqueeze(2).broadcast_to([128, 10, 64]), op=ALU.mult), reads=[B_ebt, B_cst], writes=[B_ebt])

            B_kT = [Buf("kT0"), Buf("kT1")]
            B_qT = [Buf("qT0"), Buf("qT1")]
            B_vt = [Buf("vt0"), Buf("vt1")]
            B_psQ = [Buf("psQ%d" % i) for i in range(4)]
            B_pTQ = [Buf("pTQ%d" % i) for i in range(4)]
            B_psS = [[B_psQ[0], B_psQ[1]], [B_psQ[2], B_psQ[3]]]
            B_psO = [Buf("psO0"), Buf("psO1")]
            B_eS = [Buf("eS0"), Buf("eS1")]
            B_eS2 = [Buf("eS20"), Buf("eS21")]
            B_pT = [[B_pTQ[0], B_pTQ[1]], [B_pTQ[2], B_pTQ[3]]]
            B_oTn = [[Buf("oT%d_%d" % (j, n)) for n in range(16)] for j in range(8)]
            ac = {"h": 0, "s": 0, "o": 0, "q4": 0}
            rvo = coff["rv"][0]
            kbo = coff["kvb"][0]
            pend2 = []

            def push2(a_fn, b_fn, depth=1):
                a_fn()
                pend2.append(b_fn)
                while len(pend2) > depth:
                    pend2.pop(0)()

            def drain2():
                while pend2:
                    pend2.pop(0)()

            for u in range(NU):
                for h in range(4):
                    hbf = ac["h"] % 2
                    ac["h"] += 1
                    krow = (u * 16 + h) * 128
                    P.dma("sp", lambda e, krow=krow, hbf=hbf: e.dma_start(out=kT[:, hbf, 640:3456], in_=kTw[krow:krow + 128, 640:3456]), writes=[B_kT[hbf]])
                    P.dma("sp", lambda e, h=h, u=u, hbf=hbf: e.dma_start(out=qTt[:, hbf, :], in_=qT[h * 128:(h + 1) * 128, u * U:(u + 1) * U]), writes=[B_qT[hbf]])
                    P.dma("sp", lambda e, h=h, u=u, hbf=hbf: e.dma_start(out=vt[:, hbf, 0:22, :], in_=vw[u * WIN + 640:u * WIN + 3456, h * 128:(h + 1) * 128].rearrange("(t p) d -> p t d", p=128)), writes=[B_vt[hbf]])
                    for n in range(16):
                        sb = ac["s"] % 2
                        ac["s"] += 1
                        ob = ac["o"] % 2
                        ac["o"] += 1

                        interior = 2 <= n <= 13
                        dis = list(range(1, 6)) if interior else list(range(7))
                        nd = len(dis)

                        def a_fn(u=u, h=h, n=n, hbf=hbf, sb=sb, interior=interior, dis=dis, nd=nd):
                            for j, di in enumerate(dis):
                                m = n + di
                                P.op("pe", lambda e, m=m, j=j: e.matmul(out=psS[:, sb, j * 128:(j + 1) * 128], lhsT=kT[:, hbf, 640 + m * 128:640 + (m + 1) * 128], rhs=qTt[:, hbf, n * 128:(n + 1) * 128], start=True, stop=True),
                                     reads=[B_kT[hbf], B_qT[hbf]], writes=B_psS[sb])
                            P.op("act", lambda e: e.activation(out=eS[:, sb, 0:512], in_=psS[:, sb, 0:512], func=AF.Exp, scale=SCALE), reads=B_psS[sb], writes=[B_eS[sb]])
                            P.op("act", lambda e: e.activation(out=eS[:, sb, 512:nd * 128], in_=psS[:, sb, 512:nd * 128], func=AF.Exp, scale=SCALE), reads=B_psS[sb], writes=[B_eS[sb]])
                            if interior:
                                P.op("dve", lambda e: e.tensor_tensor(out=pT[:, sb, 0:640], in0=eS[:, sb, 0:640], in1=ebt2[:, h, :, :].rearrange("p a b -> p (a b)"), op=ALU.mult),
                                     reads=[B_eS[sb], B_ebt], writes=B_pT[sb])
                                return
                            P.op("dve", lambda e: e.tensor_tensor(out=eS2[:, sb, 0:896], in0=eS[:, sb, 0:896], in1=ebt[:, h, :, :].rearrange("p a b -> p (a b)"), op=ALU.mult),
                                 reads=[B_eS[sb], B_ebt], writes=[B_eS2[sb]])
                            ro = rvo + (u * 16 + n) * 14
                            P.op("pool", lambda e: e.tensor_tensor(out=pT[:, sb, 0:896].rearrange("p (a b) -> p a b", b=64), in0=eS2[:, sb, 0:896].rearrange("p (a b) -> p a b", b=64),
                                                                  in1=cst[:, ro:ro + 14].unsqueeze(2).broadcast_to([128, 14, 64]), op=ALU.mult),
                                 reads=[B_eS2[sb], B_cst], writes=B_pT[sb])

                        def b_fn(h=h, n=n, hbf=hbf, sb=sb, ob=ob, dis=dis, nd=nd):
                            for j, di in enumerate(dis):
                                m = n + di
                                P.op("pe", lambda e, m=m, j=j: e.matmul(out=psO[:, ob, 0:128], lhsT=vt[:, hbf, m, :], rhs=pT[:, sb, j * 128:(j + 1) * 128], start=(j == 0), stop=(j == nd - 1)),
                                     reads=[B_vt[hbf]] + B_pT[sb], writes=[B_psO[ob]])
                            for j in range(nd):
                                P.op("pe", lambda e, j=j: e.matmul(out=psO[:, ob, 128:256], lhsT=onesb[:], rhs=pT[:, sb, j * 128:(j + 1) * 128], start=(j == 0), stop=(j == nd - 1)),
                                     reads=[B_cb] + B_pT[sb], writes=[B_psO[ob]])
                            B_r = Buf("rDn")
                            P.op("dve", lambda e: e.reciprocal(out=rD[:, n * 128:(n + 1) * 128], in_=psO[:, ob, 128:256]), reads=[B_psO[ob]], writes=[B_r])
                            P.op("dve", lambda e: e.tensor_tensor(out=oTt[:, h, n * 128:(n + 1) * 128], in0=psO[:, ob, 0:128], in1=rD[:, n * 128:(n + 1) * 128], op=ALU.mult),
                                 reads=[B_psO[ob], B_r], writes=[B_oTn[h][n]])

                        push2(a_fn, b_fn)
                for hs in range(4):
                    B_accb = [[Buf("acc%d_%d" % (g, i)) for i in range(16)] for g in range(3)]
                    for g, d in enumerate(DILS):
                        head = 4 + 4 * g + hs
                        nb = 16 // d
                        hbf = ac["h"] % 2
                        ac["h"] += 1
                        krow = (u * 16 + head) * 128
                        P.dma("sp", lambda e, krow=krow, hbf=hbf: e.dma_start(out=kT[:, hbf, :], in_=kTw[krow:krow + 128, :]), writes=[B_kT[hbf]])
                        P.dma("sp", lambda e, head=head, u=u, hbf=hbf: e.dma_start(out=qTt[:, hbf, :], in_=qT[head * 128:(head + 1) * 128, u * U:(u + 1) * U]), writes=[B_qT[hbf]])
                        for r in range(d):
                            base = (u * WIN + 1024 - 64 * d + r) * D + head * 128
                            src = bass.AP(tensor=vw.tensor, offset=base, ap=[[d * D, 128], [128 * d * D, nb + 1], [1, 128]])
                            P.dma("sp", lambda e, src=src, r=r, nb=nb, hbf=hbf: e.dma_start(out=vt[:, hbf, r * (nb + 1):(r + 1) * (nb + 1), :], in_=src), writes=[B_vt[hbf]])
                        blocks = [(r, n) for r in range(d) for n in range(nb)]
                        for pi in range(8):
                            k4 = ac["q4"] % 4
                            ac["q4"] += 1
                            ob = ac["o"] % 2
                            ac["o"] += 1
                            sbh, so = k4 // 2, (k4 % 2) * 512

                            def a_fn(u=u, g=g, d=d, pi=pi, hbf=hbf, k4=k4, sbh=sbh, so=so, blocks=blocks):
                                for bi in range(2):
                                    r, n = blocks[2 * pi + bi]
                                    q0 = n * 128 * d + r
                                    for ab in range(2):
                                        m = n + ab
                                        k0 = 1024 + (m * 128 - 64) * d + r
                                        sub = bi * 2 + ab
                                        P.op("pe", lambda e, k0=k0, q0=q0, sub=sub: e.matmul(
                                            out=psS[:, sbh, so + sub * 128:so + (sub + 1) * 128],
                                            lhsT=kT[:, hbf, k0:k0 + 127 * d + 1:d], rhs=qTt[:, hbf, q0:q0 + 127 * d + 1:d], start=True, stop=False),
                                            reads=[B_kT[hbf], B_qT[hbf]], writes=[B_psQ[k4]])
                                        P.op("pe", lambda e, sub=sub, ab=ab: e.matmul(
                                            out=psS[:, sbh, so + sub * 128:so + (sub + 1) * 128], lhsT=identb[:], rhs=negb[:, ab, :], start=False, stop=True),
                                            reads=[B_cb], writes=[B_psQ[k4]])
                                ko = kbo + ((u * 3 + g) * 8 + pi) * 4
                                for sub in range(4):
                                    P.op("act", lambda e, sub=sub: e.activation(out=pT[:, sbh, so + sub * 128:so + (sub + 1) * 128], in_=psS[:, sbh, so + sub * 128:so + (sub + 1) * 128], func=AF.Exp,
                                                                                bias=cst[:, ko + sub:ko + sub + 1], scale=SCALE),
                                         reads=[B_psQ[k4], B_cst], writes=[B_pTQ[k4]])

                            def b_fn(g=g, d=d, nb=nb, pi=pi, hbf=hbf, k4=k4, sbh=sbh, so=so, ob=ob, blocks=blocks, B_accb=B_accb):
                                for bi in range(2):
                                    r, n = blocks[2 * pi + bi]
                                    for ab in range(2):
                                        m = n + ab
                                        P.op("pe", lambda e, r=r, m=m, bi=bi, ab=ab: e.matmul(
                                            out=psO[:, ob, bi * 256:bi * 256 + 128], lhsT=vt[:, hbf, r * (nb + 1) + m, :],
                                            rhs=pT[:, sbh, so + (bi * 2 + ab) * 128:so + (bi * 2 + ab + 1) * 128], start=(ab == 0), stop=(ab == 1)),
                                            reads=[B_vt[hbf], B_pTQ[k4]], writes=[B_psO[ob]])
                                    for ab in range(2):
                                        P.op("pe", lambda e, bi=bi, ab=ab: e.matmul(
                                            out=psO[:, ob, bi * 256 + 128:bi * 256 + 256], lhsT=onesb[:],
                                            rhs=pT[:, sbh, so + (bi * 2 + ab) * 128:so + (bi * 2 + ab + 1) * 128], start=(ab == 0), stop=(ab == 1)),
                                            reads=[B_cb, B_pTQ[k4]], writes=[B_psO[ob]])
                                for bi in range(2):
                                    r, n = blocks[2 * pi + bi]
                                    q0 = n * 128 * d + r
                                    src = psO[:, ob, bi * 256:(bi + 1) * 256].rearrange("p (a b) -> p a b", b=128)
                                    dst = acc[:, :, q0:q0 + 127 * d + 1:d]
                                    bb = B_accb[g][2 * pi + bi]
                                    if g == 0:
                                        P.op("dve", lambda e, src=src, dst=dst: e.tensor_copy(out=dst, in_=src), reads=[B_psO[ob]], writes=[bb])
                                    else:
                                        P.op("dve", lambda e, src=src, dst=dst: e.tensor_tensor(out=dst, in0=dst, in1=src, op=ALU.add), reads=[B_psO[ob]], writes=[bb])

                            push2(a_fn, b_fn, 2)
                    drain2()
                    allacc = [bb for gl in B_accb for bb in gl]
                    B_r = Buf("rDfull")
                    P.op("dve", lambda e: e.reciprocal(out=rD[:], in_=acc[:, 1, :]), reads=allacc, writes=[B_r])
                    P.op("dve", lambda e, hs=hs: e.tensor_tensor(out=oTt[:, 4 + hs, :], in0=acc[:, 0, :], in1=rD[:], op=ALU.mult), reads=allacc + [B_r], writes=B_oTn[4 + hs])
                for j in range(8):
                    P.dma("sp", lambda e, j=j, u=u: e.dma_start(out=oT[j * 128:(j + 1) * 128, u * U:(u + 1) * U], in_=oTt[:, j, :]), reads=B_oTn[j])
            P.flush(include_deferred=True)
        es_z.close()

        with ExitStack() as es:
            wab = es.enter_context(nc.sbuf_tensor("sb_wab", [128, 8, D], BF16))
            wo = es.enter_context(nc.sbuf_tensor("sb_wo", [128, 16, D], BF16))
            wrt = es.enter_context(nc.sbuf_tensor("sb_wrt", [128, 16, 36], F32))
            g2bc = es.enter_context(nc.sbuf_tensor("sb_g2bc", [128, D], F32))
            oc = es.enter_context(nc.sbuf_tensor("sb_oc", [128, 8, 256], BF16))
            gc = es.enter_context(nc.sbuf_tensor("sb_gc", [128, 32, 256], BF16))
            t12 = es.enter_context(nc.sbuf_tensor("sb_t12", [128, 2, 2, 256], F32))
            mix = es.enter_context(nc.sbuf_tensor("sb_mix", [128, 16, 256], BF16))
            x1 = es.enter_context(nc.sbuf_tensor("sb_x1", [128, 1, D], F32))
            h2f = es.enter_context(nc.sbuf_tensor("sb_h2f", [128, D], F32))
            h2b = es.enter_context(nc.sbuf_tensor("sb_h2b", [128, 2, D], BF16))
            h2T = es.enter_context(nc.sbuf_tensor("sb_h2T", [128, 16, 128], F32))
            rs2 = es.enter_context(nc.sbuf_tensor("sb_rs", [128, 2, 96], F32))
            lg2 = es.enter_context(nc.sbuf_tensor("sb_lg", [128, 2, 36], F32))
            ohb2 = es.enter_context(nc.sbuf_tensor("sb_ohb", [128, 2, 32], BF16))
            oh2 = es.enter_context(nc.sbuf_tensor("sb_oh", [128, 2, 3, 32], F32))
            tot = es.enter_context(nc.sbuf_tensor("sb_tot", [128, 32], F32))
            pos2 = es.enter_context(nc.sbuf_tensor("sb_pos", [128, 2, 32], F32))
            pya = es.enter_context(nc.psum_tensor("ps_pya", [128, 2, 512], F32))
            pout = es.enter_context(nc.psum_tensor("ps_pout", [128, 2, 512], F32))
            ptr3 = es.enter_context(nc.psum_tensor("ps_ptr3", [128, 2, 512], F32))
            plg = es.enter_context(nc.psum_tensor("ps_plg", [128, 512], F32))
            B_w3 = Buf("w3")
            B_oc = Buf("oc")
            B_gc = Buf("gc")
            B_t12 = [Buf("t120"), Buf("t121")]
            B_mix = Buf("mix")
            B_x3 = [Buf("x30"), Buf("x31")]
            B_x1 = [Buf("x10"), Buf("x11")]
            B_h2f = Buf("h2f")
            B_h2b = [Buf("h2b0"), Buf("h2b1")]
            B_h2T = Buf("h2T")
            B_rs = Buf("rs")
            B_lg = Buf("lg")
            B_oh = Buf("oh")
            B_tot = Buf("tot")
            B_pos = Buf("pos")
            B_pya = [Buf("pya0"), Buf("pya1")]
            B_pout = [Buf("pout0"), Buf("pout1")]
            B_ptr3 = [Buf("ptr30"), Buf("ptr31")]
            B_plg = Buf("plg")
            P.dma("pool", lambda e: e.dma_start(out=wab[:], in_=w_ab.rearrange("(c p) n -> p c n", p=128)), writes=[B_w3])
            for q4 in range(4):
                P.dma("pool", lambda e, q4=q4: e.dma_start(out=wo[:, q4 * 4:(q4 + 1) * 4, :], in_=w_out[q4 * 512:(q4 + 1) * 512, :].rearrange("(c p) n -> p c n", p=128)), writes=[B_w3])
            P.dma("sp", lambda e: e.dma_start(out=wrt[:], in_=wr_d.rearrange("(c p) n -> p c n", p=128)), writes=[B_w3])
            P.op("dve", lambda e: e.memset(tot[:], 0.0), writes=[B_tot])
            c3 = {"t": 0, "x": 0, "o": 0, "tr": 0, "h": 0}
            P.dma("sp", lambda e: e.dma_start(out=g2bc[:], in_=g12_d[1, :].partition_broadcast(128)), writes=[B_g])
            B_rs2 = [Buf("rs0"), Buf("rs1")]
            B_lg2 = [Buf("lg0"), Buf("lg1")]
            B_oh2 = [Buf("oh0"), Buf("oh1")]
            B_pos2 = [Buf("pos0"), Buf("pos1")]
            B_plg2 = [Buf("plg0"), Buf("plg1")]

            def branch_pair(row0):
                P.dma("sp", lambda e: e.dma_start(out=oc[:], in_=oT[:, row0:row0 + 256].rearrange("(j p) t -> p j t", p=128)), writes=[B_oc])
                P.dma("sp", lambda e: e.dma_start(out=gc[:], in_=gT[:, row0:row0 + 256].rearrange("(j p) t -> p j t", p=128)), writes=[B_gc])
                for ft in range(16):
                    tb = c3["t"] % 2
                    c3["t"] += 1
                    for br in range(2):
                        for c in range(4):
                            P.op("pe", lambda e, br=br, c=c, ft=ft: e.matmul(out=pya[:, br, 0:256], lhsT=wab[:, br * 4 + c, ft * 128:(ft + 1) * 128], rhs=oc[:, br * 4 + c, :], start=(c == 0), stop=(c == 3)),
                                 reads=[B_w3, B_oc], writes=[B_pya[br]])
                        P.op("dve", lambda e, br=br, ft=ft, tb=tb: e.tensor_tensor(out=t12[:, tb, br, :], in0=pya[:, br, 0:256], in1=gc[:, br * 16 + ft, :], op=ALU.mult),
                             reads=[B_pya[br], B_gc], writes=[B_t12[tb]])
                    P.op("pool", lambda e, ft=ft, tb=tb: e.tensor_tensor(out=mix[:, ft, :], in0=t12[:, tb, 0, :], in1=t12[:, tb, 1, :], op=ALU.add), reads=[B_t12[tb]], writes=[B_mix])

            def pre_part(tix, s_):
                row0 = tix * 128
                rs = rs2[:, s_, :]
                P.dma("sp", lambda e: e.dma_start(out=x1[:, 0, :], in_=x_own[row0:row0 + 128, :]), writes=[B_x1[0]])
                for cb in range(4):
                    ob = c3["o"] % 2
                    c3["o"] += 1
                    for ft in range(16):
                        P.op("pe", lambda e, ft=ft, cb=cb, ob=ob: e.matmul(out=pout[:, ob, :], lhsT=mix[:, ft, s_ * 128:(s_ + 1) * 128], rhs=wo[:, ft, cb * 512:(cb + 1) * 512], start=(ft == 0), stop=(ft == 15)),
                             reads=[B_mix, B_w3], writes=[B_pout[ob]])
                    P.op("dve", lambda e, cb=cb, ob=ob: e.tensor_tensor(out=x1[:, 0, cb * 512:(cb + 1) * 512], in0=pout[:, ob, :], in1=x1[:, 0, cb * 512:(cb + 1) * 512], op=ALU.add),
                         reads=[B_pout[ob], B_x1[0]], writes=[B_x1[0]])
                P.dma("sp", lambda e: e.dma_start(out=x1s[row0:row0 + 128, :], in_=x1[:, 0, :]), reads=[B_x1[0]])
                P.op("dve", lambda e: e.memset(rs[:, 0:1], 0.0), writes=[B_rs2[s_]])
                P.op("dve", lambda e: e.memset(rs[:, 5:6], 0.0), writes=[B_rs2[s_]])
                P.op("act", lambda e: e.activation(out=h2b[:, s_, :], in_=x1[:, 0, :], func=AF.Square, accum_out=rs[:, 0:1]), reads=[B_x1[0], B_rs2[s_]], writes=[B_h2b[s_], B_rs2[s_]])
                P.op("act", lambda e: e.activation(out=rs[:, 1:2], in_=rs[:, 0:1], func=AF.Sqrt, bias=EPS, scale=1.0 / D), reads=[B_rs2[s_]], writes=[B_rs2[s_]])
                P.op("dve", lambda e: e.reciprocal(out=rs[:, 2:3], in_=rs[:, 1:2]), reads=[B_rs2[s_]], writes=[B_rs2[s_]])
                P.op("dve", lambda e: e.scalar_tensor_tensor(out=h2f[:], in0=x1[:, 0, :], scalar=rs[:, 2:3], in1=g2bc[:], op0=ALU.mult, op1=ALU.mult),
                     reads=[B_x1[0], B_rs2[s_], B_g], writes=[B_h2f])
                P.op("act", lambda e: e.copy(out=h2b[:, s_, :], in_=h2f[:]), reads=[B_h2f], writes=[B_h2b[s_]])
                for q4 in range(4):
                    pb = c3["tr"] % 2
                    c3["tr"] += 1
                    for j in range(4):
                        c = q4 * 4 + j
                        P.op("pe", lambda e, c=c, j=j, pb=pb: e.transpose(out=ptr3[:, pb, j * 128:(j + 1) * 128], in_=h2f[:, c * 128:(c + 1) * 128], identity=identf), reads=[B_h2f, B_cst], writes=[B_ptr3[pb]])
                    P.op("act", lambda e, q4=q4, pb=pb: e.copy(out=h2T[:, q4 * 4:(q4 + 1) * 4, :], in_=ptr3[:, pb, :].rearrange("p (a b) -> p a b", b=128)), reads=[B_ptr3[pb]], writes=[B_h2T])
                for c in range(16):
                    P.op("pe", lambda e, c=c: e.matmul(out=plg[:, s_ * 256:s_ * 256 + 36], lhsT=h2T[:, c, :], rhs=wrt[:, c, :], start=(c == 0), stop=(c == 15)), reads=[B_h2T, B_w3], writes=[B_plg2[s_]])
                P.op("dve", lambda e: e.tensor_tensor(out=lg2[:, s_, :], in0=plg[:, s_ * 256:s_ * 256 + 36], in1=cs(cst, "rbias"), op=ALU.add), reads=[B_plg2[s_], B_cst], writes=[B_lg2[s_]])

            def chain1(tix, s_):
                rs = rs2[:, s_, :]
                lg = lg2[:, s_, :]
                oh = oh2[:, s_, :, :]
                ohb = ohb2[:, s_, :]
                R = [B_rs2[s_], B_lg2[s_], B_oh2[s_]]
                ops = []

                def dv(fn, extra_r=(), extra_w=()):
                    ops.append(("dve", fn, R + list(extra_r), R + list(extra_w)))
                dv(lambda e: e.reduce_max(out=rs[:, 3:4], in_=lg[:, 0:4], axis=AX.X))
                dv(lambda e: e.tensor_scalar(out=rs[:, 8:12], in0=lg[:, 0:4], scalar1=rs[:, 3:4], scalar2=None, op0=ALU.is_equal))
                dv(lambda e: e.tensor_scalar(out=rs[:, 4:5], in0=rs[:, 3:4], scalar1=-1.0, scalar2=None, op0=ALU.mult))
                ops.append(("act", lambda e: e.activation(out=rs[:, 12:16], in_=lg[:, 0:4], func=AF.Exp, bias=rs[:, 4:5], scale=1.0, accum_out=rs[:, 5:6]), R, R))
                dv(lambda e: e.reciprocal(out=rs[:, 6:7], in_=rs[:, 5:6]))
                dv(lambda e: e.tensor_scalar(out=rs[:, 16:24], in0=lg[:, 4:12], scalar1=rs[:, 8:9], scalar2=None, op0=ALU.mult))
                for g_ in range(1, 4):
                    dv(lambda e, g_=g_: e.scalar_tensor_tensor(out=rs[:, 16:24], in0=lg[:, 4 + 8 * g_:12 + 8 * g_], scalar=rs[:, 8 + g_:9 + g_], in1=rs[:, 16:24], op0=ALU.mult, op1=ALU.add))
                dv(lambda e: e.reduce_max(out=rs[:, 24:25], in_=rs[:, 16:24], axis=AX.X))
                dv(lambda e: e.tensor_scalar(out=rs[:, 32:40], in0=rs[:, 16:24], scalar1=rs[:, 24:25], scalar2=None, op0=ALU.is_equal))
                dv(lambda e: e.scalar_tensor_tensor(out=rs[:, 40:48], in0=rs[:, 32:40], scalar=-1e30, in1=rs[:, 16:24], op0=ALU.mult, op1=ALU.add))
                dv(lambda e: e.reduce_max(out=rs[:, 25:26], in_=rs[:, 40:48], axis=AX.X))
                dv(lambda e: e.tensor_scalar(out=rs[:, 48:56], in0=rs[:, 40:48], scalar1=rs[:, 25:26], scalar2=None, op0=ALU.is_equal))
                dv(lambda e: e.tensor_scalar(out=rs[:, 26:27], in0=rs[:, 24:25], scalar1=-1.0, scalar2=None, op0=ALU.mult))
                ops.append(("act", lambda e: e.activation(out=rs[:, 27:28], in_=rs[:, 25:26], func=AF.Exp, bias=rs[:, 26:27], scale=1.0), R, R))
                dv(lambda e: e.tensor_scalar(out=rs[:, 28:29], in0=rs[:, 27:28], scalar1=1.0, scalar2=None, op0=ALU.add))
                dv(lambda e: e.reciprocal(out=rs[:, 29:30], in_=rs[:, 28:29]))
                dv(lambda e: e.tensor_tensor(out=wts_t[:, tix, 0:1], in0=rs[:, 29:30], in1=rs[:, 6:7], op=ALU.mult), extra_w=[B_idx])
                dv(lambda e: e.tensor_tensor(out=wts_t[:, tix, 1:2], in0=wts_t[:, tix, 0:1], in1=rs[:, 27:28], op=ALU.mult), extra_r=[B_idx], extra_w=[B_idx])
                dv(lambda e: e.tensor_tensor(out=rs[:, 56:60], in0=rs[:, 8:12], in1=cs(cst, "iota4"), op=ALU.mult), extra_r=[B_cst])
                dv(lambda e: e.reduce_sum(out=rs[:, 60:61], in_=rs[:, 56:60], axis=AX.X))
                dv(lambda e: e.tensor_tensor(out=rs[:, 64:72], in0=rs[:, 32:40], in1=cs(cst, "iota8"), op=ALU.mult), extra_r=[B_cst])
                dv(lambda e: e.reduce_sum(out=rs[:, 61:62], in_=rs[:, 64:72], axis=AX.X))
                dv(lambda e: e.tensor_tensor(out=rs[:, 72:80], in0=rs[:, 48:56], in1=cs(cst, "iota8"), op=ALU.mult), extra_r=[B_cst])
                dv(lambda e: e.reduce_sum(out=rs[:, 62:63], in_=rs[:, 72:80], axis=AX.X))
                dv(lambda e: e.scalar_tensor_tensor(out=rs[:, 80:81], in0=rs[:, 60:61], scalar=8.0, in1=rs[:, 61:62], op0=ALU.mult, op1=ALU.add))
                dv(lambda e: e.scalar_tensor_tensor(out=rs[:, 81:82], in0=rs[:, 60:61], scalar=8.0, in1=rs[:, 62:63], op0=ALU.mult, op1=ALU.add))
                dv(lambda e: e.tensor_scalar(out=oh[:, 0, :], in0=cs(cst, "iota32"), scalar1=rs[:, 80:81], scalar2=None, op0=ALU.is_equal), extra_r=[B_cst])
                dv(lambda e: e.tensor_scalar(out=oh[:, 1, :], in0=cs(cst, "iota32"), scalar1=rs[:, 81:82], scalar2=None, op0=ALU.is_equal), extra_r=[B_cst])
                dv(lambda e: e.tensor_tensor(out=ohb, in0=oh[:, 0, :], in1=oh[:, 1, :], op=ALU.add))
                return ops

            def chain2(tix, s_):
                ohb = ohb2[:, s_, :]
                pos = pos2[:, s_, :]
                o_ = s_ * 256
                P.op("pe", lambda e: e.matmul(out=plg[:, o_ + 64:o_ + 96], lhsT=ustrb[:], rhs=ohb, start=True, stop=True), reads=[B_oh2[s_], B_cb], writes=[B_plg2[s_]])
                P.op("pe", lambda e: e.matmul(out=plg[:, o_ + 128:o_ + 160], lhsT=onesb[:], rhs=ohb, start=True, stop=True), reads=[B_oh2[s_], B_cb], writes=[B_plg2[s_]])
                P.op("dve", lambda e: e.tensor_tensor(out=pos, in0=plg[:, o_ + 64:o_ + 96], in1=tot[:], op=ALU.add), reads=[B_plg2[s_], B_tot], writes=[B_pos2[s_]])
                P.op("dve", lambda e: e.tensor_tensor(out=tot[:], in0=plg[:, o_ + 128:o_ + 160], in1=tot[:], op=ALU.add), reads=[B_plg2[s_], B_tot], writes=[B_tot])

            def chain3(tix, s_):
                rs = rs2[:, s_, :]
                oh = oh2[:, s_, :, :]
                pos = pos2[:, s_, :]
                R = [B_rs2[s_], B_oh2[s_]]
                ops = []

                def dv(fn, extra_r=(), extra_w=()):
                    ops.append(("dve", fn, R + list(extra_r), R + list(extra_w)))
                for k_ in range(2):
                    dv(lambda e, k_=k_: e.tensor_tensor(out=oh[:, 2, :], in0=oh[:, k_, :], in1=pos, op=ALU.mult), extra_r=[B_pos2[s_]])
                    dv(lambda e, k_=k_: e.reduce_sum(out=rs[:, 84 + k_:85 + k_], in_=oh[:, 2, :], axis=AX.X))
                    dv(lambda e, k_=k_: e.tensor_scalar(out=rs[:, 84 + k_:85 + k_], in0=rs[:, 84 + k_:85 + k_], scalar1=float(CAP - 1), scalar2=None, op0=ALU.min))
                    dv(lambda e, k_=k_: e.scalar_tensor_tensor(out=rs[:, 88 + k_:89 + k_], in0=rs[:, 80 + k_:81 + k_], scalar=float(CAP), in1=rs[:, 84 + k_:85 + k_], op0=ALU.mult, op1=ALU.add))
                    dv(lambda e, k_=k_: e.tensor_copy(out=idx_t[:, tix, k_:k_ + 1], in_=rs[:, 88 + k_:89 + k_]), extra_w=[B_idx])
                return ops

            def interleave(la, lb):
                for oa, ob_ in zip(la, lb):
                    P.op(oa[0], oa[1], reads=oa[2], writes=oa[3])
                    P.op(ob_[0], ob_[1], reads=ob_[2], writes=ob_[3])

            for pr in range(NT // 256):
                tA, tB = 2 * pr, 2 * pr + 1
                branch_pair(pr * 256)
                pre_part(tA, 0)
                pre_part(tB, 1)
                interleave(chain1(tA, 0), chain1(tB, 1))
                chain2(tA, 0)
                chain2(tB, 1)
                interleave(chain3(tA, 0), chain3(tB, 1))
                for (tix, s_) in ((tA, 0), (tB, 1)):
                    for k_ in range(2):
                        P.dma("pool", lambda e, k_=k_, tix=tix, s_=s_: e.indirect_dma_start(
                            out=xbuf[:, :], out_offset=bass.IndirectOffsetOnAxis(ap=idx_t[:, tix, k_:k_ + 1], axis=0),
                            in_=h2b[:, s_, :], in_offset=None),
                            reads=[B_h2b[s_], B_idx])
            if debug:
                P.dma("sp", lambda e: e.dma_start(out=dbg[:, 0:32], in_=tot[:]), reads=[B_tot])
            P.flush()

        NS = CAP // 128
        with ExitStack() as es:
            wg = es.enter_context(nc.sbuf_tensor("sb_wg", [128, 2, 16, 512], BF16))
            wu = es.enter_context(nc.sbuf_tensor("sb_wu", [128, 2, 16, 512], BF16))
            wd = es.enter_context(nc.sbuf_tensor("sb_wd", [128, 2, 4, D], BF16))
            xg = es.enter_context(nc.sbuf_tensor("sb_xg", [128, NS, D], BF16))
            xgT = es.enter_context(nc.sbuf_tensor("sb_xgT", [128, 16, CAP], BF16))
            sa = es.enter_context(nc.sbuf_tensor("sb_sa", [128, 2, CAP], F32))
            hTe = es.enter_context(nc.sbuf_tensor("sb_hTe", [128, 4, CAP], BF16))
            osb = es.enter_context(nc.sbuf_tensor("sb_osb", [128, NS, D], F32))
            ptr4 = es.enter_context(nc.psum_tensor("ps_ptr4", [128, 2, 8, 128], BF16))
            pau = es.enter_context(nc.psum_tensor("ps_pau", [128, 4, 512], F32))
            pdn = es.enter_context(nc.psum_tensor("ps_pdn", [128, 2, 512], F32))
            B_wgu = [Buf("wgu0"), Buf("wgu1")]
            B_wd = [Buf("wd0"), Buf("wd1")]
            B_xg = Buf("xg")
            B_xgT = Buf("xgT")
            B_sa = [Buf("sa0"), Buf("sa1")]
            B_hTe = Buf("hTe")
            B_osb = Buf("osb")
            B_ptr4 = [Buf("ptr40"), Buf("ptr41")]
            B_pau = [Buf("pa0"), Buf("pu0"), Buf("pa1"), Buf("pu1")]
            B_pdn = [Buf("pdn0"), Buf("pdn1")]
            c4 = {"w": 0, "tr": 0, "au": 0, "dn": 0}

            def load_w(e_, hh):
                wb = c4["w"] % 2
                c4["w"] += 1
                for (dst, src) in ((wg, w_gate), (wu, w_up)):
                    for q2 in range(2):
                        P.dma("pool", lambda e, dst=dst, src=src, e_=e_, hh=hh, wb=wb, q2=q2: e.dma_start(
                            out=dst[:, wb, q2 * 8:(q2 + 1) * 8, :], in_=src[e_ * D + q2 * 1024:e_ * D + (q2 + 1) * 1024, hh * 512:(hh + 1) * 512].rearrange("(c p) n -> p c n", p=128)),
                            writes=[B_wgu[wb]])
                P.dma("pool", lambda e, e_=e_, hh=hh, wb=wb: e.dma_start(
                    out=wd[:, wb, :, :], in_=w_down[e_ * DEXP + hh * 512:e_ * DEXP + (hh + 1) * 512, :].rearrange("(c p) n -> p c n", p=128)),
                    writes=[B_wd[wb]])
                return wb

            for e_ in range(NEXP):
                P.dma("sp", lambda e, e_=e_: e.dma_start(out=xg[:], in_=xbuf[e_ * CAP:(e_ + 1) * CAP, :].rearrange("(t p) d -> p t d", p=128)), writes=[B_xg])
                wbs = [load_w(e_, 0)]
                for st_ in range(NS):
                    for half in range(2):
                        pb = c4["tr"] % 2
                        c4["tr"] += 1
                        for j in range(8):
                            c = half * 8 + j
                            P.op("pe", lambda e, st_=st_, c=c, j=j, pb=pb: e.transpose(out=ptr4[:, pb, j, :], in_=xg[:, st_, c * 128:(c + 1) * 128], identity=identb[:]), reads=[B_xg, B_cb], writes=[B_ptr4[pb]])
                        if half == 0:
                            P.op("act", lambda e, st_=st_, half=half, pb=pb: e.copy(out=xgT[:, half * 8:(half + 1) * 8, st_ * 128:(st_ + 1) * 128], in_=ptr4[:, pb, :, :]), reads=[B_ptr4[pb]], writes=[B_xgT])
                        else:
                            P.op("dve", lambda e, st_=st_, half=half, pb=pb: e.tensor_copy(out=xgT[:, half * 8:(half + 1) * 8, st_ * 128:(st_ + 1) * 128], in_=ptr4[:, pb, :, :]), reads=[B_ptr4[pb]], writes=[B_xgT])
                for hh in range(2):
                    wb = wbs[hh]
                    if hh == 0:
                        wbs.append(load_w(e_, 1))
                    for ht in range(4):
                        ab = c4["au"] % 2
                        c4["au"] += 1
                        for (wsrc, pi_) in ((wg, 0), (wu, 1)):
                            for c in range(16):
                                P.op("pe", lambda e, wsrc=wsrc, pi_=pi_, c=c, ht=ht, wb=wb, ab=ab: e.matmul(out=pau[:, ab * 2 + pi_, 0:CAP], lhsT=wsrc[:, wb, c, ht * 128:(ht + 1) * 128], rhs=xgT[:, c, :], start=(c == 0), stop=(c == 15)),
                                     reads=[B_wgu[wb], B_xgT], writes=[B_pau[ab * 2 + pi_]])
                        P.op("act", lambda e, ab=ab: e.activation(out=sa[:, ab, :], in_=pau[:, ab * 2, 0:CAP], func=AF.Silu), reads=[B_pau[ab * 2]], writes=[B_sa[ab]])
                        P.op("dve", lambda e, ab=ab, ht=ht: e.tensor_tensor(out=hTe[:, ht, :], in0=pau[:, ab * 2 + 1, 0:CAP], in1=sa[:, ab, :], op=ALU.mult), reads=[B_pau[ab * 2 + 1], B_sa[ab]], writes=[B_hTe])
                    for st_ in range(NS):
                        for cb in range(4):
                            db = c4["dn"] % 2
                            c4["dn"] += 1
                            for ht in range(4):
                                P.op("pe", lambda e, st_=st_, cb=cb, ht=ht, wb=wb, db=db: e.matmul(out=pdn[:, db, :], lhsT=hTe[:, ht, st_ * 128:(st_ + 1) * 128], rhs=wd[:, wb, ht, cb * 512:(cb + 1) * 512], start=(ht == 0), stop=(ht == 3)),
                                     reads=[B_hTe, B_wd[wb]], writes=[B_pdn[db]])
                            if hh == 0:
                                P.op("act", lambda e, st_=st_, cb=cb, db=db: e.copy(out=osb[:, st_, cb * 512:(cb + 1) * 512], in_=pdn[:, db, :]), reads=[B_pdn[db]], writes=[B_osb])
                            else:
                                P.op("dve", lambda e, st_=st_, cb=cb, db=db: e.tensor_tensor(out=osb[:, st_, cb * 512:(cb + 1) * 512], in0=pdn[:, db, :], in1=osb[:, st_, cb * 512:(cb + 1) * 512], op=ALU.add), reads=[B_pdn[db], B_osb], writes=[B_osb])
                P.dma("sp", lambda e, e_=e_: e.dma_start(out=ybuf[e_ * CAP:(e_ + 1) * CAP, :].rearrange("(t p) d -> p t d", p=128), in_=osb[:]), reads=[B_osb])
            P.flush()

        with ExitStack() as es:
            y1 = es.enter_context(nc.sbuf_tensor("sb_y1", [128, 2, 2, D], F32))
            x5 = es.enter_context(nc.sbuf_tensor("sb_x5", [128, 2, D], F32))
            o5 = es.enter_context(nc.sbuf_tensor("sb_o5", [128, 2, D], F32))
            B_y1 = [Buf("y10"), Buf("y11")]
            B_x5 = [Buf("x50"), Buf("x51")]
            B_o5 = [Buf("o50"), Buf("o51")]
            for t in range(NT // 128):
                b = t % 2
                for k_ in range(2):
                    P.dma("pool", lambda e, t=t, k_=k_, b=b: e.indirect_dma_start(
                        out=y1[:, b, k_, :], out_offset=None, in_=ybuf[:, :],
                        in_offset=bass.IndirectOffsetOnAxis(ap=idx_t[:, t, k_:k_ + 1], axis=0)),
                        reads=[B_idx], writes=[B_y1[b]])
                P.dma("sp", lambda e, t=t, b=b: e.dma_start(out=x5[:, b, :], in_=x1s[t * 128:(t + 1) * 128, :]), writes=[B_x5[b]])
                P.op("dve", lambda e, t=t, b=b: e.scalar_tensor_tensor(out=o5[:, b, :], in0=y1[:, b, 0, :], scalar=wts_t[:, t, 0:1], in1=x5[:, b, :], op0=ALU.mult, op1=ALU.add),
                     reads=[B_y1[b], B_x5[b], B_idx], writes=[B_o5[b]])
                P.op("dve", lambda e, t=t, b=b: e.scalar_tensor_tensor(out=o5[:, b, :], in0=y1[:, b, 1, :], scalar=wts_t[:, t, 1:2], in1=o5[:, b, :], op0=ALU.mult, op1=ALU.add),
                     reads=[B_y1[b], B_o5[b], B_idx], writes=[B_o5[b]])
                P.dma("sp", lambda e, t=t, b=b: e.dma_start(out=y_out[t * 128:(t + 1) * 128, :], in_=o5[:, b, :]), reads=[B_o5[b]])
            P.flush()
    P.close()
    return nc


def _core_inputs(c, NU, HALO, x_prompt, x_sample, shared):
    xs = [x_prompt[c]]
    if NU == 3:
        s = c // 4
        s0 = (c % 4) * 4096
        xs.append(x_sample[s, s0:s0 + 4096])
    m = {"x_own": np.ascontiguousarray(np.concatenate(xs, 0))}
    if HALO:
        s = c // 4
        s0 = (c % 4) * 4096
        halo = np.zeros((2048, D), np.float32)
        if s0 > 0:
            halo[0:1024] = x_sample[s, s0 - 1024:s0]
        if s0 + 4096 < 16384:
            halo[1024:2048] = x_sample[s, s0 + 4096:s0 + 5120]
        m["x_halo"] = halo
    m["cst"] = _build_cst(c, NU, shared["rbias"], shared["gqk"])
    m["rope"] = _rope_tables(c, NU, HALO).reshape(-1, U)
    for k in ("g12", "w_in", "w_ab", "w_out", "wr", "rpb", "w_gate", "w_up", "w_down"):
        m[k] = shared[k]
    return m


def _shared_inputs(norm1_g, w_in, qn_a, kn_a, rpb_a, qn_b, kn_b, w_branch_a, w_branch_b, w_out, norm2_g,
                   router_group_w, router_group_b, router_expert_w, router_expert_b, w_gate, w_up, w_down):
    f = lambda a: np.ascontiguousarray(np.asarray(a, np.float32))
    return {
        "g12": f(np.stack([norm1_g[0], norm2_g[0]], 0)),
        "w_in": f(w_in[0]),
        "w_ab": f(np.concatenate([w_branch_a[0], w_branch_b[0]], 0)),
        "w_out": f(w_out[0]),
        "wr": f(np.concatenate([router_group_w[0], router_expert_w[0]], 1)),
        "rbias": f(np.concatenate([router_group_b[0], router_expert_b[0]], 0)),
        "gqk": f(np.stack([qn_a[0], kn_a[0], qn_b[0], kn_b[0]], 1)),
        "rpb": f(rpb_a[0].reshape(60, 31)),
        "w_gate": f(w_gate[0].reshape(NEXP * D, DEXP)),
        "w_up": f(w_up[0].reshape(NEXP * D, DEXP)),
        "w_down": f(w_down[0].reshape(NEXP * DEXP, D)),
    }


def kernel(x_prompt, x_sample, norm1_g, w_in, qn_a, kn_a, rpb_a, qn_b, kn_b, w_branch_a, w_branch_b, w_out,
           norm2_g, router_group_w, router_group_b, router_expert_w, router_expert_b, w_gate, w_up, w_down):
    x_prompt = np.asarray(x_prompt, np.float32)
    x_sample = np.asarray(x_sample, np.float32)
    shared = _shared_inputs(norm1_g, w_in, qn_a, kn_a, rpb_a, qn_b, kn_b, w_branch_a, w_branch_b, w_out, norm2_g,
                            router_group_w, router_group_b, router_expert_w, router_expert_b, w_gate, w_up, w_down)
    NU, CAP, HALO = 3, 512, True
    nc = build_program(NU, CAP, HALO)
    in_maps = [_core_inputs(c, NU, HALO, x_prompt, x_sample, shared) for c in range(8)]
    res = run_bass_kernel_spmd(nc, in_maps, core_ids=list(range(8)))
    y_prompt = np.empty((8, 2048, D), np.float32)
    y_sample = np.empty((2, 16384, D), np.float32)
    for c in range(8):
        y = res.results[c]["y"]
        y_prompt[c] = y[0:2048]
        s0 = (c % 4) * 4096
        y_sample[c // 4, s0:s0 + 4096] = y[2048:6144]
    return (y_prompt, y_sample)
```
